# Optimizing a Trainium2 kernel written in Bass

```python
import math
import jax, jax.numpy as jnp
from jax import lax
import numpy as np

D_MODEL = 2048
BATCH = 1
SEQ = 8192
DEPTH = 4

HEAD_DIM = 64
ROT_DIM = HEAD_DIM // 4
ROPE_THETA = 500000.0
Q_BLOCK = 128
MOBA_HEADS = 8
MOBA_BLOCK = 256
MOBA_TOPK = 3
SSM_HEADS = 8
SSM_HEAD_DIM = 64
SSM_INNER = SSM_HEADS * SSM_HEAD_DIM
SSM_STATE = 128
SSM_GROUPS = 2
SSM_CONV = 4
SSM_CONV_CH = SSM_INNER + 2 * SSM_GROUPS * SSM_STATE
SSM_CHUNK = 256
DSA_HEADS = 8
IDX_HEADS = 4
IDX_DIM = 64
DSA_TOPK = 256
IDX_SCALE = (IDX_HEADS * IDX_DIM) ** -0.5
MLA_HEADS = 8
MLA_Q_RANK = 384
MLA_KV_RANK = 256
MLA_NOPE = 64
MLA_ROPE = 32
MLA_V = 64
MLA_THETA = 10000.0
N_BRANCH = 4
BRANCH_W = 512
SPLIT_SIZES = (
    MOBA_HEADS * HEAD_DIM, MOBA_HEADS * HEAD_DIM, MOBA_HEADS * HEAD_DIM,
    SSM_INNER, SSM_CONV_CH, SSM_HEADS,
    DSA_HEADS * HEAD_DIM, DSA_HEADS * HEAD_DIM, DSA_HEADS * HEAD_DIM,
    IDX_HEADS * IDX_DIM, IDX_DIM, IDX_HEADS,
    MLA_Q_RANK, MLA_KV_RANK, MLA_ROPE,
    N_BRANCH * D_MODEL,
)
D_IN = sum(SPLIT_SIZES)
N_EXPERTS = 32
TOP_K = 4
D_FF = 768
SWIGLU_ALPHA = 1.702
SWIGLU_LIMIT = 7.0
MOE_BLOCK = 128
DN_ALPHA = (2 * DEPTH) ** 0.25
DN_BETA = (8 * DEPTH) ** -0.25
LN_EPS = 1e-5

kernel_name = 'hybrid_gated_moba_ssd_dsa_mla_moe'


def layer_norm(x, g, b):
    xf = x.astype(jnp.float32)
    mu = jnp.mean(xf, axis=-1, keepdims=True)
    var = jnp.mean(jnp.square(xf - mu), axis=-1, keepdims=True)
    return ((xf - mu) * lax.rsqrt(var + LN_EPS) * g.astype(jnp.float32) + b.astype(jnp.float32)).astype(x.dtype)


def rms_norm(x, w):
    xf = x.astype(jnp.float32)
    y = xf * lax.rsqrt(jnp.mean(jnp.square(xf), axis=-1, keepdims=True) + LN_EPS)
    return (y * w.astype(jnp.float32)).astype(x.dtype)


def rope_cos_sin(positions, dim, theta):
    inv = theta ** (-jnp.arange(0, dim, 2, dtype=jnp.float32) / dim)
    ang = positions.astype(jnp.float32)[..., None] * inv
    return jnp.cos(ang), jnp.sin(ang)


def rotate(x, cos, sin):
    half = x.shape[-1] // 2
    x1 = x[..., :half].astype(jnp.float32)
    x2 = x[..., half:].astype(jnp.float32)
    c = cos[:, :, None, :]
    s = sin[:, :, None, :]
    return jnp.concatenate([x1 * c - x2 * s, x2 * c + x1 * s], axis=-1).astype(x.dtype)


def partial_rope(x, cos, sin):
    return jnp.concatenate([rotate(x[..., :ROT_DIM], cos, sin), x[..., ROT_DIM:]], axis=-1)


def moba_attention(q, k, v):
    B, S, H, dh = q.shape
    nb = -(-S // MOBA_BLOCK)
    pad = nb * MOBA_BLOCK - S
    kb = jnp.pad(k, ((0, 0), (0, pad), (0, 0), (0, 0))).reshape(B, nb, MOBA_BLOCK, H, dh).transpose(0, 3, 1, 2, 4)
    vb = jnp.pad(v, ((0, 0), (0, pad), (0, 0), (0, 0))).reshape(B, nb, MOBA_BLOCK, H, dh).transpose(0, 3, 1, 2, 4)
    k_mean = jnp.mean(kb, axis=3)
    topk = min(MOBA_TOPK, nb)
    nq = S // Q_BLOCK
    qb = q.reshape(B, nq, Q_BLOCK, H, dh).transpose(1, 0, 3, 2, 4)
    scale = dh ** -0.5
    b_ix = jnp.arange(B)[:, None, None, None]
    h_ix = jnp.arange(H)[None, :, None, None]
    blk_ids = jnp.arange(nb)
    key_off = jnp.arange(MOBA_BLOCK)

    def one_block(args):
        q_blk, qi = args
        q_pos = qi * Q_BLOCK + jnp.arange(Q_BLOCK)
        own = (qi * Q_BLOCK) // MOBA_BLOCK
        gate = jnp.einsum('bhqd,bhnd->bhqn', q_blk, k_mean).astype(jnp.float32)
        gate = jnp.where(blk_ids < own, gate, -jnp.inf)
        _, sel = lax.top_k(gate, topk)
        sel_ok = sel < own
        k_sel = kb[b_ix, h_ix, sel]
        v_sel = vb[b_ix, h_ix, sel]
        s_sel = jnp.einsum('bhqd,bhqjkd->bhqjk', q_blk, k_sel).astype(jnp.float32) * scale
        s_sel = jnp.where(sel_ok[..., None], s_sel, -jnp.inf).reshape(B, H, Q_BLOCK, topk * MOBA_BLOCK)
        k_own = lax.dynamic_index_in_dim(kb, own, axis=2, keepdims=False)
        v_own = lax.dynamic_index_in_dim(vb, own, axis=2, keepdims=False)
        s_own = jnp.einsum('bhqd,bhkd->bhqk', q_blk, k_own).astype(jnp.float32) * scale
        causal = (own * MOBA_BLOCK + key_off)[None, :] <= q_pos[:, None]
        s_own = jnp.where(causal, s_own, -jnp.inf)
        p = jax.nn.softmax(jnp.concatenate([s_sel, s_own], axis=-1), axis=-1).astype(v.dtype)
        p_sel = p[..., :topk * MOBA_BLOCK].reshape(B, H, Q_BLOCK, topk, MOBA_BLOCK)
        p_own = p[..., topk * MOBA_BLOCK:]
        return (jnp.einsum('bhqjk,bhqjkd->bhqd', p_sel, v_sel)
                + jnp.einsum('bhqk,bhkd->bhqd', p_own, v_own))

    out = lax.map(one_block, (qb, jnp.arange(nq)))
    return out.transpose(1, 0, 3, 2, 4).reshape(B, S, H * dh)


def dsa_attention(q, k, v, q_idx, k_idx, w_idx):
    B, S, H, dh = q.shape
    topk = min(DSA_TOPK, S // 4)
    nq = S // Q_BLOCK
    qb = q.reshape(B, nq, Q_BLOCK, H, dh).transpose(1, 0, 2, 3, 4)
    qib = q_idx.reshape(B, nq, Q_BLOCK, IDX_HEADS, IDX_DIM).transpose(1, 0, 2, 3, 4)
    wb = w_idx.reshape(B, nq, Q_BLOCK, IDX_HEADS).transpose(1, 0, 2, 3)
    key_pos = jnp.arange(S)
    b_ix = jnp.arange(B)[:, None, None]
    scale = dh ** -0.5

    def one_block(args):
        q_blk, qi_blk, w_blk, qi = args
        q_pos = qi * Q_BLOCK + jnp.arange(Q_BLOCK)
        rel = jax.nn.relu(jnp.einsum('bqhd,bsd->bqhs', qi_blk, k_idx).astype(jnp.float32))
        score = jnp.einsum('bqh,bqhs->bqs', w_blk.astype(jnp.float32), rel)
        score = jnp.where(key_pos[None, :] <= q_pos[:, None], score, -jnp.inf)
        _, sel = lax.top_k(score, topk)
        sel_ok = sel <= q_pos[None, :, None]
        k_sel = k[b_ix, sel]
        v_sel = v[b_ix, sel]
        s = jnp.einsum('bqhd,bqkhd->bhqk', q_blk, k_sel).astype(jnp.float32) * scale
        s = jnp.where(sel_ok[:, None], s, -jnp.inf)
        p = jax.nn.softmax(s, axis=-1).astype(v.dtype)
        return jnp.einsum('bhqk,bqkhd->bqhd', p, v_sel)

    out = lax.map(one_block, (qb, qib, wb, jnp.arange(nq)))
    return out.transpose(1, 0, 2, 3, 4).reshape(B, S, H * dh)


def causal_attention_blocked(q, k, v):
    B, S, H, dqk = q.shape
    dv = v.shape[-1]
    nq = S // Q_BLOCK
    qb = q.reshape(B, nq, Q_BLOCK, H, dqk).transpose(1, 0, 2, 3, 4)
    key_pos = jnp.arange(S)
    scale = dqk ** -0.5

    def one_block(args):
        q_blk, qi = args
        q_pos = qi * Q_BLOCK + jnp.arange(Q_BLOCK)
        s = jnp.einsum('bqhd,bkhd->bhqk', q_blk, k).astype(jnp.float32) * scale
        s = jnp.where(key_pos[None, :] <= q_pos[:, None], s, -jnp.inf)
        p = jax.nn.softmax(s, axis=-1).astype(v.dtype)
        return jnp.einsum('bhqk,bkhd->bqhd', p, v)

    out = lax.map(one_block, (qb, jnp.arange(nq)))
    return out.transpose(1, 0, 2, 3, 4).reshape(B, S, H * dv)


def mla_attention(c_q, c_kv, k_pe_raw, cos_m, sin_m, q_norm_w, w_uq, kv_norm_w, w_ukv):
    B, S, _ = c_q.shape
    q = jnp.einsum('bsr,re->bse', rms_norm(c_q, q_norm_w), w_uq).reshape(B, S, MLA_HEADS, MLA_NOPE + MLA_ROPE)
    q = jnp.concatenate([q[..., :MLA_NOPE], rotate(q[..., MLA_NOPE:], cos_m, sin_m)], axis=-1)
    kv = jnp.einsum('bsr,re->bse', rms_norm(c_kv, kv_norm_w), w_ukv).reshape(B, S, MLA_HEADS, MLA_NOPE + MLA_V)
    k_pe = rotate(k_pe_raw[:, :, None, :], cos_m, sin_m)
    k = jnp.concatenate([kv[..., :MLA_NOPE], jnp.broadcast_to(k_pe, (B, S, MLA_HEADS, MLA_ROPE))], axis=-1)
    v = kv[..., MLA_NOPE:]
    return causal_attention_blocked(q, k, v)


def ssd_chunked(xdt, da, bm, cm):
    B, S, H, P = xdt.shape
    N = bm.shape[-1]
    nc = -(-S // SSM_CHUNK)
    pad = nc * SSM_CHUNK - S

    def chunked(t):
        t = jnp.pad(t, [(0, 0), (0, pad)] + [(0, 0)] * (t.ndim - 2))
        return t.reshape((B, nc, SSM_CHUNK) + t.shape[2:])

    xc = chunked(xdt)
    bc = chunked(bm)
    cc = chunked(cm)
    ac = chunked(da).transpose(0, 1, 3, 2)
    a_cum = jnp.cumsum(ac, axis=-1)
    tri = jnp.tril(jnp.ones((SSM_CHUNK, SSM_CHUNK), dtype=bool))
    seg = jnp.exp(jnp.where(tri, a_cum[..., :, None] - a_cum[..., None, :], -jnp.inf))
    scores = jnp.einsum('bclhn,bcshn->bchls', cc, bc) * seg
    y_diag = jnp.einsum('bchls,bcshp->bclhp', scores, xc)
    decay_to_end = jnp.exp(a_cum[..., -1:] - a_cum).transpose(0, 1, 3, 2)
    states = jnp.einsum('bclhn,bclhp->bchpn', bc * decay_to_end[..., None], xc)
    chunk_decay = jnp.exp(a_cum[..., -1])

    def carry_state(h, inp):
        st, dec = inp
        return h * dec[..., None, None] + st, h

    h0 = jnp.zeros((B, H, P, N), xdt.dtype)
    _, h_in = lax.scan(carry_state, h0, (jnp.moveaxis(states, 1, 0), jnp.moveaxis(chunk_decay, 1, 0)))
    h_in = jnp.moveaxis(h_in, 0, 1)
    decay_from_start = jnp.exp(a_cum).transpose(0, 1, 3, 2)
    y_off = jnp.einsum('bclhn,bchpn->bclhp', cc, h_in) * decay_from_start[..., None]
    return (y_diag + y_off).reshape(B, nc * SSM_CHUNK, H, P)[:, :S]


def mamba2_ssd(z, xbc, dt_raw, conv_w, conv_b, dt_bias, a_log, d_skip, norm_w):
    B, S, _ = xbc.shape
    conv = lax.conv_general_dilated(xbc, conv_w[:, None, :], window_strides=(1,),
                                    padding=[(SSM_CONV - 1, 0)],
                                    dimension_numbers=('NWC', 'WIO', 'NWC'),
                                    feature_group_count=SSM_CONV_CH) + conv_b
    xbc = jax.nn.silu(conv)
    xs, bm, cm = jnp.split(xbc, [SSM_INNER, SSM_INNER + SSM_GROUPS * SSM_STATE], axis=-1)
    xs = xs.reshape(B, S, SSM_HEADS, SSM_HEAD_DIM).astype(jnp.float32)
    rep = SSM_HEADS // SSM_GROUPS
    bm = jnp.repeat(bm.reshape(B, S, SSM_GROUPS, SSM_STATE), rep, axis=2).astype(jnp.float32)
    cm = jnp.repeat(cm.reshape(B, S, SSM_GROUPS, SSM_STATE), rep, axis=2).astype(jnp.float32)
    dt = jax.nn.softplus((dt_raw + dt_bias).astype(jnp.float32))
    a = -jnp.exp(a_log.astype(jnp.float32))
    y = ssd_chunked(xs * dt[..., None], dt * a, bm, cm)
    y = y + xs * d_skip.astype(jnp.float32)[:, None]
    y = y.reshape(B, S, SSM_INNER) * jax.nn.silu(z.astype(jnp.float32))
    return rms_norm(y, norm_w).astype(z.dtype)


def token_mixer(x, cos_p, sin_p, cos_m, sin_m, w_in, b_gate, conv_w, conv_b, dt_bias, a_log,
                d_skip, ssm_norm_w, q_norm_w, w_uq, kv_norm_w, w_ukv, w_branch, w_out):
    B, S, _ = x.shape
    proj = jnp.einsum('bsd,de->bse', x, w_in)
    points = np.cumsum(SPLIT_SIZES)[:-1].tolist()
    (a_q, a_k, a_v, m_z, m_xbc, m_dt, c_q, c_k, c_v, c_qi, c_ki, c_wi,
     d_cq, d_ckv, d_kpe, g) = jnp.split(proj, points, axis=-1)

    def heads(t, h):
        return t.reshape(B, S, h, -1)

    o_a = moba_attention(partial_rope(heads(a_q, MOBA_HEADS), cos_p, sin_p),
                         partial_rope(heads(a_k, MOBA_HEADS), cos_p, sin_p),
                         heads(a_v, MOBA_HEADS))
    o_b = mamba2_ssd(m_z, m_xbc, m_dt, conv_w, conv_b, dt_bias, a_log, d_skip, ssm_norm_w)
    o_c = dsa_attention(partial_rope(heads(c_q, DSA_HEADS), cos_p, sin_p),
                        partial_rope(heads(c_k, DSA_HEADS), cos_p, sin_p),
                        heads(c_v, DSA_HEADS),
                        partial_rope(heads(c_qi, IDX_HEADS), cos_p, sin_p),
                        partial_rope(c_ki[:, :, None, :], cos_p, sin_p)[:, :, 0],
                        c_wi * IDX_SCALE)
    o_d = mla_attention(d_cq, d_ckv, d_kpe, cos_m, sin_m, q_norm_w, w_uq, kv_norm_w, w_ukv)

    branches = jnp.stack([o_a, o_b, o_c, o_d], axis=2)
    y = jnp.einsum('bsgw,gwd->bsgd', branches, w_branch)
    gates = jax.nn.sigmoid((g.reshape(B, S, N_BRANCH, D_MODEL) + b_gate).astype(jnp.float32)).astype(y.dtype)
    merged = jnp.sum(gates * y, axis=2)
    return jnp.einsum('bsd,de->bse', merged, w_out)


def moe_ffn(h, router_w, router_b, w_gate_up, b_gate_up, w_down, b_down):
    B, S, D = h.shape
    T = B * S
    TK = T * TOP_K
    xt = h.reshape(T, D)
    logits = (jnp.einsum('td,de->te', xt, router_w) + router_b).astype(jnp.float32)
    top_vals, top_idx = lax.top_k(logits, TOP_K)
    gate = jax.nn.softmax(top_vals, axis=-1)
    flat_e = top_idx.reshape(-1)
    counts = jnp.bincount(flat_e, length=N_EXPERTS)
    padded = ((counts + MOE_BLOCK - 1) // MOE_BLOCK) * MOE_BLOCK
    pad_end = jnp.cumsum(padded)
    pad_start = pad_end - padded
    start = jnp.cumsum(counts) - counts
    order = jnp.argsort(flat_e)
    sorted_e = flat_e[order]
    rank = jnp.arange(TK) - start[sorted_e]
    dest = pad_start[sorted_e] + rank
    tok = order // TOP_K
    gate_sorted = gate.reshape(-1)[order]
    n_blocks = -(-TK // MOE_BLOCK) + N_EXPERTS
    n_rows = n_blocks * MOE_BLOCK
    buf_x = jnp.zeros((n_rows, D), h.dtype).at[dest].set(xt[tok])
    block_start = jnp.arange(n_blocks) * MOE_BLOCK
    block_e = jnp.minimum(jnp.sum(pad_end[None, :] <= block_start[:, None], axis=1), N_EXPERTS - 1)

    def expert_block(args):
        x_blk, e = args
        gu = x_blk @ w_gate_up[e] + b_gate_up[e]
        glu = jnp.minimum(gu[:, :D_FF], SWIGLU_LIMIT)
        lin = jnp.clip(gu[:, D_FF:], -SWIGLU_LIMIT, SWIGLU_LIMIT)
        act = glu * jax.nn.sigmoid(SWIGLU_ALPHA * glu) * (lin + 1)
        return act @ w_down[e] + b_down[e]

    out = lax.map(expert_block, (buf_x.reshape(n_blocks, MOE_BLOCK, D), block_e)).reshape(n_rows, D)
    contrib = out[dest] * gate_sorted[:, None].astype(out.dtype)
    y = jax.ops.segment_sum(contrib, tok, num_segments=T)
    return y.reshape(B, S, D).astype(h.dtype)


def setup_inputs(seed: int = 0) -> dict:
    key = jax.random.key(seed)
    ks = jax.random.split(key, 32)
    f32 = jnp.float32
    L = DEPTH

    def nrm(k, shape, scale):
        return jax.random.normal(k, shape, f32) * scale

    x = jax.random.normal(ks[0], (BATCH, SEQ, D_MODEL), f32)
    positions = jnp.broadcast_to(jnp.arange(SEQ, dtype=jnp.int32), (BATCH, SEQ))
    w_in = nrm(ks[1], (L, D_MODEL, D_IN), D_MODEL ** -0.5)
    b_gate = nrm(ks[2], (L, N_BRANCH, D_MODEL), 0.02)
    conv_w = nrm(ks[3], (L, SSM_CONV, SSM_CONV_CH), SSM_CONV ** -0.5)
    conv_b = nrm(ks[4], (L, SSM_CONV_CH), 0.02)
    dt_init = jnp.exp(jax.random.uniform(ks[5], (L, SSM_HEADS), f32, math.log(1e-3), math.log(1e-1)))
    dt_bias = dt_init + jnp.log(-jnp.expm1(-dt_init))
    a_log = jnp.log(jax.random.uniform(ks[6], (L, SSM_HEADS), f32, 1.0, 16.0))
    d_skip = 1.0 + nrm(ks[7], (L, SSM_HEADS), 0.1)
    ssm_norm_w = 1.0 + nrm(ks[8], (L, SSM_INNER), 0.02)
    q_norm_w = 1.0 + nrm(ks[9], (L, MLA_Q_RANK), 0.02)
    w_uq = nrm(ks[10], (L, MLA_Q_RANK, MLA_HEADS * (MLA_NOPE + MLA_ROPE)), MLA_Q_RANK ** -0.5)
    kv_norm_w = 1.0 + nrm(ks[11], (L, MLA_KV_RANK), 0.02)
    w_ukv = nrm(ks[12], (L, MLA_KV_RANK, MLA_HEADS * (MLA_NOPE + MLA_V)), MLA_KV_RANK ** -0.5)
    w_branch = nrm(ks[13], (L, N_BRANCH, BRANCH_W, D_MODEL), BRANCH_W ** -0.5)
    w_out = nrm(ks[14], (L, D_MODEL, D_MODEL), D_MODEL ** -0.5 * DN_BETA)
    ln1_g = 1.0 + nrm(ks[15], (L, D_MODEL), 0.02)
    ln1_b = nrm(ks[16], (L, D_MODEL), 0.02)
    router_w = nrm(ks[17], (L, D_MODEL, N_EXPERTS), D_MODEL ** -0.5)
    router_b = nrm(ks[18], (L, N_EXPERTS), 0.01)
    w_gate_up = nrm(ks[19], (L, N_EXPERTS, D_MODEL, 2 * D_FF), D_MODEL ** -0.5)
    b_gate_up = nrm(ks[20], (L, N_EXPERTS, 2 * D_FF), 0.02)
    w_down = nrm(ks[21], (L, N_EXPERTS, D_FF, D_MODEL), D_FF ** -0.5 * DN_BETA)
    b_down = nrm(ks[22], (L, N_EXPERTS, D_MODEL), 0.02)
    ln2_g = 1.0 + nrm(ks[23], (L, D_MODEL), 0.02)
    ln2_b = nrm(ks[24], (L, D_MODEL), 0.02)
    return {'x': x, 'positions': positions, 'w_in': w_in, 'b_gate': b_gate,
            'conv_w': conv_w, 'conv_b': conv_b, 'dt_bias': dt_bias, 'a_log': a_log,
            'd_skip': d_skip, 'ssm_norm_w': ssm_norm_w, 'q_norm_w': q_norm_w, 'w_uq': w_uq,
            'kv_norm_w': kv_norm_w, 'w_ukv': w_ukv, 'w_branch': w_branch, 'w_out': w_out,
            'ln1_g': ln1_g, 'ln1_b': ln1_b, 'router_w': router_w, 'router_b': router_b,
            'w_gate_up': w_gate_up, 'b_gate_up': b_gate_up, 'w_down': w_down, 'b_down': b_down,
            'ln2_g': ln2_g, 'ln2_b': ln2_b}


def reference(x, positions, w_in, b_gate, conv_w, conv_b, dt_bias, a_log, d_skip, ssm_norm_w,
              q_norm_w, w_uq, kv_norm_w, w_ukv, w_branch, w_out, ln1_g, ln1_b, router_w, router_b,
              w_gate_up, b_gate_up, w_down, b_down, ln2_g, ln2_b):
    cos_p, sin_p = rope_cos_sin(positions, ROT_DIM, ROPE_THETA)
    cos_m, sin_m = rope_cos_sin(positions, MLA_ROPE, MLA_THETA)
    for l in range(DEPTH):
        mixed = token_mixer(x, cos_p, sin_p, cos_m, sin_m, w_in[l], b_gate[l], conv_w[l], conv_b[l],
                            dt_bias[l], a_log[l], d_skip[l], ssm_norm_w[l], q_norm_w[l], w_uq[l],
                            kv_norm_w[l], w_ukv[l], w_branch[l], w_out[l])
        x = layer_norm(DN_ALPHA * x + mixed, ln1_g[l], ln1_b[l])
        ffn = moe_ffn(x, router_w[l], router_b[l], w_gate_up[l], b_gate_up[l], w_down[l], b_down[l])
        x = layer_norm(DN_ALPHA * x + ffn, ln2_g[l], ln2_b[l])
    return x
```

```python
import contextlib
import math
import numpy as np
import concourse.bass as bass
import concourse.mybir as mybir
from concourse.bass_utils import run_bass_kernel_spmd

F32 = mybir.dt.float32
BF16 = mybir.dt.bfloat16
I32 = mybir.dt.int32
U32 = mybir.dt.uint32
ALU = mybir.AluOpType
AF = mybir.ActivationFunctionType
AX = mybir.AxisListType

D_MODEL = 2048
SEQ = 8192
DEPTH = 4
D_IN = 13804
BIG = 30000.0
LN_EPS = 1e-5
DN_ALPHA = (2 * DEPTH) ** 0.25
IDX_SCALE = 256 ** -0.5
N_EXPERTS = 32
D_FF = 768
O_AQ, O_AK, O_AV, O_Z, O_XBC, O_DT = 0, 512, 1024, 1536, 2048, 3072
O_CQ, O_CK, O_CV, O_QI, O_KI, O_WI = 3080, 3592, 4104, 4616, 4872, 4936
O_DCQ, O_DCKV, O_KPE, O_G = 4940, 5324, 5580, 5612


class Sched:
    CE = ('pe', 'act', 'dve', 'pool')
    NDMA = 24

    def __init__(self, nc, stack):
        self.nc = nc
        self.lists = {e: [] for e in ('pe', 'act', 'dve', 'pool', 'sp')}
        self.sem = {e: stack.enter_context(nc.semaphore("s_" + e)) for e in self.CE}
        self.cnt = {e: 0 for e in self.CE}
        self.dsem = [stack.enter_context(nc.semaphore("d%d" % i)) for i in range(self.NDMA)]
        self.dcnt = [0] * self.NDMA
        self.dnext = 0
        self.seen = {e: {} for e in self.lists}
        self.last_w = {}
        self.readers = {}
        self.ninst = 0

    @staticmethod
    def _norm(names):
        return [b.split('__L')[0] for b in names]

    def _deps(self, reads, writes):
        reads, writes = self._norm(reads), self._norm(writes)
        evs = []
        for b in reads:
            if b in self.last_w:
                evs.append(self.last_w[b])
        for b in writes:
            if b in self.last_w:
                evs.append(self.last_w[b])
            evs.extend(self.readers.get(b, ()))
        return evs

    def _waits(self, eng, evs):
        need = {}
        for (k, v) in evs:
            if eng == 'pe' and k == 'pe':
                continue
            if self.seen[eng].get(k, 0) >= v:
                continue
            need[k] = max(need.get(k, 0), v)
        for k, v in need.items():
            self.seen[eng][k] = v
        return list(need.items())

    def _commit(self, ev, reads, writes):
        reads, writes = self._norm(reads), self._norm(writes)
        for b in reads:
            self.readers.setdefault(b, []).append(ev)
        for b in writes:
            self.last_w[b] = ev
            self.readers[b] = []

    def op(self, eng, fn, reads=(), writes=()):
        waits = self._waits(eng, self._deps(reads, writes))
        self.cnt[eng] += 1
        ev = (eng, self.cnt[eng])
        self.lists[eng].append((waits, fn, (eng, 1)))
        self._commit(ev, reads, writes)
        return ev

    def dma(self, q, fn, reads=(), writes=()):
        k = self.dnext
        self.dnext = (self.dnext + 1) % self.NDMA
        evs = self._deps(reads, writes)
        dk = 'd%d' % k
        if self.dcnt[k] > 0:
            evs.append((dk, self.dcnt[k]))
        waits = self._waits(q, evs)
        self.dcnt[k] += 16
        ev = (dk, self.dcnt[k])
        self.lists[q].append((waits, fn, (dk, 16)))
        self._commit(ev, reads, writes)
        return ev

    def _semh(self, k):
        return self.sem[k] if k in self.sem else self.dsem[int(k[1:])]

    def _replay(self, name, e):
        for waits, fn, inc in self.lists[name]:
            for k, v in waits:
                e.wait_ge(self._semh(k), v)
            if fn is not None:
                fn(e).then_inc(self._semh(inc[0]), inc[1])
                self.ninst += 1

    def flush(self):
        evs = [(k, self.cnt[k]) for k in self.CE if self.cnt[k] > 0]
        evs += [('d%d' % i, self.dcnt[i]) for i in range(self.NDMA) if self.dcnt[i] > 0]
        for e in self.lists:
            self.lists[e].append((self._waits(e, evs), None, None))
        with self.nc.Block() as block:
            @block.tensor
            def _(e):
                self._replay('pe', e)

            @block.scalar
            def _(e):
                self._replay('act', e)

            @block.vector
            def _(e):
                self._replay('dve', e)

            @block.gpsimd
            def _(e):
                self._replay('pool', e)

            @block.sync
            def _(e):
                self._replay('sp', e)
        for e in self.lists:
            self.lists[e] = []
        self.last_w = {}
        self.readers = {}


class _LazyInputs(dict):
    def __init__(self, nc, shapes):
        super().__init__()
        self.nc, self.shapes = nc, shapes

    def __missing__(self, name):
        shape, dt = self.shapes[name]
        ap = self.nc.dram_tensor(name, shape, dt, kind="ExternalInput").ap()
        self[name] = ap
        return ap


class KB:
    def __init__(self, T, L, dbg=()):
        self.T = T
        self.L = L
        self.NT = T // 128
        self.NB = T // 256
        self.dbg = dbg
        self.nc = bass.Bass("TRN2", target_bir_lowering=False)
        self.cur_layer = 'c'
        self.sparse = True

    def mm(self, out, lhsT, rhs, start, stop, r, w):
        self.S.op('pe', lambda e: e.matmul(out, lhsT, rhs, start=start, stop=stop), reads=r, writes=w)

    def tr(self, out, in_, ident, r, w):
        self.S.op('pe', lambda e: e.transpose(out, in_, ident), reads=r, writes=w)

    def act(self, out, in_, func, r, w, bias=None, scale=None, accum=None):
        kw = {}
        if bias is not None:
            kw['bias'] = bias
        if scale is not None:
            kw['scale'] = scale
        if accum is not None:
            kw['accum_out'] = accum
        self.S.op('act', lambda e: e.activation(out=out, in_=in_, func=func, **kw), reads=r, writes=w)

    def tt(self, out, in0, in1, op, r, w, eng='dve'):
        self.S.op(eng, lambda e: e.tensor_tensor(out=out, in0=in0, in1=in1, op=op), reads=r, writes=w)

    def ts(self, out, in0, s1, op0, r, w, s2=None, op1=None, eng='dve', accum=None):
        kw = {}
        if op1 is not None:
            kw['op1'] = op1
        if accum is not None:
            kw['accum_out'] = accum
        self.S.op(eng, lambda e: e.tensor_scalar(out=out, in0=in0, scalar1=s1, scalar2=s2, op0=op0, **kw),
                  reads=r, writes=w)

    def stt(self, out, in0, scalar, in1, op0, op1, r, w, eng='dve'):
        self.S.op(eng, lambda e: e.scalar_tensor_tensor(out=out, in0=in0, scalar=scalar, in1=in1, op0=op0, op1=op1),
                  reads=r, writes=w)

    def cp(self, out, in_, r, w, eng='dve'):
        if eng == 'act':
            self.S.op('act', lambda e: e.activation(out=out, in_=in_, func=AF.Copy), reads=r, writes=w)
        else:
            self.S.op(eng, lambda e: e.tensor_copy(out=out, in_=in_), reads=r, writes=w)

    def memset(self, ap, val, w, eng='pool'):
        self.S.op(eng, lambda e: e.memset(ap, val), writes=w)

    def dma(self, out, in_, r, w, q='sp', slow=False):
        if slow:
            self.S.dma(q, lambda e: e.dma_start(out=out, in_=in_, allow_slow_non_contiguous=True), reads=r, writes=w)
        else:
            self.S.dma(q, lambda e: e.dma_start(out=out, in_=in_), reads=r, writes=w)

    def sb(self, st, name, shape, dt):
        return st.enter_context(self.nc.sbuf_tensor("%s__L%s" % (name, self.cur_layer), shape, dt))

    def dram(self, name, shape, dt):
        return self.nc.dram_tensor(name, shape, dt).ap()

    def build(self, n_phases=99):
        nc, T, L, NT = self.nc, self.T, self.L, self.NT
        self.shapes = {
            'x': ([T, D_MODEL], F32), 'positions': ([1, T], I32), 'w_in': ([L, D_MODEL, D_IN], F32),
            'b_gate': ([L, 4, D_MODEL], F32), 'conv_w': ([L, 4, 1024], F32), 'conv_b': ([L, 1024], F32),
            'dt_bias': ([L, 8], F32), 'a_log': ([L, 8], F32), 'd_skip': ([L, 8], F32),
            'ssm_norm_w': ([L, 512], F32), 'q_norm_w': ([L, 384], F32), 'w_uq': ([L, 384, 768], F32),
            'kv_norm_w': ([L, 256], F32), 'w_ukv': ([L, 256, 1024], F32),
            'w_branch': ([L, 4, 512, D_MODEL], F32), 'w_out': ([L, D_MODEL, D_MODEL], F32),
            'ln1_g': ([L, D_MODEL], F32), 'ln1_b': ([L, D_MODEL], F32),
            'router_w': ([L, D_MODEL, N_EXPERTS], F32), 'router_b': ([L, N_EXPERTS], F32),
            'w_gate_up': ([L, N_EXPERTS, D_MODEL, 2 * D_FF], F32), 'b_gate_up': ([L, N_EXPERTS, 2 * D_FF], F32),
            'w_down': ([L, N_EXPERTS, D_FF, D_MODEL], F32), 'b_down': ([L, N_EXPERTS, D_MODEL], F32),
            'ln2_g': ([L, D_MODEL], F32), 'ln2_b': ([L, D_MODEL], F32)}
        I = _LazyInputs(nc, self.shapes)
        self.I = I
        self.out = nc.dram_tensor("out", [T, D_MODEL], F32, kind="ExternalOutput").ap()

        Dm = {}
        Dm['xT'] = self.dram('xT', [D_MODEL, T], BF16)
        Dm['xres'] = self.dram('xres', [T, D_MODEL], F32)
        Dm['proj'] = self.dram('proj', [T, 5632], F32)
        Dm['gates'] = self.dram('gates', [T, 4 * D_MODEL], BF16)
        Dm['featT'] = self.dram('featT', [1536, T], F32)
        Dm['xbcA'] = self.dram('xbcA', [1024, T], F32)
        for nm in ('A', 'C'):
            Dm['qT' + nm] = self.dram('qT' + nm, [8, 96 if nm == 'A' else 64, T], BF16)
            Dm['kT' + nm] = self.dram('kT' + nm, [8, 96 if nm == 'A' else 64, T], BF16)
            Dm['v' + nm] = self.dram('v' + nm, [T, 8, 64], BF16)
        Dm['qTD'] = self.dram('qTD', [8, 96, T], BF16)
        Dm['kTD'] = self.dram('kTD', [8, 96, T], BF16)
        Dm['vD'] = self.dram('vD', [T, 8, 64], BF16)
        Dm['qiT'] = self.dram('qiT', [4, 64, T], BF16)
        Dm['kiT'] = self.dram('kiT', [64, T], BF16)
        Dm['wtm'] = self.dram('wtm', [T, 4], F32)
        Dm['wT'] = self.dram('wT', [4, T], F32)
        Dm['cqT'] = self.dram('cqT', [384, T], BF16)
        Dm['ckvT'] = self.dram('ckvT', [256, T], BF16)
        Dm['kpe'] = self.dram('kpe', [T, 32], BF16)
        Dm['thr'] = self.dram('thr', [self.NT, 128], F32)
        Dm['maskT'] = self.dram('maskT', [T, T], BF16)
        Dm['oT'] = self.dram('oT', [4 * 512, T], BF16)
        Dm['x1'] = self.dram('x1', [T, D_MODEL], F32)
        Dm['x1T'] = self.dram('x1T', [D_MODEL, T], BF16)
        Dm['gsel'] = self.dram('gsel', [T, N_EXPERTS], F32)
        Dm['mergedT'] = self.dram('mergedT', [D_MODEL, T], BF16)
        Dm['mixed'] = self.dram('mixed', [T, D_MODEL], F32)
        Dm['yacc'] = self.dram('yacc', [T, D_MODEL], F32)
        self.CAP = max(256, ((T * 4 // N_EXPERTS) * 3 // 2 + 127) // 128 * 128)
        Dm['xg'] = self.dram('xg', [N_EXPERTS * self.CAP, D_MODEL], BF16)
        Dm['og'] = self.dram('og', [N_EXPERTS * self.CAP, D_MODEL], BF16)
        Dm['gsel4'] = self.dram('gsel4', [T, 4], F32)
        Dm['dest4'] = self.dram('dest4', [T, 4], I32)
        self.Dm = Dm

        with contextlib.ExitStack() as gst:
            self.S = Sched(nc, gst)
            self.ps = [gst.enter_context(nc.psum_tensor("ps%d" % i, [128, 512], F32)) for i in range(6)]
            self.pb = [gst.enter_context(nc.psum_tensor("pb%d" % i, [128, 1024], BF16)) for i in range(2)]
            self.phase_consts(gst)
            self.S.flush()
            ph = 1
            for l in range(L):
                self.cur_layer = str(l)
                src = I['x'] if l == 0 else Dm['xres']
                if l == 0:
                    self.phase_xT(src)
                    self.phase_moba_onehot()
                    if self.sparse:
                        self.phase_moe_zero()
                if ph >= n_phases:
                    break
                steps = [lambda: self.phase_inproj(l), lambda: self.phase_prep(l), lambda: self.phase_mla_up(l),
                         lambda: self.phase_attn(Dm['qTD'], Dm['kTD'], Dm['vD'], 96, 96 ** -0.5, 3 * 512, tag="atD"),
                         lambda: self.phase_moba_gates(),
                         lambda: self.phase_attn(Dm['qTA'], Dm['kTA'], Dm['vA'], 64 + self.NB, 0.125, 0, tag="atA"),
                         lambda: self.phase_ssm(l),
                         lambda: self.phase_dsa_thr(),
                         lambda: self.phase_dsa_mask(),
                         lambda: self.phase_attn(Dm['qTC'], Dm['kTC'], Dm['vC'], 64, 0.125, 2 * 512, use_mask=True, tag="atC"),
                         lambda: self.phase_merge(l),
                         lambda: self.phase_wout(l),
                         lambda: self.phase_ln1_router(l, src),
                         lambda: (self.phase_moe_sparse(l) if self.sparse else self.phase_moe_dense(l)),
                         lambda: self.phase_ln2(l, l == L - 1)]
                stop = False
                for stp in steps:
                    stp()
                    ph += 1
                    if ph >= n_phases:
                        stop = True
                        break
                if stop:
                    break
            self.finish()
        return nc

    def phase_consts(self, gst):
        nc, T, NT = self.nc, self.T, self.NT
        sb = self.sb
        self.ident_f = sb(gst, "ident_f", [128, 128], F32)
        self.ident_b = sb(gst, "ident_b", [128, 128], BF16)
        self.ones_b = sb(gst, "ones_b", [128, 128], BF16)
        self.ones_f = sb(gst, "ones_f", [128, 128], F32)
        self.cm = [sb(gst, "cm%d" % j, [128, 512], BF16) for j in range(4)]
        self.pos_tm = sb(gst, "pos_tm", [128, NT], F32)
        self.idx_tm = sb(gst, "idx_tm", [128, NT], F32)
        self.cosP = sb(gst, "cosP", [128, NT, 8], F32)
        self.sinP = sb(gst, "sinP", [128, NT, 8], F32)
        self.cosM = sb(gst, "cosM", [128, NT, 16], F32)
        self.sinM = sb(gst, "sinM", [128, NT, 16], F32)
        self.tri_f = sb(gst, "tri_f", [128, 128], F32)
        self.cmneg = sb(gst, "cmneg", [128, 128], F32)
        self.tris_b = sb(gst, "tris_b", [128, 128], BF16)
        self.erow = sb(gst, "erow", [128, N_EXPERTS], F32)
        self.negpi = sb(gst, "negpi", [128, 1], F32)
        self.memset(self.negpi[:], -math.pi, ['negpi'])
        with contextlib.ExitStack() as st:
            io = sb(st, "c_io", [128, 512], F32)
            posi = sb(st, "c_posi", [128, NT], I32)
            ang = sb(st, "c_ang", [128, NT, 16], F32)
            rr = sb(st, "c_rr", [128, NT, 16], F32)
            uu = sb(st, "c_uu", [128, NT, 16], F32)
            ki = sb(st, "c_ki", [128, NT, 16], I32)
            self.S.op('pool', lambda e: e.iota(io[:, 0:128], pattern=[[1, 128]], base=0, channel_multiplier=-1,
                                               allow_small_or_imprecise_dtypes=True), writes=['c_io'])
            self.ts(self.ident_f[:], io[:, 0:128], 0.0, ALU.is_equal, ['c_io'], ['ident_f'])
            self.cp(self.ident_b[:], self.ident_f[:], ['ident_f'], ['ident_b'])
            self.ts(self.tri_f[:], io[:, 0:128], 0.0, ALU.is_ge, ['c_io'], ['tri_f'])
            self.ts(self.cmneg[:], self.tri_f[:], 1.0, ALU.subtract, ['tri_f'], ['cmneg'], s2=BIG, op1=ALU.mult)
            self.memset(self.ones_b[:], 1.0, ['ones_b'])
            self.S.op('pool', lambda e: e.iota(io[:, 0:128], pattern=[[1, 128]], base=-1, channel_multiplier=-1,
                                               allow_small_or_imprecise_dtypes=True), reads=['ident_f', 'tri_f'], writes=['c_io'])
            self.ts(self.tris_b[:], io[:, 0:128], 0.0, ALU.is_ge, ['c_io'], ['tris_b'])
            self.S.op('pool', lambda e: e.iota(self.erow[:], pattern=[[1, N_EXPERTS]], base=0, channel_multiplier=0,
                                               allow_small_or_imprecise_dtypes=True), writes=['erow'])
            self.memset(self.ones_f[:], 1.0, ['ones_f'])
            for j in range(4):
                self.S.op('pool', lambda e, j=j: e.iota(io[:], pattern=[[1, 512]], base=-128 * j, channel_multiplier=-1,
                                                        allow_small_or_imprecise_dtypes=True),
                          reads=[], writes=['c_io'])
                self.ts(self.cm[j][:], io[:], 0.0, ALU.is_ge, ['c_io'], ['cm%d' % j])
            self.S.op('pool', lambda e: e.iota(self.idx_tm[:], pattern=[[128, NT]], base=0, channel_multiplier=1,
                                               allow_small_or_imprecise_dtypes=True), writes=['idx_tm'])
            self.dma(posi[:], self.I['positions'][0, :].rearrange("(n p) -> p n", p=128), [], ['c_posi'], slow=True)
            self.cp(self.pos_tm[:], posi[:], ['c_posi'], ['pos_tm'])
            for (cs, sn, nf, dim, theta) in ((self.cosP, self.sinP, 8, 16, 500000.0), (self.cosM, self.sinM, 16, 32, 10000.0)):
                for i in range(nf):
                    inv = float(np.float32(theta) ** (-np.float32(2 * i) / np.float32(dim)))
                    self.ts(ang[:, :, i], self.pos_tm[:], inv, ALU.mult, ['pos_tm'], ['c_ang'])
                tmp = ang[:, :, 0:nf]
                for (dst, shift) in ((sn, 0.0), (cs, 0.5 * math.pi)):
                    self.ts(rr[:, :, 0:nf], tmp, shift, ALU.add, ['c_ang'], ['c_rr'])
                    self.ts(uu[:, :, 0:nf], rr[:, :, 0:nf], 1.0 / (2 * math.pi), ALU.mult, ['c_rr'], ['c_uu'])
                    self.cp(ki[:, :, 0:nf], uu[:, :, 0:nf], ['c_uu'], ['c_ki'])
                    self.cp(uu[:, :, 0:nf], ki[:, :, 0:nf], ['c_ki'], ['c_uu'])
                    self.stt(rr[:, :, 0:nf], uu[:, :, 0:nf], -2 * math.pi, rr[:, :, 0:nf], ALU.mult, ALU.add,
                             ['c_uu', 'c_rr'], ['c_rr'])
                    self.ts(uu[:, :, 0:nf], rr[:, :, 0:nf], math.pi, ALU.is_gt, ['c_rr'], ['c_uu'])
                    self.stt(rr[:, :, 0:nf], uu[:, :, 0:nf], -2 * math.pi, rr[:, :, 0:nf], ALU.mult, ALU.add,
                             ['c_uu', 'c_rr'], ['c_rr'])
                    self.act(dst[:], rr[:, :, 0:nf], AF.Sin, ['c_rr'], [dst.name])
            self.S.flush()

    def negpi_ap(self, gst):
        return self.negpi[:, 0:1]

    def emit_xT(self, src_sb, src_name, n, dstT, xts, k):
        for g in range(4):
            ps = self.ps[(k * 4 + g) % 4]
            pn = ps.name
            for j in range(4):
                c = g * 4 + j
                self.tr(ps[:, j * 128:(j + 1) * 128], src_sb[:, c * 128:(c + 1) * 128], self.ident_f[:],
                        [src_name, 'ident_f'], [pn])
            self.cp(xts[:, g * 4:(g + 1) * 4, :], ps[:].rearrange("p (a b) -> p a b", a=4), [pn], [xts.name],
                    eng='act' if g % 2 else 'dve')
        self.dma(dstT.rearrange("(kc p) t -> p kc t", p=128)[:, :, n * 128:(n + 1) * 128], xts[:], [xts.name],
                 [], q='pool')

    def phase_xT(self, src):
        with contextlib.ExitStack() as st:
            xin = [self.sb(st, "xin%d" % i, [128, D_MODEL], F32) for i in range(2)]
            xts = [self.sb(st, "xts%d" % i, [128, 16, 128], BF16) for i in range(2)]
            for n in range(self.NT):
                b = n % 2
                self.dma(xin[b][:], src[n * 128:(n + 1) * 128, :], [], [xin[b].name])
                self.emit_xT(xin[b], xin[b].name, n, self.Dm['xT'], xts[b], n)
            self.S.flush()

    def linear_stream(self, st, aT, K, w, N, epilogue, tag):
        KC = K // 128
        wb = [self.sb(st, "%s_w%d" % (tag, i), [128, KC, 512], BF16) for i in range(2)]
        ab = [self.sb(st, "%s_a%d" % (tag, i), [128, KC, 512], BF16) for i in range(2)]
        aTv = aT.rearrange("(kc p) t -> p kc t", p=128)
        wv = w.rearrange("(kc p) n -> p kc n", p=128)
        it = 0
        pi = 0
        for ci, c0 in enumerate(range(0, N, 512)):
            cw = min(512, N - c0)
            wt = wb[ci % 2]
            self.dma(wt[:, :, 0:cw], wv[:, :, c0:c0 + cw], [], [wt.name], q='pool')
            for tb in range(self.T // 512):
                at = ab[it % 2]
                it += 1
                self.dma(at[:], aTv[:, :, tb * 512:(tb + 1) * 512], [], [at.name])
                for j in range(4):
                    ps = self.ps[pi % 4]
                    pi += 1
                    for kc in range(KC):
                        self.mm(ps[:, 0:cw], at[:, kc, j * 128:(j + 1) * 128], wt[:, kc, 0:cw], kc == 0, kc == KC - 1,
                                [at.name, wt.name], [ps.name])
                    epilogue(tb * 4 + j, c0, cw, ps, ps.name)

    def phase_inproj(self, l):
        T = self.T
        w_in = self.I['w_in'][l]
        proj = self.Dm['proj']
        with contextlib.ExitStack() as st:
            ev = [self.sb(st, "ip_ev%d" % i, [128, 512], F32) for i in range(4)]
            cnt = [0]

            def ep_plain(base):
                def ep(n, c0, cw, ps, pn):
                    b = ev[cnt[0] % 4]
                    self.cp(b[:, 0:cw], ps[:, 0:cw], [pn], [b.name], eng='act' if cnt[0] % 2 else 'dve')
                    cnt[0] += 1
                    self.dma(proj[n * 128:(n + 1) * 128, base + c0:base + c0 + cw], b[:, 0:cw], [b.name], [], q='pool')
                return ep
            self.linear_stream(st, self.Dm['xT'], D_MODEL, w_in[:, 0:1536], 1536, ep_plain(0), "ipa")
            self.linear_stream(st, self.Dm['xT'], D_MODEL, w_in[:, 3072:5612], 5612 - 3072, ep_plain(3072), "ipb")
            self.S.flush()
        with contextlib.ExitStack() as st:
            bg = self.sb(st, "ip_bg", [128, 4 * D_MODEL], F32)
            gf = [self.sb(st, "ip_gf%d" % i, [128, 512], F32) for i in range(2)]
            gb = [self.sb(st, "ip_gb%d" % i, [128, 512], BF16) for i in range(2)]
            self.dma(bg[:], self.I['b_gate'][l].rearrange("g d -> (g d)").rearrange("(o n) -> o n", o=1).partition_broadcast(128),
                     [], ['ip_bg'])
            cnt = [0]
            gates = self.Dm['gates']

            def ep_gate(n, c0, cw, ps, pn):
                i = cnt[0] % 2
                cnt[0] += 1
                self.tt(gf[i][:, 0:cw], ps[:, 0:cw], bg[:, c0:c0 + cw], ALU.add, [pn, 'ip_bg'], [gf[i].name])
                self.act(gb[i][:, 0:cw], gf[i][:, 0:cw], AF.Sigmoid, [gf[i].name], [gb[i].name])
                self.dma(gates[n * 128:(n + 1) * 128, c0:c0 + cw], gb[i][:, 0:cw], [gb[i].name], [], q='pool')
            self.linear_stream(st, self.Dm['xT'], D_MODEL, w_in[:, O_G:O_G + 4 * D_MODEL], 4 * D_MODEL, ep_gate, "ipg")
            self.S.flush()
        with contextlib.ExitStack() as st:
            NCH = 12
            wf = self.sb(st, "ipf_w", [128, 16, NCH * 128], BF16)
            ab = [self.sb(st, "ipf_a%d" % i, [128, 16, 512], BF16) for i in range(2)]
            ev = [self.sb(st, "ipf_ev%d" % i, [128, 512], F32) for i in range(4)]
            self.dma(wf[:], w_in[:, O_Z:O_Z + NCH * 128].rearrange("(kc p) n -> p kc n", p=128), [], ['ipf_w'], q='pool')
            aTv = self.Dm['xT'].rearrange("(kc p) t -> p kc t", p=128)
            k = 0
            for tb in range(T // 512):
                at = ab[tb % 2]
                self.dma(at[:], aTv[:, :, tb * 512:(tb + 1) * 512], [], [at.name])
                for c in range(NCH):
                    ps = self.ps[k % 4]
                    for kc in range(16):
                        self.mm(ps[:], wf[:, kc, c * 128:(c + 1) * 128], at[:, kc, :], kc == 0, kc == 15,
                                [at.name, 'ipf_w'], [ps.name])
                    b = ev[k % 4]
                    self.cp(b[:], ps[:], [ps.name], [b.name], eng='act' if k % 2 else 'dve')
                    k += 1
                    self.dma(self.Dm['featT'][c * 128:(c + 1) * 128, tb * 512:(tb + 1) * 512], b[:], [b.name], [], q='pool')
            self.S.flush()

    def rope(self, src, dst, H, o1, half, cs, sn, n, tmp, rd):
        x1 = src[:, :, o1:o1 + half]
        x2 = src[:, :, o1 + half:o1 + 2 * half]
        c = cs[:, n, :].unsqueeze(1).broadcast_to([128, H, half])
        sgn = sn[:, n, :].unsqueeze(1).broadcast_to([128, H, half])
        ta, tb_, tc, td = [t[:, 0:H, 0:half] for t in tmp]
        nm = [t.name for t in tmp]
        self.tt(ta, x1, c, ALU.mult, rd, [nm[0]])
        self.tt(tb_, x2, sgn, ALU.mult, rd, [nm[1]])
        self.tt(tc, x2, c, ALU.mult, rd, [nm[2]])
        self.tt(td, x1, sgn, ALU.mult, rd, [nm[3]])
        return ((dst[:, :, o1:o1 + half], ta, tb_, ALU.subtract, [nm[0], nm[1]]),
                (dst[:, :, o1 + half:o1 + 2 * half], tc, td, ALU.add, [nm[2], nm[3]]))

    def phase_prep(self, l):
        T, NT, Dm = self.T, self.NT, self.Dm
        proj = Dm['proj']
        with contextlib.ExitStack() as st:
            pa = [self.sb(st, "pp_a%d" % i, [128, 1536], F32) for i in range(2)]
            pc = [self.sb(st, "pp_c%d" % i, [128, 1860], F32) for i in range(2)]
            pd = [self.sb(st, "pp_d%d" % i, [128, 672], F32) for i in range(2)]
            ab = [self.sb(st, "pp_ab%d" % i, [128, 3, 8, 64], BF16) for i in range(2)]
            cb = [self.sb(st, "pp_cb%d" % i, [128, 1856], BF16) for i in range(2)]
            tmp = [self.sb(st, "pp_t%d" % i, [128, 8, 16], F32) for i in range(4)]
            tsb = [self.sb(st, "pp_ts%d" % i, [128, 1024], BF16) for i in range(2)]
            wsb = self.sb(st, "pp_w", [128, 4], F32)
            wts = self.sb(st, "pp_wt", [4, 128], F32)
            ssq = self.sb(st, "pp_ss", [128, 2], F32)
            junk = self.sb(st, "pp_junk", [128, 384], F32)
            qnw = self.sb(st, "pp_qnw", [128, 384], F32)
            kvnw = self.sb(st, "pp_kvnw", [128, 256], F32)
            cn = [self.sb(st, "pp_cn%d" % i, [128, 640], BF16) for i in range(2)]
            kpb = [self.sb(st, "pp_kp%d" % i, [128, 1, 32], BF16) for i in range(2)]
            self.dma(qnw[:], self.I['q_norm_w'][l].rearrange("(o n) -> o n", o=1).partition_broadcast(128), [], ['pp_qnw'])
            self.dma(kvnw[:], self.I['kv_norm_w'][l].rearrange("(o n) -> o n", o=1).partition_broadcast(128), [], ['pp_kvnw'])
            tcount = [0]

            def transposes(src_fn, H, Dk, dst_dram, tag):
                i = tcount[0] % 2
                tcount[0] += 1
                pbk = self.pb[i]
                for h in range(H):
                    self.tr(pbk[0:Dk, h * 128:(h + 1) * 128], src_fn(h), self.ident_b[:], [tag, 'ident_b'], [pbk.name])
                self.cp(tsb[i][0:Dk, 0:H * 128], pbk[0:Dk, 0:H * 128], [pbk.name], [tsb[i].name],
                        eng='act' if tcount[0] % 2 else 'dve')
                return tsb[i]

            for n in range(NT):
                b = n % 2
                rows = slice(n * 128, (n + 1) * 128)
                cols = slice(n * 128, (n + 1) * 128)
                self.dma(pa[b][:], proj[rows, 0:1536], [], [pa[b].name])
                self.dma(pc[b][:], proj[rows, O_CQ:O_CQ + 1860], [], [pc[b].name])
                self.dma(pd[b][:], proj[rows, O_DCQ:O_DCQ + 672], [], [pd[b].name])
                self.cp(ab[b][:].rearrange("p a h d -> p (a h d)"), pa[b][:], [pa[b].name], [ab[b].name], eng='act')
                for qi_ in range(2):
                    src = pa[b][:, qi_ * 512:(qi_ + 1) * 512].rearrange("p (h d) -> p h d", h=8)
                    for (o, x, y, op, rn) in self.rope(src, ab[b][:, qi_], 8, 0, 8, self.cosP, self.sinP, n, tmp,
                                                       [pa[b].name, 'cosP', 'sinP']):
                        self.tt(o, x, y, op, rn, [ab[b].name])
                for qi_, nm in ((0, 'qTA'), (1, 'kTA')):
                    t_ = transposes(lambda h: ab[b][:, qi_, h, :], 8, 64, None, ab[b].name)
                    self.dma(Dm[nm][:, 0:64, cols].rearrange("h d t -> d h t"),
                             t_[0:64, :].rearrange("d (h t) -> d h t", h=8), [t_.name], [], q='pool')
                self.dma(Dm['vA'][rows], ab[b][:, 2], [ab[b].name], [], q='pool')
                self.cp(cb[b][:], pc[b][:, 0:1856], [pc[b].name], [cb[b].name], eng='act')
                for (o0, H) in ((0, 8), (512, 8), (1536, 4), (1792, 1)):
                    src = pc[b][:, o0:o0 + H * 64].rearrange("p (h d) -> p h d", h=H)
                    dst = cb[b][:, o0:o0 + H * 64].rearrange("p (h d) -> p h d", h=H)
                    for (o, x, y, op, rn) in self.rope(src, dst, H, 0, 8, self.cosP, self.sinP, n, tmp,
                                                       [pc[b].name, 'cosP', 'sinP']):
                        self.tt(o, x, y, op, rn, [cb[b].name])
                for (o0, H, nm) in ((0, 8, 'qTC'), (512, 8, 'kTC'), (1536, 4, 'qiT')):
                    t_ = transposes(lambda h: cb[b][:, o0 + h * 64:o0 + (h + 1) * 64], H, 64, None, cb[b].name)
                    self.dma(Dm[nm][:, 0:64, cols].rearrange("h d t -> d h t"),
                             t_[0:64, 0:H * 128].rearrange("d (h t) -> d h t", h=H), [t_.name], [], q='pool')
                t_ = transposes(lambda h: cb[b][:, 1792:1856], 1, 64, None, cb[b].name)
                self.dma(Dm['kiT'][:, cols], t_[0:64, 0:128], [t_.name], [], q='pool')
                self.dma(Dm['vC'][rows], cb[b][:, 1024:1536].rearrange("p (h d) -> p h d", h=8), [cb[b].name], [], q='pool')
                self.ts(wsb[:], pc[b][:, 1856:1860], IDX_SCALE, ALU.mult, [pc[b].name], ['pp_w'])
                self.dma(Dm['wtm'][rows, :], wsb[:], ['pp_w'], [], q='pool')
                pw = self.ps[4]
                self.tr(pw[0:4, 0:128], wsb[:], self.ident_f[:], ['pp_w', 'ident_f'], [pw.name])
                self.cp(wts[:], pw[0:4, 0:128], [pw.name], ['pp_wt'])
                self.dma(Dm['wT'][:, cols], wts[:], ['pp_wt'], [], q='pool')
                for j, (o0, R, nw) in enumerate(((0, 384, qnw), (384, 256, kvnw))):
                    self.act(junk[:, 0:R], pd[b][:, o0:o0 + R], AF.Square, [pd[b].name], ['pp_junk', 'pp_ss%d' % j],
                             accum=ssq[:, j:j + 1])
                    self.ts(ssq[:, j:j + 1], ssq[:, j:j + 1], 1.0 / R, ALU.mult, ['pp_ss%d' % j], ['pp_ss%d' % j],
                            s2=LN_EPS, op1=ALU.add)
                    self.act(ssq[:, j:j + 1], ssq[:, j:j + 1], AF.Sqrt, ['pp_ss%d' % j], ['pp_ss%d' % j])
                    self.S.op('dve', lambda e, j=j: e.reciprocal(out=ssq[:, j:j + 1], in_=ssq[:, j:j + 1]),
                              reads=['pp_ss%d' % j], writes=['pp_ss%d' % j])
                    self.stt(cn[b][:, o0:o0 + R], pd[b][:, o0:o0 + R], ssq[:, j:j + 1], nw[:], ALU.mult, ALU.mult,
                             [pd[b].name, 'pp_ss%d' % j, nw.name], [cn[b].name])
                t_ = transposes(lambda h: cn[b][:, h * 128:(h + 1) * 128], 5, 128, None, cn[b].name)
                self.dma(Dm['cqT'].rearrange("(c p) t -> p c t", p=128)[:, :, cols],
                         t_[:, 0:384].rearrange("p (c t) -> p c t", c=3), [t_.name], [], q='pool')
                self.dma(Dm['ckvT'].rearrange("(c p) t -> p c t", p=128)[:, :, cols],
                         t_[:, 384:640].rearrange("p (c t) -> p c t", c=2), [t_.name], [], q='pool')
                src = pd[b][:, 640:672].rearrange("p (h d) -> p h d", h=1)
                for (o, x, y, op, rn) in self.rope(src, kpb[b][:], 1, 0, 16, self.cosM, self.sinM, n, tmp,
                                                   [pd[b].name, 'cosM', 'sinM']):
                    self.tt(o, x, y, op, rn, [kpb[b].name])
                self.dma(Dm['kpe'][rows, :], kpb[b][:, 0, :], [kpb[b].name], [], q='pool')
            self.S.flush()

    def phase_mla_up(self, l):
        T, NT, Dm = self.T, self.NT, self.Dm
        with contextlib.ExitStack() as st:
            wuq = self.sb(st, "mu_wq", [128, 3, 768], BF16)
            wukv = self.sb(st, "mu_wkv", [128, 2, 1024], BF16)
            self.dma(wuq[:], self.I['w_uq'][l].rearrange("(c p) n -> p c n", p=128), [], ['mu_wq'], q='pool')
            self.dma(wukv[:], self.I['w_ukv'][l].rearrange("(c p) n -> p c n", p=128), [], ['mu_wkv'], q='pool')
            cq = [self.sb(st, "mu_cq%d" % i, [128, 3, 128], BF16) for i in range(2)]
            ckv = [self.sb(st, "mu_ckv%d" % i, [128, 2, 128], BF16) for i in range(2)]
            kp = [self.sb(st, "mu_kp%d" % i, [128, 1, 32], BF16) for i in range(2)]
            qf = [self.sb(st, "mu_qf%d" % i, [128, 8, 96], F32) for i in range(2)]
            qb = [self.sb(st, "mu_qb%d" % i, [128, 8, 96], BF16) for i in range(2)]
            kvf = [self.sb(st, "mu_kvf%d" % i, [128, 8, 128], F32) for i in range(2)]
            kb_ = [self.sb(st, "mu_kb%d" % i, [128, 8, 96], BF16) for i in range(2)]
            vb = [self.sb(st, "mu_vb%d" % i, [128, 8, 64], BF16) for i in range(2)]
            tmp = [self.sb(st, "mu_t%d" % i, [128, 8, 16], F32) for i in range(4)]
            tsb = [self.sb(st, "mu_ts%d" % i, [128, 1024], BF16) for i in range(2)]
            cqv = Dm['cqT'].rearrange("(c p) t -> p c t", p=128)
            ckvv = Dm['ckvT'].rearrange("(c p) t -> p c t", p=128)
            for n in range(NT):
                b = n % 2
                rows = slice(n * 128, (n + 1) * 128)
                self.dma(cq[b][:], cqv[:, :, rows], [], [cq[b].name])
                self.dma(ckv[b][:], ckvv[:, :, rows], [], [ckv[b].name])
                self.dma(kp[b][:, 0, :], Dm['kpe'][rows, :], [], [kp[b].name])
                qfl = qf[b][:].rearrange("p h d -> p (h d)")
                for ci, (c0, cw) in enumerate(((0, 512), (512, 256))):
                    ps = self.ps[ci]
                    for kc in range(3):
                        self.mm(ps[:, 0:cw], cq[b][:, kc, :], wuq[:, kc, c0:c0 + cw], kc == 0, kc == 2,
                                [cq[b].name, 'mu_wq'], [ps.name])
                    self.cp(qfl[:, c0:c0 + cw], ps[:, 0:cw], [ps.name], [qf[b].name], eng='act' if ci else 'dve')
                self.cp(qb[b][:], qf[b][:], [qf[b].name], [qb[b].name], eng='act')
                for (o, x, y, op, rn) in self.rope(qf[b][:], qb[b][:], 8, 64, 16, self.cosM, self.sinM, n, tmp,
                                                   [qf[b].name, 'cosM', 'sinM']):
                    self.tt(o, x, y, op, rn, [qb[b].name])
                kvfl = kvf[b][:].rearrange("p h d -> p (h d)")
                for ci, c0 in enumerate((0, 512)):
                    ps = self.ps[2 + ci]
                    for kc in range(2):
                        self.mm(ps[:], ckv[b][:, kc, :], wukv[:, kc, c0:c0 + 512], kc == 0, kc == 1,
                                [ckv[b].name, 'mu_wkv'], [ps.name])
                    self.cp(kvfl[:, c0:c0 + 512], ps[:], [ps.name], [kvf[b].name], eng='act' if ci else 'dve')
                self.cp(kb_[b][:, :, 0:64], kvf[b][:, :, 0:64], [kvf[b].name], [kb_[b].name])
                self.cp(kb_[b][:, :, 64:96], kp[b][:].broadcast_to([128, 8, 32]), [kp[b].name], [kb_[b].name])
                self.cp(vb[b][:], kvf[b][:, :, 64:128], [kvf[b].name], [vb[b].name], eng='act')
                self.dma(Dm['vD'][rows], vb[b][:], [vb[b].name], [], q='pool')
                for (srcb, nm, i) in ((qb[b], 'qTD', 0), (kb_[b], 'kTD', 1)):
                    pbk = self.pb[i]
                    for h in range(8):
                        self.tr(pbk[0:96, h * 128:(h + 1) * 128], srcb[:, h, :], self.ident_b[:], [srcb.name, 'ident_b'],
                                [pbk.name])
                    self.cp(tsb[i][0:96, :], pbk[0:96, :], [pbk.name], [tsb[i].name], eng='act' if i else 'dve')
                    self.dma(Dm[nm][:, :, rows].rearrange("h d t -> d h t"),
                             tsb[i][0:96, :].rearrange("d (h t) -> d h t", h=8), [tsb[i].name], [], q='pool')
            self.S.flush()

    def phase_attn(self, qT, kT, v, Dk, scale, orow0, use_mask=False, tag="at"):
        T, NT, Dm = self.T, self.NT, self.Dm
        NQB = T // 512
        with contextlib.ExitStack() as st:
            ks = [self.sb(st, tag + "_k%d" % i, [128, T], BF16) for i in range(2)]
            qs = [self.sb(st, tag + "_q%d" % i, [128, T], BF16) for i in range(2)]
            vs = [self.sb(st, tag + "_v%d" % i, [128, NT, 64], BF16) for i in range(2)]
            pt = [self.sb(st, tag + "_p%d" % i, [128, 512], BF16) for i in range(3)]
            mk = [self.sb(st, tag + "_m%d" % i, [128, 512], BF16) for i in range(3)] if use_mask else None
            rden = self.sb(st, tag + "_rd", [64, 512], F32)
            ob = [self.sb(st, tag + "_o%d" % i, [64, 512], BF16) for i in range(2)]
            step = 0
            for h in range(8):
                b = h % 2
                self.dma(ks[b][0:Dk, :], kT[h, 0:Dk, :], [], [ks[b].name])
                self.dma(qs[b][0:Dk, :], qT[h, 0:Dk, :], [], [qs[b].name])
                self.dma(vs[b][:], v[:, h, :].rearrange("(n p) d -> p n d", p=128), [], [vs[b].name])
                for qb in range(NQB):
                    nkt = 4 * (qb + 1)
                    psN = self.ps[2 + (qb % 2)]
                    psD = self.ps[4 + (qb % 2)]
                    for kt in range(nkt):
                        psS = self.ps[step % 2]
                        p_ = pt[step % 3]
                        self.mm(psS[:], ks[b][0:Dk, kt * 128:(kt + 1) * 128], qs[b][0:Dk, qb * 512:(qb + 1) * 512],
                                True, True, [ks[b].name, qs[b].name], [psS.name])
                        self.act(p_[:], psS[:], AF.Exp, [psS.name], [p_.name], scale=scale)
                        if use_mask:
                            m_ = mk[step % 3]
                            self.dma(m_[:], Dm['maskT'][kt * 128:(kt + 1) * 128, qb * 512:(qb + 1) * 512], [], [m_.name])
                            self.tt(p_[:], p_[:], m_[:], ALU.mult, [p_.name, m_.name], [p_.name])
                        elif kt >= 4 * qb:
                            self.tt(p_[:], p_[:], self.cm[kt - 4 * qb][:], ALU.mult, [p_.name, 'cm%d' % (kt - 4 * qb)], [p_.name])
                        self.mm(psN[0:64, :], vs[b][:, kt, :], p_[:], kt == 0, kt == nkt - 1, [vs[b].name, p_.name], [psN.name])
                        self.mm(psD[0:64, :], self.ones_b[:, 0:64], p_[:], kt == 0, kt == nkt - 1, ['ones_b', p_.name],
                                [psD.name])
                        step += 1
                    o_ = ob[qb % 2]
                    self.S.op('dve', lambda e, psD=psD: e.reciprocal(out=rden[:], in_=psD[0:64, :]), reads=[psD.name],
                              writes=[rden.name])
                    self.tt(o_[:], psN[0:64, :], rden[:], ALU.mult, [psN.name, rden.name], [o_.name])
                    self.dma(Dm['oT'][orow0 + h * 64:orow0 + (h + 1) * 64, qb * 512:(qb + 1) * 512], o_[:], [o_.name], [],
                             q='pool')
            self.S.flush()

    def phase_moba_onehot(self):
        T, NB = self.T, self.NB
        with contextlib.ExitStack() as st:
            io = self.sb(st, "oh_io", [NB, T], F32)
            a = self.sb(st, "oh_a", [NB, T], F32)
            ob = self.sb(st, "oh_b", [NB, T], BF16)
            self.S.op('pool', lambda e: e.iota(io[:], pattern=[[1, T]], base=0, channel_multiplier=-256,
                                               allow_small_or_imprecise_dtypes=True), writes=['oh_io'])
            self.ts(a[:], io[:], 0.0, ALU.is_ge, ['oh_io'], ['oh_a'])
            self.ts(io[:], io[:], 255.0, ALU.is_le, ['oh_io'], ['oh_io'])
            self.tt(ob[:], a[:], io[:], ALU.mult, ['oh_a', 'oh_io'], ['oh_b'])
            for h in range(8):
                self.dma(self.Dm['kTA'][h, 64:64 + NB, :], ob[:], ['oh_b'], [], q='pool')
            self.S.flush()

    def phase_moba_gates(self):
        T, NT, NB, Dm = self.T, self.NT, self.NB, self.Dm
        W = max(NB, 8)
        with contextlib.ExitStack() as st:
            ks = [self.sb(st, "mg_k%d" % i, [64, T], BF16) for i in range(2)]
            kmf = self.sb(st, "mg_kmf", [64, NB], F32)
            km = self.sb(st, "mg_km", [64, 8, NB], BF16)
            qs = [self.sb(st, "mg_q%d" % i, [64, 8, 128], BF16) for i in range(2)]
            gm = self.sb(st, "mg_gm", [128, 8, W], F32)
            mx = self.sb(st, "mg_mx", [128, 8, 8], F32)
            sbias = self.sb(st, "mg_sb", [128, 8, NB], F32)
            sbb = self.sb(st, "mg_sbb", [128, 8, NB], BF16)
            tsb = [self.sb(st, "mg_ts%d" % i, [NB, 1024], BF16) for i in range(2)]
            for h in range(8):
                b = h % 2
                self.dma(ks[b][:], Dm['kTA'][h, 0:64, :], [], [ks[b].name])
                self.S.op('dve', lambda e, b=b: e.tensor_reduce(out=kmf[:], in_=ks[b][:].rearrange("d (n k) -> d n k", k=256),
                                                                axis=AX.X, op=ALU.add), reads=[ks[b].name], writes=['mg_kmf'])
                self.ts(km[:, h, :], kmf[:], 1.0 / 256, ALU.mult, ['mg_kmf'], ['mg_km'])
            for n in range(NT):
                b = n % 2
                own = n // 2
                cols = slice(n * 128, (n + 1) * 128)
                self.memset(sbias[:], -BIG, ['mg_sb'])
                self.memset(sbias[:, :, own:own + 1], 0.0, ['mg_sb'])
                if own > 0:
                    self.dma(qs[b][:], Dm['qTA'][:, 0:64, cols].rearrange("h d t -> d h t"), [], [qs[b].name])
                    ps = self.ps[n % 2]
                    for h in range(8):
                        self.mm(ps[:, h * NB:(h + 1) * NB], qs[b][:, h, :], km[:, h, :], True, True, [qs[b].name, 'mg_km'],
                                [ps.name])
                    self.memset(gm[:], -BIG, ['mg_gm'], eng='dve')
                    self.cp(gm[:, :, 0:own], ps[:, 0:8 * NB].rearrange("p (h n) -> p h n", h=8)[:, :, 0:own], [ps.name], ['mg_gm'])
                    for h in range(8):
                        self.S.op('dve', lambda e, h=h: e.max(out=mx[:, h, :], in_=gm[:, h, :]), reads=['mg_gm'], writes=['mg_mx'])
                        self.ts(sbias[:, h, 0:own], gm[:, h, 0:own], mx[:, h, 2:3], ALU.is_ge, ['mg_gm', 'mg_mx', 'mg_sb'],
                                ['mg_sb'], s2=1.0, op1=ALU.subtract)
                    self.ts(sbias[:, :, 0:own], sbias[:, :, 0:own], BIG, ALU.mult, ['mg_sb'], ['mg_sb'])
                self.cp(sbb[:], sbias[:], ['mg_sb'], ['mg_sbb'])
                pbk = self.pb[n % 2]
                for h in range(8):
                    self.tr(pbk[0:NB, h * 128:(h + 1) * 128], sbb[:, h, :], self.ident_b[:], ['mg_sbb', 'ident_b'], [pbk.name])
                t_ = tsb[n % 2]
                self.cp(t_[:], pbk[0:NB, :], [pbk.name], [t_.name], eng='act')
                self.dma(Dm['qTA'][:, 64:64 + NB, cols].rearrange("h d t -> d h t"), t_[:].rearrange("d (h t) -> d h t", h=8),
                         [t_.name], [], q='pool')
            self.S.flush()

    def phase_dsa_thr(self):
        T, NT, Dm = self.T, self.NT, self.Dm
        DELTA = 1e-10
        with contextlib.ExitStack() as st:
            ki = self.sb(st, "dt_ki", [64, T], BF16)
            srow = self.sb(st, "dt_srow", [128, T], F32)
            Isb = self.sb(st, "dt_I", [128, T], F32)
            work = self.sb(st, "dt_work", [128, T], F32)
            cmq = self.sb(st, "dt_cmq", [128, 128], F32)
            cmqn = self.sb(st, "dt_cmqn", [128, 128], F32)
            qi = [self.sb(st, "dt_qi%d" % i, [64, 4, 128], BF16) for i in range(2)]
            wsb = [self.sb(st, "dt_w%d" % i, [128, 4], F32) for i in range(2)]
            rt = [self.sb(st, "dt_r%d" % i, [128, 512], F32) for i in range(2)]
            mx = self.sb(st, "dt_mx", [128, 8], F32)
            thr = self.sb(st, "dt_thr", [128, NT], F32)
            thrT = self.sb(st, "dt_thrT", [NT, 128], F32)
            self.dma(ki[:], Dm['kiT'], [], ['dt_ki'])
            self.S.op('pool', lambda e: e.iota(srow[:], pattern=[[1, T]], base=0, channel_multiplier=0,
                                               allow_small_or_imprecise_dtypes=True), writes=['dt_srow'])
            self.S.op('pool', lambda e: e.iota(cmq[:], pattern=[[-1, 128]], base=0, channel_multiplier=1,
                                               allow_small_or_imprecise_dtypes=True), writes=['dt_cmq'])
            self.ts(cmq[:], cmq[:], 0.0, ALU.is_ge, ['dt_cmq'], ['dt_cmq'])
            self.ts(cmqn[:], cmq[:], 1.0, ALU.subtract, ['dt_cmq'], ['dt_cmqn'], s2=BIG, op1=ALU.mult)
            k = 0
            for n in range(NT):
                b = n % 2
                cols = slice(n * 128, (n + 1) * 128)
                nch = (n + 4) // 4
                nkp = nch * 512
                self.dma(qi[b][:], Dm['qiT'][:, :, cols].rearrange("h d t -> d h t"), [], [qi[b].name])
                self.dma(wsb[b][:], Dm['wtm'][cols, :], [], [wsb[b].name])
                for c in range(nch):
                    ch = slice(c * 512, (c + 1) * 512)
                    for h in range(4):
                        ps = self.ps[k % 4]
                        r_ = rt[k % 2]
                        k += 1
                        self.mm(ps[:], qi[b][:, h, :], ki[:, ch], True, True, [qi[b].name, 'dt_ki'], [ps.name])
                        self.act(r_[:], ps[:], AF.Relu, [ps.name], [r_.name])
                        if h == 0:
                            self.ts(Isb[:, ch], r_[:], wsb[b][:, 0:1], ALU.mult, [r_.name, wsb[b].name], ['dt_I'])
                        else:
                            self.stt(Isb[:, ch], r_[:], wsb[b][:, h:h + 1], Isb[:, ch], ALU.mult, ALU.add,
                                     [r_.name, wsb[b].name, 'dt_I'], ['dt_I'])
                self.ts(work[:, 0:nkp], Isb[:, 0:nkp], 0.0, ALU.is_equal, ['dt_I'], ['dt_work'])
                self.stt(work[:, 0:nkp], work[:, 0:nkp], -DELTA, srow[:, 0:nkp], ALU.mult, ALU.mult, ['dt_work', 'dt_srow'],
                         ['dt_work'])
                self.tt(Isb[:, 0:nkp], Isb[:, 0:nkp], work[:, 0:nkp], ALU.add, ['dt_I', 'dt_work'], ['dt_I'])
                self.tt(Isb[:, cols], Isb[:, cols], cmq[:], ALU.mult, ['dt_I', 'dt_cmq'], ['dt_I'])
                self.tt(Isb[:, cols], Isb[:, cols], cmqn[:], ALU.add, ['dt_I', 'dt_cmqn'], ['dt_I'])
                if (n + 1) * 128 < nkp:
                    self.memset(Isb[:, (n + 1) * 128:nkp], -BIG, ['dt_I'], eng='dve')
                self.cp(work[:, 0:nkp], Isb[:, 0:nkp], ['dt_I'], ['dt_work'], eng='pool')
                for r in range(32):
                    self.S.op('dve', lambda e, nkp=nkp: e.max(out=mx[:], in_=work[:, 0:nkp]), reads=['dt_work'], writes=['dt_mx'])
                    if r < 31:
                        self.S.op('dve', lambda e, nkp=nkp: e.match_replace(out=work[:, 0:nkp], in_to_replace=mx[:],
                                                                   in_values=work[:, 0:nkp], imm_value=-BIG),
                                  reads=['dt_work', 'dt_mx'], writes=['dt_work'])
                self.cp(thr[:, n:n + 1], mx[:, 7:8], ['dt_mx'], ['dt_thr'])
            pw = self.ps[4]
            self.tr(pw[0:NT, 0:128], thr[:], self.ident_f[:], ['dt_thr', 'ident_f'], [pw.name])
            self.cp(thrT[:], pw[0:NT, 0:128], [pw.name], ['dt_thrT'])
            self.dma(Dm['thr'], thrT[:], ['dt_thrT'], [], q='pool')
            self.S.flush()

    def phase_dsa_mask(self):
        T, NT, Dm = self.T, self.NT, self.Dm
        DELTA = 1e-10
        NQB = T // 512
        with contextlib.ExitStack() as st:
            ki = self.sb(st, "dm_ki", [64, T], BF16)
            qi = [self.sb(st, "dm_qi%d" % i, [64, 4, 512], BF16) for i in range(2)]
            wb = [self.sb(st, "dm_wb%d" % i, [128, 4, 512], F32) for i in range(2)]
            thrb = [self.sb(st, "dm_thr%d" % i, [128, 512], F32) for i in range(2)]
            rt = [self.sb(st, "dm_r%d" % i, [128, 512], F32) for i in range(2)]
            acc = [self.sb(st, "dm_acc%d" % i, [128, 512], F32) for i in range(2)]
            tmp2 = self.sb(st, "dm_tmp2", [128, 512], F32)
            zt = self.sb(st, "dm_z", [128, 512], F32)
            mb = [self.sb(st, "dm_mb%d" % i, [128, 512], BF16) for i in range(2)]
            self.dma(ki[:], Dm['kiT'], [], ['dm_ki'])
            thr_flat = Dm['thr'].rearrange("n p -> (n p)").rearrange("(o t) -> o t", o=1)
            k = 0
            step = 0
            for qb in range(NQB):
                b = qb % 2
                cols = slice(qb * 512, (qb + 1) * 512)
                self.dma(qi[b][:], Dm['qiT'][:, :, cols].rearrange("h d t -> d h t"), [], [qi[b].name])
                for h in range(4):
                    self.dma(wb[b][:, h, :], Dm['wT'][h:h + 1, cols].partition_broadcast(128), [], [wb[b].name])
                self.dma(thrb[b][:], thr_flat[:, cols].partition_broadcast(128), [], [thrb[b].name])
                for kt in range(4 * (qb + 1)):
                    a_ = acc[step % 2]
                    m_ = mb[step % 2]
                    step += 1
                    for h in range(4):
                        ps = self.ps[k % 4]
                        r_ = rt[k % 2]
                        k += 1
                        self.mm(ps[:], ki[:, kt * 128:(kt + 1) * 128], qi[b][:, h, :], True, True, [qi[b].name, 'dm_ki'],
                                [ps.name])
                        self.act(r_[:], ps[:], AF.Relu, [ps.name], [r_.name])
                        if h == 0:
                            self.tt(a_[:], r_[:], wb[b][:, 0, :], ALU.mult, [r_.name, wb[b].name], [a_.name])
                        else:
                            self.tt(tmp2[:], r_[:], wb[b][:, h, :], ALU.mult, [r_.name, wb[b].name], ['dm_tmp2'])
                            self.tt(a_[:], a_[:], tmp2[:], ALU.add, [a_.name, 'dm_tmp2'], [a_.name])
                    self.ts(zt[:], a_[:], 0.0, ALU.is_equal, [a_.name], ['dm_z'])
                    self.ts(zt[:], zt[:], self.idx_tm[:, kt:kt + 1], ALU.mult, ['dm_z', 'idx_tm'], ['dm_z'], s2=-DELTA,
                            op1=ALU.mult)
                    self.tt(a_[:], a_[:], zt[:], ALU.add, [a_.name, 'dm_z'], [a_.name])
                    if kt >= 4 * qb:
                        self.tt(zt[:], a_[:], thrb[b][:], ALU.is_ge, [a_.name, thrb[b].name], ['dm_z'])
                        self.tt(m_[:], zt[:], self.cm[kt - 4 * qb][:], ALU.mult, ['dm_z', 'cm%d' % (kt - 4 * qb)], [m_.name])
                    else:
                        self.tt(m_[:], a_[:], thrb[b][:], ALU.is_ge, [a_.name, thrb[b].name], [m_.name])
                    self.dma(Dm['maskT'][kt * 128:(kt + 1) * 128, cols], m_[:], [m_.name], [], q='pool')
            self.S.flush()

    def phase_ssm(self, l):
        T, NT, Dm, I = self.T, self.NT, self.Dm, self.I
        featT, xbcA, proj = Dm['featT'], Dm['xbcA'], Dm['proj']
        TB = min(T, 2048)
        with contextlib.ExitStack() as st:
            xin = [self.sb(st, "sc_x%d" % i, [128, TB + 3], F32) for i in range(2)]
            acc = [self.sb(st, "sc_a%d" % i, [128, TB], F32) for i in range(2)]
            cw = self.sb(st, "sc_cw", [128, 8, 4], F32)
            cbias = self.sb(st, "sc_cb", [128, 8], F32)
            for j in range(4):
                self.dma(cw[:, :, j], I['conv_w'][l, j].rearrange("(c p) -> p c", p=128), [], ['sc_cw'], slow=True)
            self.dma(cbias[:], I['conv_b'][l].rearrange("(c p) -> p c", p=128), [], ['sc_cb'], slow=True)
            k = 0
            for c in range(8):
                r0 = 512 + c * 128
                for tb in range(T // TB):
                    x_ = xin[k % 2]
                    a_ = acc[k % 2]
                    k += 1
                    t0 = tb * TB
                    if tb == 0:
                        self.memset(x_[:, 0:3], 0.0, [x_.name], eng='dve')
                        self.dma(x_[:, 3:3 + TB], featT[r0:r0 + 128, 0:TB], [], [x_.name])
                    else:
                        self.dma(x_[:], featT[r0:r0 + 128, t0 - 3:t0 + TB], [], [x_.name])
                    self.ts(a_[:], x_[:, 0:TB], cw[:, c, 0:1], ALU.mult, [x_.name, 'sc_cw'], [a_.name])
                    for j in range(1, 4):
                        self.stt(a_[:], x_[:, j:j + TB], cw[:, c, j:j + 1], a_[:], ALU.mult, ALU.add, [x_.name, 'sc_cw', a_.name],
                                 [a_.name])
                    self.act(a_[:], a_[:], AF.Silu, [a_.name, 'sc_cb'], [a_.name], bias=cbias[:, c:c + 1])
                    self.dma(xbcA[c * 128:(c + 1) * 128, t0:t0 + TB], a_[:], [a_.name], [], q='pool')
            self.S.flush()
        with contextlib.ExitStack() as st:
            sb = lambda nm, sh, dt=F32: self.sb(st, "ss_" + nm, sh, dt)
            dtb, arow, dsk, nw = sb("dtb", [128, 8]), sb("arow", [128, 8]), sb("dsk", [64, 8]), sb("nw", [64, 8])
            xsT, zT = sb("xsT", [64, 8, 256]), sb("zT", [64, 8, 256])
            BT, CT = sb("BT", [128, 2, 256]), sb("CT", [128, 2, 256])
            BTb, CTb = sb("BTb", [128, 2, 256], BF16), sb("CTb", [128, 2, 256], BF16)
            dtr, dt, da = sb("dtr", [128, 2, 8]), sb("dt", [128, 2, 8]), sb("da", [128, 2, 8])
            acum, dte = sb("acum", [128, 2, 8]), sb("dte", [128, 2, 8])
            Dg = sb("Dg", [128, 8, 128])
            arow_ = sb("acr", [128, 8, 256])
            expA = sb("expA", [128, 8, 256])
            xdt = sb("xdt", [128, 2, 8, 64], BF16)
            Btm = sb("Btm", [128, 2, 2, 128])
            Bdec = sb("Bdec", [128, 2, 8, 128], BF16)
            CB = sb("CB", [128, 2, 2, 256])
            Cdec = sb("Cdec", [128, 8, 256], BF16)
            dif = [sb("dif%d" % i, [128, 256]) for i in range(2)]
            M0 = [sb("M0%d" % i, [128, 256], BF16) for i in range(2)]
            M1 = [sb("M1%d" % i, [128, 128], BF16) for i in range(2)]
            hT = sb("hT", [128, 8, 64])
            hTb = sb("hTb", [128, 8, 64], BF16)
            yall = sb("yall", [64, 8, 256])
            sz = sb("sz", [64, 8, 256])
            rstd = sb("rstd", [64, 256])
            ob = sb("ob", [64, 8, 256], BF16)
            bc = lambda ap, n=128: ap.rearrange("(o n) -> o n", o=1).partition_broadcast(n)
            self.dma(dtb[:], bc(I['dt_bias'][l]), [], ['ss_dtb'])
            self.dma(arow[:], bc(I['a_log'][l]), [], ['ss_arow'])
            self.dma(dsk[:], bc(I['d_skip'][l], 64), [], ['ss_dsk'])
            self.dma(nw[:], I['ssm_norm_w'][l].rearrange("(h p) -> p h", p=64), [], ['ss_nw'], slow=True)
            self.act(arow[:], arow[:], AF.Exp, ['ss_arow'], ['ss_arow'])
            self.ts(arow[:], arow[:], -1.0, ALU.mult, ['ss_arow'], ['ss_arow'])
            self.memset(hT[:], 0.0, ['ss_hT'])
            self.memset(hTb[:], 0.0, ['ss_hTb%d' % h for h in range(8)])
            for c in range(T // 256):
                cols = slice(c * 256, (c + 1) * 256)
                self.dma(xsT[:], xbcA[0:512, cols].rearrange("(h p) t -> p h t", p=64), [], ['ss_xsT'])
                self.dma(zT[:], featT[0:512, cols].rearrange("(h p) t -> p h t", p=64), [], ['ss_zT'])
                self.dma(BT[:], xbcA[512:768, cols].rearrange("(g n) t -> n g t", n=128), [], ['ss_BT'])
                self.dma(CT[:], xbcA[768:1024, cols].rearrange("(g n) t -> n g t", n=128), [], ['ss_CT'])
                for i in range(2):
                    self.dma(dtr[:, i, :], proj[c * 256 + i * 128:c * 256 + (i + 1) * 128, O_DT:O_DT + 8], [], ['ss_dtr'])
                self.cp(BTb[:], BT[:], ['ss_BT'], ['ss_BTb'], eng='pool')
                self.cp(CTb[:], CT[:], ['ss_CT'], ['ss_CTb'], eng='pool')
                self.tt(dt[:], dtr[:], dtb[:].unsqueeze(1).broadcast_to([128, 2, 8]), ALU.add, ['ss_dtr', 'ss_dtb'], ['ss_dt'])
                self.act(dt[:], dt[:], AF.Exp, ['ss_dt'], ['ss_dt'])
                self.act(dt[:], dt[:], AF.Ln, ['ss_dt'], ['ss_dt'], bias=1.0)
                self.tt(da[:], dt[:], arow[:].unsqueeze(1).broadcast_to([128, 2, 8]), ALU.mult, ['ss_dt', 'ss_arow'], ['ss_da'])
                p3 = self.ps[3]
                self.mm(p3[:, 0:8], self.tri_f[:], da[:, 0, :], True, True, ['tri_f', 'ss_da'], [p3.name])
                self.mm(p3[:, 8:16], self.ones_f[:], da[:, 0, :], True, False, ['ones_f', 'ss_da'], [p3.name])
                self.mm(p3[:, 8:16], self.tri_f[:], da[:, 1, :], False, True, ['tri_f', 'ss_da'], [p3.name])
                self.cp(acum[:].rearrange("p i h -> p (i h)"), p3[:, 0:16], [p3.name], ['ss_acum'])
                for i in range(2):
                    self.tt(Dg[:], self.ident_f[:].unsqueeze(1).broadcast_to([128, 8, 128]),
                            acum[:, i, :].unsqueeze(2).broadcast_to([128, 8, 128]), ALU.mult, ['ident_f', 'ss_acum'], ['ss_Dg'])
                    for j in range(2):
                        pj = self.ps[4 + j]
                        self.mm(pj[:], self.ones_f[:], Dg[:, 4 * j:4 * j + 4, :].rearrange("p h l -> p (h l)"), True, True,
                                ['ones_f', 'ss_Dg'], [pj.name])
                        self.cp(arow_[:, 4 * j:4 * j + 4, i * 128:(i + 1) * 128], pj[:].rearrange("p (h l) -> p h l", h=4), [pj.name],
                                ['ss_acr'], eng='act' if j else 'dve')
                self.act(expA[:], arow_[:], AF.Exp, ['ss_acr'], ['ss_expA'])
                for i in range(2):
                    self.tt(dte[:, i, :], arow_[:, :, 255], acum[:, i, :], ALU.subtract, ['ss_acr', 'ss_acum'], ['ss_dte'])
                self.act(dte[:], dte[:], AF.Exp, ['ss_dte'], ['ss_dte'])
                for i in range(2):
                    px = self.ps[i]
                    for h in range(8):
                        self.tr(px[:, h * 64:(h + 1) * 64], xsT[:, h, i * 128:(i + 1) * 128], self.ident_f[0:64, 0:64],
                                ['ss_xsT', 'ident_f'], [px.name])
                    self.tt(xdt[:, i], px[:].rearrange("p (h d) -> p h d", h=8), dt[:, i, :].unsqueeze(2).broadcast_to([128, 8, 64]),
                            ALU.mult, [px.name, 'ss_dt'], ['ss_xdt'])
                    pbm = self.ps[2]
                    for g in range(2):
                        self.tr(pbm[:, g * 128:(g + 1) * 128], BT[:, g, i * 128:(i + 1) * 128], self.ident_f[:], ['ss_BT', 'ident_f'],
                                [pbm.name])
                    self.cp(Btm[:, i].rearrange("p g n -> p (g n)"), pbm[:, 0:256], [pbm.name], ['ss_Btm'])
                    for h in range(8):
                        self.ts(Bdec[:, i, h, :], Btm[:, i, h // 4, :], dte[:, i, h:h + 1], ALU.mult, ['ss_Btm', 'ss_dte'], ['ss_Bdec'],
                                eng='pool' if h % 2 else 'dve')
                for g in range(2):
                    for i in range(2):
                        pc_ = self.ps[(g * 2 + i) % 2]
                        self.mm(pc_[:, 0:256], BTb[:, g, i * 128:(i + 1) * 128], CTb[:, g, :], True, True, ['ss_BTb', 'ss_CTb'],
                                [pc_.name])
                        self.cp(CB[:, g, i, :], pc_[:, 0:256], [pc_.name], ['ss_CB'], eng='act' if i else 'dve')
                for h in range(8):
                    self.tt(Cdec[:, h, :], CT[:, h // 4, :], expA[:, h, :], ALU.mult, ['ss_CT', 'ss_expA'], ['ss_Cdec'],
                            eng='pool' if h % 2 else 'dve')
                for h in range(8):
                    g = h // 4
                    d0, d1 = dif[0], dif[1]
                    m0, m1 = M0[h % 2], M1[h % 2]
                    self.ts(d0[:], arow_[:, h, :], acum[:, 0, h:h + 1], ALU.subtract, ['ss_acr', 'ss_acum'], [d0.name])
                    self.tt(d0[:, 0:128], d0[:, 0:128], self.cmneg[:], ALU.add, [d0.name, 'cmneg'], [d0.name])
                    self.act(d0[:], d0[:], AF.Exp, [d0.name], [d0.name])
                    self.tt(m0[:], CB[:, g, 0, :], d0[:], ALU.mult, ['ss_CB', d0.name], [m0.name])
                    self.ts(d1[:, 0:128], arow_[:, h, 128:256], acum[:, 1, h:h + 1], ALU.subtract, ['ss_acr', 'ss_acum'], [d1.name])
                    self.tt(d1[:, 0:128], d1[:, 0:128], self.cmneg[:], ALU.add, [d1.name, 'cmneg'], [d1.name])
                    self.act(d1[:, 0:128], d1[:, 0:128], AF.Exp, [d1.name], [d1.name])
                    self.tt(m1[:], CB[:, g, 1, 128:256], d1[:, 0:128], ALU.mult, ['ss_CB', d1.name], [m1.name])
                    py = self.ps[2 + h % 2]
                    hn = 'ss_hTb%d' % h
                    self.mm(py[0:64, 0:256], xdt[:, 0, h, :], m0[:], True, False, ['ss_xdt', m0.name], [py.name])
                    self.mm(py[0:64, 128:256], xdt[:, 1, h, :], m1[:], False, False, ['ss_xdt', m1.name], [py.name])
                    self.mm(py[0:64, 0:256], hTb[:, h, :], Cdec[:, h, :], False, True, [hn, 'ss_Cdec'], [py.name])
                    self.stt(yall[:, h, :], xsT[:, h, :], dsk[:, h:h + 1], py[0:64, 0:256], ALU.mult, ALU.add,
                             ['ss_xsT', 'ss_dsk', py.name], ['ss_yall'])
                    pst = self.ps[h % 2]
                    self.mm(pst[:, 0:64], Bdec[:, 0, h, :], xdt[:, 0, h, :], True, False, ['ss_Bdec', 'ss_xdt'], [pst.name])
                    self.mm(pst[:, 0:64], Bdec[:, 1, h, :], xdt[:, 1, h, :], False, True, ['ss_Bdec', 'ss_xdt'], [pst.name])
                    self.stt(hT[:, h, :], hT[:, h, :], expA[:, h, 255:256], pst[:, 0:64], ALU.mult, ALU.add,
                             ['ss_hT', 'ss_expA', pst.name], ['ss_hT'])
                    self.cp(hTb[:, h, :], hT[:, h, :], ['ss_hT'], [hn], eng='pool')
                self.act(sz[:], zT[:], AF.Silu, ['ss_zT'], ['ss_sz'])
                self.tt(yall[:], yall[:], sz[:], ALU.mult, ['ss_yall', 'ss_sz'], ['ss_yall'])
                self.tt(sz[:], yall[:], yall[:], ALU.mult, ['ss_yall'], ['ss_sz'])
                pss = self.ps[4]
                for h in range(8):
                    self.mm(pss[0:64, 0:256], self.ones_f[0:64, 0:64], sz[:, h, :], h == 0, h == 7, ['ones_f', 'ss_sz'], [pss.name])
                self.ts(rstd[:], pss[0:64, 0:256], 1.0 / 512, ALU.mult, [pss.name], ['ss_rstd'], s2=LN_EPS, op1=ALU.add)
                self.act(rstd[:], rstd[:], AF.Sqrt, ['ss_rstd'], ['ss_rstd'])
                self.S.op('dve', lambda e: e.reciprocal(out=rstd[:], in_=rstd[:]), reads=['ss_rstd'], writes=['ss_rstd'])
                for h in range(8):
                    self.stt(ob[:, h, :], yall[:, h, :], nw[:, h:h + 1], rstd[:], ALU.mult, ALU.mult, ['ss_yall', 'ss_nw', 'ss_rstd'],
                             ['ss_ob'])
                self.dma(Dm['oT'][512:1024, cols].rearrange("(h p) t -> p h t", p=64), ob[:], ['ss_ob'], [], q='pool')
            self.S.flush()

    def phase_merge(self, l):
        T, NT, Dm, I = self.T, self.NT, self.Dm, self.I
        with contextlib.ExitStack() as st:
            wb = self.sb(st, "mr_w", [128, 16, D_MODEL], BF16)
            self.dma(wb[:], I['w_branch'][l].rearrange("g (c p) n -> p (g c) n", p=128), [], ['mr_w'], q='pool')
            oT = [self.sb(st, "mr_o%d" % i, [128, 16, 128], BF16) for i in range(2)]
            gt = [self.sb(st, "mr_g%d" % i, [128, 4, D_MODEL], BF16) for i in range(2)]
            mg = [self.sb(st, "mr_m%d" % i, [128, D_MODEL], F32) for i in range(2)]
            tmp = [self.sb(st, "mr_t%d" % i, [128, 512], F32) for i in range(2)]
            mb = self.sb(st, "mr_mb", [128, D_MODEL], BF16)
            mT = [self.sb(st, "mr_mT%d" % i, [128, 16, 128], BF16) for i in range(2)]
            oTv = Dm['oT'].rearrange("(c p) t -> p c t", p=128)
            k = 0
            for n in range(NT):
                b = n % 2
                cols = slice(n * 128, (n + 1) * 128)
                self.dma(oT[b][:], oTv[:, :, cols], [], [oT[b].name])
                self.dma(gt[b][:], Dm['gates'][cols, :].rearrange("t (g d) -> t g d", g=4), [], [gt[b].name])
                for cc in range(4):
                    cs_ = slice(cc * 512, (cc + 1) * 512)
                    for g in range(4):
                        ps = self.ps[k % 4]
                        k += 1
                        for kc in range(4):
                            self.mm(ps[:], oT[b][:, g * 4 + kc, :], wb[:, g * 4 + kc, cs_], kc == 0, kc == 3, [oT[b].name, 'mr_w'],
                                    [ps.name])
                        if g == 0:
                            self.tt(mg[b][:, cs_], ps[:], gt[b][:, 0, cs_], ALU.mult, [ps.name, gt[b].name], [mg[b].name])
                        else:
                            t_ = tmp[g % 2]
                            self.tt(t_[:], ps[:], gt[b][:, g, cs_], ALU.mult, [ps.name, gt[b].name], [t_.name])
                            self.tt(mg[b][:, cs_], mg[b][:, cs_], t_[:], ALU.add, [mg[b].name, t_.name], [mg[b].name], eng='pool')
                self.cp(mb[:], mg[b][:], [mg[b].name], ['mr_mb'], eng='act')
                for g4 in range(2):
                    pbk = self.pb[g4]
                    for j in range(8):
                        c = g4 * 8 + j
                        self.tr(pbk[:, j * 128:(j + 1) * 128], mb[:, c * 128:(c + 1) * 128], self.ident_b[:], ['mr_mb', 'ident_b'],
                                [pbk.name])
                    self.cp(mT[b][:, g4 * 8:(g4 + 1) * 8, :], pbk[:].rearrange("p (a t) -> p a t", a=8), [pbk.name], [mT[b].name],
                            eng='act' if g4 else 'dve')
                self.dma(Dm['mergedT'].rearrange("(c p) t -> p c t", p=128)[:, :, cols], mT[b][:], [mT[b].name], [], q='pool')
            self.S.flush()

    def phase_wout(self, l):
        with contextlib.ExitStack() as st:
            ev = [self.sb(st, "wo_ev%d" % i, [128, 512], F32) for i in range(4)]
            cnt = [0]
            mixed = self.Dm['mixed']

            def ep(n, c0, cw, ps, pn):
                b = ev[cnt[0] % 4]
                self.cp(b[:, 0:cw], ps[:, 0:cw], [pn], [b.name], eng='act' if cnt[0] % 2 else 'dve')
                cnt[0] += 1
                self.dma(mixed[n * 128:(n + 1) * 128, c0:c0 + cw], b[:, 0:cw], [b.name], [], q='pool')
            self.linear_stream(st, self.Dm['mergedT'], D_MODEL, self.I['w_out'][l], D_MODEL, ep, "wo")
            self.S.flush()

    def layer_norm_tile(self, v, vn, g_bc, b_bc, junk, stat, out, outn):
        self.act(junk[:], v[:], AF.Identity, [vn], [junk.name, stat.name], accum=stat[:, 0:1])
        self.ts(stat[:, 0:1], stat[:, 0:1], 1.0 / D_MODEL, ALU.mult, [stat.name], [stat.name])
        self.ts(v[:], v[:], stat[:, 0:1], ALU.subtract, [vn, stat.name], [vn])
        self.act(junk[:], v[:], AF.Square, [vn], [junk.name, stat.name], accum=stat[:, 1:2])
        self.ts(stat[:, 1:2], stat[:, 1:2], 1.0 / D_MODEL, ALU.mult, [stat.name], [stat.name], s2=LN_EPS, op1=ALU.add)
        self.act(stat[:, 1:2], stat[:, 1:2], AF.Sqrt, [stat.name], [stat.name])
        self.S.op('dve', lambda e: e.reciprocal(out=stat[:, 1:2], in_=stat[:, 1:2]), reads=[stat.name], writes=[stat.name])
        self.stt(out[:], v[:], stat[:, 1:2], g_bc[:], ALU.mult, ALU.mult, [vn, stat.name, g_bc.name], [outn])
        self.tt(out[:], out[:], b_bc[:], ALU.add, [outn, b_bc.name], [outn], eng='pool')

    def phase_ln1_router(self, l, src):
        T, NT, Dm, I = self.T, self.NT, self.Dm, self.I
        bc = lambda ap: ap.rearrange("(o n) -> o n", o=1).partition_broadcast(128)
        with contextlib.ExitStack() as st:
            g_bc = self.sb(st, "l1_g", [128, D_MODEL], F32)
            b_bc = self.sb(st, "l1_b", [128, D_MODEL], F32)
            rw = self.sb(st, "l1_rw", [128, 16, N_EXPERTS], F32)
            rb = self.sb(st, "l1_rb", [128, N_EXPERTS], F32)
            self.dma(g_bc[:], bc(I['ln1_g'][l]), [], ['l1_g'])
            self.dma(b_bc[:], bc(I['ln1_b'][l]), [], ['l1_b'])
            self.dma(rw[:], I['router_w'][l].rearrange("(c p) e -> p c e", p=128), [], ['l1_rw'])
            self.dma(rb[:], bc(I['router_b'][l]), [], ['l1_rb'])
            xin = [self.sb(st, "l1_x%d" % i, [128, D_MODEL], F32) for i in range(2)]
            mx_ = [self.sb(st, "l1_m%d" % i, [128, D_MODEL], F32) for i in range(2)]
            x1 = [self.sb(st, "l1_o%d" % i, [128, D_MODEL], F32) for i in range(2)]
            junk = self.sb(st, "l1_junk", [128, D_MODEL], F32)
            stat = self.sb(st, "l1_stat", [128, 2], F32)
            xtf = self.sb(st, "l1_xtf", [128, 16, 128], F32)
            xts = [self.sb(st, "l1_xts%d" % i, [128, 16, 128], BF16) for i in range(2)]
            lg = self.sb(st, "l1_lg", [128, N_EXPERTS], F32)
            m8 = self.sb(st, "l1_m8", [128, 8], F32)
            sel = self.sb(st, "l1_sel", [128, N_EXPERTS], F32)
            ex = self.sb(st, "l1_ex", [128, N_EXPERTS], F32)
            den = self.sb(st, "l1_den", [128, 2], F32)
            if self.sparse:
                i8 = self.sb(st, "l1_i8", [128, 8], U32)
                i8f = self.sb(st, "l1_i8f", [128, 8], F32)
                e4 = self.sb(st, "l1_e4", [128, 4], F32)
                den4 = self.sb(st, "l1_den4", [128, 1], F32)
                selb = self.sb(st, "l1_selb", [128, N_EXPERTS], BF16)
                dfull = self.sb(st, "l1_dfull", [128, N_EXPERTS], F32)
                basecap = self.sb(st, "l1_basecap", [128, N_EXPERTS], F32)
                oh = self.sb(st, "l1_oh", [128, N_EXPERTS], F32)
                d4f = self.sb(st, "l1_d4f", [128, 4], F32)
                d4i = [self.sb(st, "l1_d4i%d" % i, [128, 4], I32) for i in range(2)]
                xb16 = [self.sb(st, "l1_xb%d" % i, [128, D_MODEL], BF16) for i in range(2)]
                self.ts(basecap[:], self.erow[:], float(self.CAP), ALU.mult, ['erow'], ['l1_basecap'])
            for n in range(NT):
                b = n % 2
                rows = slice(n * 128, (n + 1) * 128)
                self.dma(xin[b][:], src[rows, :], [], [xin[b].name])
                self.dma(mx_[b][:], Dm['mixed'][rows, :], [], [mx_[b].name])
                self.stt(mx_[b][:], xin[b][:], DN_ALPHA, mx_[b][:], ALU.mult, ALU.add, [xin[b].name, mx_[b].name], [mx_[b].name])
                self.layer_norm_tile(mx_[b], mx_[b].name, g_bc, b_bc, junk, stat, x1[b], x1[b].name)
                self.dma(Dm['x1'][rows, :], x1[b][:], [x1[b].name], [], q='pool')
                for g in range(4):
                    ps = self.ps[g]
                    for j in range(4):
                        c = g * 4 + j
                        self.tr(ps[:, j * 128:(j + 1) * 128], x1[b][:, c * 128:(c + 1) * 128], self.ident_f[:], [x1[b].name, 'ident_f'],
                                [ps.name])
                    self.cp(xtf[:, g * 4:(g + 1) * 4, :], ps[:].rearrange("p (a t) -> p a t", a=4), [ps.name], ['l1_xtf'],
                            eng='act' if g % 2 else 'dve')
                self.cp(xts[b][:], xtf[:], ['l1_xtf'], [xts[b].name], eng='pool')
                self.dma(Dm['x1T'].rearrange("(c p) t -> p c t", p=128)[:, :, rows], xts[b][:], [xts[b].name], [], q='pool')
                pl = self.ps[4 + n % 2]
                for kc in range(16):
                    self.mm(pl[:, 0:N_EXPERTS], xtf[:, kc, :], rw[:, kc, :], kc == 0, kc == 15, ['l1_xtf', 'l1_rw'], [pl.name])
                self.tt(lg[:], pl[:, 0:N_EXPERTS], rb[:], ALU.add, [pl.name, 'l1_rb'], ['l1_lg'])
                self.S.op('dve', lambda e: e.max(out=m8[:], in_=lg[:]), reads=['l1_lg'], writes=['l1_m8'])
                self.ts(sel[:], lg[:], m8[:, 3:4], ALU.is_ge, ['l1_lg', 'l1_m8'], ['l1_sel'])
                self.ts(den[:, 0:1], m8[:, 0:1], -1.0, ALU.mult, ['l1_m8'], ['l1_den'])
                self.act(ex[:], lg[:], AF.Exp, ['l1_lg', 'l1_den'], ['l1_ex'], bias=den[:, 0:1])
                self.tt(ex[:], ex[:], sel[:], ALU.mult, ['l1_ex', 'l1_sel'], ['l1_ex'])
                self.S.op('dve', lambda e: e.tensor_reduce(out=den[:, 1:2], in_=ex[:], axis=AX.X, op=ALU.add), reads=['l1_ex'],
                          writes=['l1_den'])
                self.S.op('dve', lambda e: e.reciprocal(out=den[:, 1:2], in_=den[:, 1:2]), reads=['l1_den'], writes=['l1_den'])
                self.ts(sel[:], ex[:], den[:, 1:2], ALU.mult, ['l1_ex', 'l1_den'], ['l1_sel'])
                self.dma(Dm['gsel'][rows, :], sel[:], ['l1_sel'], [], q='pool')
                if self.sparse:
                    self.S.op('dve', lambda e: e.max_index(out=i8[:], in_max=m8[:], in_values=lg[:]), reads=['l1_lg', 'l1_m8'],
                              writes=['l1_i8'])
                    self.cp(i8f[:], i8[:], ['l1_i8'], ['l1_i8f'])
                    self.act(e4[:], m8[:, 0:4], AF.Exp, ['l1_m8', 'l1_den'], ['l1_e4'], bias=den[:, 0:1])
                    self.S.op('dve', lambda e: e.tensor_reduce(out=den4[:, 0:1], in_=e4[:], axis=AX.X, op=ALU.add), reads=['l1_e4'],
                              writes=['l1_den4'])
                    self.S.op('dve', lambda e: e.reciprocal(out=den4[:, 0:1], in_=den4[:, 0:1]), reads=['l1_den4'], writes=['l1_den4'])
                    self.ts(e4[:], e4[:], den4[:, 0:1], ALU.mult, ['l1_e4', 'l1_den4'], ['l1_e4'])
                    self.dma(Dm['gsel4'][rows, :], e4[:], ['l1_e4'], [], q='pool')
                    self.ts(selb[:], lg[:], m8[:, 3:4], ALU.is_ge, ['l1_lg', 'l1_m8'], ['l1_selb'])
                    pp = self.ps[4 + (n + 1) % 2]
                    self.mm(pp[:, 0:N_EXPERTS], self.tris_b[:], selb[:], True, True, ['tris_b', 'l1_selb'], [pp.name])
                    self.mm(pp[:, 64:64 + N_EXPERTS], self.ones_b[:], selb[:], True, True, ['ones_b', 'l1_selb'], [pp.name])
                    self.tt(dfull[:], pp[:, 0:N_EXPERTS], basecap[:], ALU.add, [pp.name, 'l1_basecap'], ['l1_dfull'])
                    self.tt(basecap[:], pp[:, 64:64 + N_EXPERTS], basecap[:], ALU.add, [pp.name, 'l1_basecap', 'l1_dfull'], ['l1_basecap'])
                    for k4 in range(4):
                        self.ts(oh[:], self.erow[:], i8f[:, k4:k4 + 1], ALU.is_equal, ['erow', 'l1_i8f'], ['l1_oh'])
                        self.tt(oh[:], oh[:], dfull[:], ALU.mult, ['l1_oh', 'l1_dfull'], ['l1_oh'])
                        self.S.op('dve', lambda e, k4=k4: e.tensor_reduce(out=d4f[:, k4:k4 + 1], in_=oh[:], axis=AX.X, op=ALU.add),
                                  reads=['l1_oh'], writes=['l1_d4f'])
                    d4 = d4i[b]
                    self.cp(d4[:], d4f[:], ['l1_d4f'], [d4.name])
                    self.dma(Dm['dest4'][rows, :], d4[:], [d4.name], [], q='pool')
                    self.cp(xb16[b][:], x1[b][:], [x1[b].name], [xb16[b].name], eng='pool')
                    for k4 in range(4):
                        self.S.dma('pool', lambda e, k4=k4, d4=d4, xb=xb16[b]: e.indirect_dma_start(
                            out=Dm['xg'], out_offset=bass.IndirectOffsetOnAxis(ap=d4[:, k4:k4 + 1].bitcast(U32), axis=0),
                            in_=xb[:], in_offset=None), reads=[d4.name, xb16[b].name], writes=[])
            self.S.flush()

    def phase_moe_dense(self, l):
        T, NT, Dm, I = self.T, self.NT, self.Dm, self.I
        bc = lambda ap: ap.rearrange("(o n) -> o n", o=1).partition_broadcast(128)
        with contextlib.ExitStack() as st:
            wgu = self.sb(st, "mo_wgu", [128, 16, 2 * D_FF], BF16)
            wd = [self.sb(st, "mo_wd%d" % i, [128, 6, D_MODEL], BF16) for i in range(2)]
            bgu = [self.sb(st, "mo_bgu%d" % i, [128, 2 * D_FF], F32) for i in range(2)]
            bd = [self.sb(st, "mo_bd%d" % i, [128, D_MODEL], F32) for i in range(2)]
            gs = self.sb(st, "mo_gs", [128, NT, N_EXPERTS], F32)
            xt = [self.sb(st, "mo_x%d" % i, [128, 16, 128], BF16) for i in range(2)]
            hb = self.sb(st, "mo_hb", [128, 2 * D_FF], F32)
            sg = self.sb(st, "mo_sg", [128, D_FF], F32)
            ab = self.sb(st, "mo_ab", [128, D_FF], BF16)
            aT = self.sb(st, "mo_aT", [128, 6, 128], BF16)
            yp = [self.sb(st, "mo_yp%d" % i, [128, D_MODEL], F32) for i in range(2)]
            tq = [self.sb(st, "mo_tq%d" % i, [128, 512], F32) for i in range(2)]
            self.dma(gs[:], Dm['gsel'].rearrange("(n p) e -> p n e", p=128), [], ['mo_gs'])
            x1Tv = Dm['x1T'].rearrange("(c p) t -> p c t", p=128)
            k = 0
            for e in range(N_EXPERTS):
                eb = e % 2
                self.dma(wgu[:], I['w_gate_up'][l, e].rearrange("(c p) n -> p c n", p=128), [], ['mo_wgu'], q='pool')
                self.dma(wd[eb][:], I['w_down'][l, e].rearrange("(c p) n -> p c n", p=128), [], [wd[eb].name], q='pool')
                self.dma(bgu[eb][:], bc(I['b_gate_up'][l, e]), [], [bgu[eb].name])
                self.dma(bd[eb][:], bc(I['b_down'][l, e]), [], [bd[eb].name])
                for n in range(NT):
                    b = k % 2
                    k += 1
                    rows = slice(n * 128, (n + 1) * 128)
                    self.dma(xt[b][:], x1Tv[:, :, rows], [], [xt[b].name])
                    if e > 0:
                        self.dma(yp[b][:], Dm['yacc'][rows, :], ['yacc%d' % n], [yp[b].name])
                    for cc in range(3):
                        ps = self.ps[cc]
                        cs_ = slice(cc * 512, (cc + 1) * 512)
                        for kc in range(16):
                            self.mm(ps[:], xt[b][:, kc, :], wgu[:, kc, cs_], kc == 0, kc == 15, [xt[b].name, 'mo_wgu'], [ps.name])
                        self.tt(hb[:, cs_], ps[:], bgu[eb][:, cs_], ALU.add, [ps.name, bgu[eb].name], ['mo_hb'])
                    self.ts(hb[:, 0:D_FF], hb[:, 0:D_FF], 7.0, ALU.min, ['mo_hb'], ['mo_hb'])
                    self.act(sg[:], hb[:, 0:D_FF], AF.Sigmoid, ['mo_hb'], ['mo_sg'], scale=1.702)
                    self.ts(hb[:, D_FF:], hb[:, D_FF:], 7.0, ALU.min, ['mo_hb'], ['mo_hb'], s2=-7.0, op1=ALU.max)
                    self.tt(sg[:], sg[:], hb[:, 0:D_FF], ALU.mult, ['mo_sg', 'mo_hb'], ['mo_sg'], eng='pool')
                    self.stt(ab[:], hb[:, D_FF:], 1.0, sg[:], ALU.add, ALU.mult, ['mo_hb', 'mo_sg'], ['mo_ab'])
                    pbk = self.pb[k % 2]
                    for j in range(6):
                        self.tr(pbk[:, j * 128:(j + 1) * 128], ab[:, j * 128:(j + 1) * 128], self.ident_b[:], ['mo_ab', 'ident_b'],
                                [pbk.name])
                    self.cp(aT[:], pbk[:, 0:768].rearrange("p (a t) -> p a t", a=6), [pbk.name], ['mo_aT'], eng='act')
                    for cc in range(4):
                        ps = self.ps[(3 + cc) % 6]
                        cs_ = slice(cc * 512, (cc + 1) * 512)
                        for kc in range(6):
                            self.mm(ps[:], aT[:, kc, :], wd[eb][:, kc, cs_], kc == 0, kc == 5, ['mo_aT', wd[eb].name], [ps.name])
                        t_ = tq[cc % 2]
                        self.tt(t_[:], ps[:], bd[eb][:, cs_], ALU.add, [ps.name, bd[eb].name], [t_.name])
                        if e == 0:
                            self.ts(yp[b][:, cs_], t_[:], gs[:, n, e:e + 1], ALU.mult, [t_.name, 'mo_gs'], [yp[b].name])
                        else:
                            self.stt(yp[b][:, cs_], t_[:], gs[:, n, e:e + 1], yp[b][:, cs_], ALU.mult, ALU.add,
                                     [t_.name, 'mo_gs', yp[b].name], [yp[b].name])
                    self.dma(Dm['yacc'][rows, :], yp[b][:], [yp[b].name], ['yacc%d' % n], q='pool')
            self.S.flush()

    def phase_moe_zero(self):
        with contextlib.ExitStack() as st:
            z = self.sb(st, "mz_z", [128, D_MODEL], BF16)
            self.memset(z[:], 0.0, ['mz_z'])
            for r in range(N_EXPERTS * self.CAP // 128):
                self.dma(self.Dm['xg'][r * 128:(r + 1) * 128, :], z[:], ['mz_z'], [], q='sp' if r % 2 else 'pool')
            self.S.flush()

    def phase_moe_sparse(self, l):
        T, NT, Dm, I, CAP = self.T, self.NT, self.Dm, self.I, self.CAP
        bc = lambda ap: ap.rearrange("(o n) -> o n", o=1).partition_broadcast(128)
        with contextlib.ExitStack() as st:
            wgu1 = self.sb(st, "ms_wgu", [128, 16, 2 * D_FF], BF16)
            wgu = [wgu1, wgu1]
            wd = [self.sb(st, "ms_wd%d" % i, [128, 6, D_MODEL], BF16) for i in range(2)]
            bgu = [self.sb(st, "ms_bgu%d" % i, [128, 2 * D_FF], F32) for i in range(2)]
            bd = [self.sb(st, "ms_bd%d" % i, [128, D_MODEL], F32) for i in range(2)]
            xr = [self.sb(st, "ms_xr%d" % i, [128, D_MODEL], BF16) for i in range(2)]
            xt = self.sb(st, "ms_xt", [128, 16, 128], BF16)
            hb = self.sb(st, "ms_hb", [128, 2 * D_FF], F32)
            sg = self.sb(st, "ms_sg", [128, D_FF], F32)
            ab = self.sb(st, "ms_ab", [128, D_FF], BF16)
            aT = self.sb(st, "ms_aT", [128, 6, 128], BF16)
            yo = [self.sb(st, "ms_yo%d" % i, [128, D_MODEL], BF16) for i in range(2)]
            k = 0
            for e in range(N_EXPERTS):
                eb = e % 2
                self.dma(wgu[eb][:], I['w_gate_up'][l, e].rearrange("(c p) n -> p c n", p=128), [], [wgu[eb].name], q='pool')
                self.dma(wd[eb][:], I['w_down'][l, e].rearrange("(c p) n -> p c n", p=128), [], [wd[eb].name], q='pool')
                self.dma(bgu[eb][:], bc(I['b_gate_up'][l, e]), [], [bgu[eb].name])
                self.dma(bd[eb][:], bc(I['b_down'][l, e]), [], [bd[eb].name])
                for j in range(CAP // 128):
                    b = k % 2
                    k += 1
                    rows = slice(e * CAP + j * 128, e * CAP + (j + 1) * 128)
                    self.dma(xr[b][:], Dm['xg'][rows, :], [], [xr[b].name])
                    for g4 in range(2):
                        pbk = self.pb[g4]
                        for jj in range(8):
                            c = g4 * 8 + jj
                            self.tr(pbk[:, jj * 128:(jj + 1) * 128], xr[b][:, c * 128:(c + 1) * 128], self.ident_b[:],
                                    [xr[b].name, 'ident_b'], [pbk.name])
                        self.cp(xt[:, g4 * 8:(g4 + 1) * 8, :], pbk[:].rearrange("p (a t) -> p a t", a=8), [pbk.name], ['ms_xt'],
                                eng='act' if g4 else 'dve')
                    for cc in range(3):
                        ps = self.ps[cc]
                        cs_ = slice(cc * 512, (cc + 1) * 512)
                        for kc in range(16):
                            self.mm(ps[:], xt[:, kc, :], wgu[eb][:, kc, cs_], kc == 0, kc == 15, ['ms_xt', wgu[eb].name], [ps.name])
                        self.tt(hb[:, cs_], ps[:], bgu[eb][:, cs_], ALU.add, [ps.name, bgu[eb].name], ['ms_hb'])
                    self.ts(hb[:, 0:D_FF], hb[:, 0:D_FF], 7.0, ALU.min, ['ms_hb'], ['ms_hb'])
                    self.act(sg[:], hb[:, 0:D_FF], AF.Sigmoid, ['ms_hb'], ['ms_sg'], scale=1.702)
                    self.ts(hb[:, D_FF:], hb[:, D_FF:], 7.0, ALU.min, ['ms_hb'], ['ms_hb'], s2=-7.0, op1=ALU.max)
                    self.tt(sg[:], sg[:], hb[:, 0:D_FF], ALU.mult, ['ms_sg', 'ms_hb'], ['ms_sg'], eng='pool')
                    self.stt(ab[:], hb[:, D_FF:], 1.0, sg[:], ALU.add, ALU.mult, ['ms_hb', 'ms_sg'], ['ms_ab'])
                    pbk = self.pb[k % 2]
                    for jj in range(6):
                        self.tr(pbk[:, jj * 128:(jj + 1) * 128], ab[:, jj * 128:(jj + 1) * 128], self.ident_b[:], ['ms_ab', 'ident_b'],
                                [pbk.name])
                    self.cp(aT[:], pbk[:, 0:768].rearrange("p (a t) -> p a t", a=6), [pbk.name], ['ms_aT'], eng='act')
                    for cc in range(4):
                        ps = self.ps[(3 + cc) % 6]
                        cs_ = slice(cc * 512, (cc + 1) * 512)
                        for kc in range(6):
                            self.mm(ps[:], aT[:, kc, :], wd[eb][:, kc, cs_], kc == 0, kc == 5, ['ms_aT', wd[eb].name], [ps.name])
                        self.tt(yo[b][:, cs_], ps[:], bd[eb][:, cs_], ALU.add, [ps.name, bd[eb].name], [yo[b].name])
                    self.dma(Dm['og'][rows, :], yo[b][:], [yo[b].name], [], q='sp')
            self.S.flush()

    def phase_ln2(self, l, last):
        T, NT, Dm, I = self.T, self.NT, self.Dm, self.I
        bc = lambda ap: ap.rearrange("(o n) -> o n", o=1).partition_broadcast(128)
        with contextlib.ExitStack() as st:
            g_bc = self.sb(st, "l2_g", [128, D_MODEL], F32)
            b_bc = self.sb(st, "l2_b", [128, D_MODEL], F32)
            self.dma(g_bc[:], bc(I['ln2_g'][l]), [], ['l2_g'])
            self.dma(b_bc[:], bc(I['ln2_b'][l]), [], ['l2_b'])
            xin = [self.sb(st, "l2_x%d" % i, [128, D_MODEL], F32) for i in range(2)]
            yy = [self.sb(st, "l2_y%d" % i, [128, D_MODEL], F32) for i in range(2)]
            xo = [self.sb(st, "l2_o%d" % i, [128, D_MODEL], F32) for i in range(2)]
            junk = self.sb(st, "l2_junk", [128, D_MODEL], F32)
            stat = self.sb(st, "l2_stat", [128, 2], F32)
            xts = [self.sb(st, "l2_xts%d" % i, [128, 16, 128], BF16) for i in range(2)]
            if self.sparse:
                d4 = [self.sb(st, "l2_d4%d" % i, [128, 4], I32) for i in range(2)]
                g4 = [self.sb(st, "l2_g4%d" % i, [128, 4], F32) for i in range(2)]
                gk = [self.sb(st, "l2_gk%d" % i, [128, D_MODEL], BF16) for i in range(2)]
            dst = self.out if last else Dm['xres']
            for n in range(NT):
                b = n % 2
                rows = slice(n * 128, (n + 1) * 128)
                self.dma(xin[b][:], Dm['x1'][rows, :], [], [xin[b].name])
                if self.sparse:
                    self.dma(d4[b][:], Dm['dest4'][rows, :], [], [d4[b].name])
                    self.dma(g4[b][:], Dm['gsel4'][rows, :], [], [g4[b].name])
                    for k4 in range(4):
                        gk_ = gk[k4 % 2]
                        self.S.dma('pool', lambda e, k4=k4, gk_=gk_, dd=d4[b]: e.indirect_dma_start(
                            out=gk_[:], out_offset=None, in_=Dm['og'],
                            in_offset=bass.IndirectOffsetOnAxis(ap=dd[:, k4:k4 + 1].bitcast(U32), axis=0)),
                            reads=[d4[b].name], writes=[gk_.name])
                        if k4 == 0:
                            self.ts(yy[b][:], gk_[:], g4[b][:, 0:1], ALU.mult, [gk_.name, g4[b].name], [yy[b].name])
                        else:
                            self.stt(yy[b][:], gk_[:], g4[b][:, k4:k4 + 1], yy[b][:], ALU.mult, ALU.add,
                                     [gk_.name, g4[b].name, yy[b].name], [yy[b].name])
                else:
                    self.dma(yy[b][:], Dm['yacc'][rows, :], [], [yy[b].name])
                self.stt(yy[b][:], xin[b][:], DN_ALPHA, yy[b][:], ALU.mult, ALU.add, [xin[b].name, yy[b].name], [yy[b].name])
                self.layer_norm_tile(yy[b], yy[b].name, g_bc, b_bc, junk, stat, xo[b], xo[b].name)
                self.dma(dst[rows, :], xo[b][:], [xo[b].name], [], q='pool')
                if not last:
                    self.emit_xT(xo[b], xo[b].name, n, Dm['xT'], xts[b], n)
            self.S.flush()

    def finish(self):
        for nm in self.dbg:
            src = self.Dm[nm]
            o = self.nc.dram_tensor("dbg_" + nm, list(src.shape), src.dtype, kind="ExternalOutput").ap()
            self.dma(o, src, [], ['dbgout'], q='sp')
        self.S.flush()


_CACHE = {}


def kernel(**inputs):
    T, L = SEQ, DEPTH
    if 'nc' not in _CACHE:
        _CACHE['kb'] = KB(T, L)
        _CACHE['nc'] = _CACHE['kb'].build()
    nc = _CACHE['nc']
    kb = _CACHE['kb']
    im = {k: np.ascontiguousarray(inputs[k]) for k in kb.I.keys()}
    im['x'] = im['x'].reshape(T, D_MODEL)
    res = run_bass_kernel_spmd(nc, [im], core_ids=[0])
    return res.results[0]['out'].reshape(1, T, D_MODEL)
```

```python
import contextlib
import math
import numpy as np
import concourse.bass as bass
import concourse.mybir as mybir
from concourse.bass_utils import run_bass_kernel_spmd

F32 = mybir.dt.float32
BF16 = mybir.dt.bfloat16
I32 = mybir.dt.int32
U32 = mybir.dt.uint32
ALU = mybir.AluOpType
AF = mybir.ActivationFunctionType
AX = mybir.AxisListType

D_MODEL = 2048
SEQ = 8192
DEPTH = 4
D_IN = 13804
BIG = 30000.0
LN_EPS = 1e-5
DN_ALPHA = (2 * DEPTH) ** 0.25
IDX_SCALE = 256 ** -0.5
N_EXPERTS = 32
D_FF = 768
O_AQ, O_AK, O_AV, O_Z, O_XBC, O_DT = 0, 512, 1024, 1536, 2048, 3072
O_CQ, O_CK, O_CV, O_QI, O_KI, O_WI = 3080, 3592, 4104, 4616, 4872, 4936
O_DCQ, O_DCKV, O_KPE, O_G = 4940, 5324, 5580, 5612


class Sched:
    CE = ('pe', 'act', 'dve', 'pool')
    NDMA = 24

    def __init__(self, nc, stack):
        self.nc = nc
        self.lists = {e: [] for e in ('pe', 'act', 'dve', 'pool', 'sp')}
        self.sem = {e: stack.enter_context(nc.semaphore("s_" + e)) for e in self.CE}
        self.cnt = {e: 0 for e in self.CE}
        self.dsem = [stack.enter_context(nc.semaphore("d%d" % i)) for i in range(self.NDMA)]
        self.dcnt = [0] * self.NDMA
        self.dnext = 0
        self.seen = {e: {} for e in self.lists}
        self.last_w = {}
        self.readers = {}
        self.ninst = 0

    @staticmethod
    def _norm(names):
        return [b.split('__L')[0] for b in names]

    def _deps(self, reads, writes):
        reads, writes = self._norm(reads), self._norm(writes)
        evs = []
        for b in reads:
            if b in self.last_w:
                evs.append(self.last_w[b])
        for b in writes:
            if b in self.last_w:
                evs.append(self.last_w[b])
            evs.extend(self.readers.get(b, ()))
        return evs

    def _waits(self, eng, evs):
        need = {}
        for (k, v) in evs:
            if eng == 'pe' and k == 'pe':
                continue
            if self.seen[eng].get(k, 0) >= v:
                continue
            need[k] = max(need.get(k, 0), v)
        for k, v in need.items():
            self.seen[eng][k] = v
        return list(need.items())

    def _commit(self, ev, reads, writes):
        reads, writes = self._norm(reads), self._norm(writes)
        for b in reads:
            self.readers.setdefault(b, []).append(ev)
        for b in writes:
            self.last_w[b] = ev
            self.readers[b] = []

    def op(self, eng, fn, reads=(), writes=()):
        waits = self._waits(eng, self._deps(reads, writes))
        self.cnt[eng] += 1
        ev = (eng, self.cnt[eng])
        self.lists[eng].append((waits, fn, (eng, 1)))
        self._commit(ev, reads, writes)
        return ev

    def dma(self, q, fn, reads=(), writes=()):
        k = self.dnext
        self.dnext = (self.dnext + 1) % self.NDMA
        evs = self._deps(reads, writes)
        dk = 'd%d' % k
        if self.dcnt[k] > 0:
            evs.append((dk, self.dcnt[k]))
        waits = self._waits(q, evs)
        self.dcnt[k] += 16
        ev = (dk, self.dcnt[k])
        self.lists[q].append((waits, fn, (dk, 16)))
        self._commit(ev, reads, writes)
        return ev

    def _semh(self, k):
        return self.sem[k] if k in self.sem else self.dsem[int(k[1:])]

    def _replay(self, name, e):
        for waits, fn, inc in self.lists[name]:
            for k, v in waits:
                e.wait_ge(self._semh(k), v)
            if fn is not None:
                fn(e).then_inc(self._semh(inc[0]), inc[1])
                self.ninst += 1

    def flush(self):
        evs = [(k, self.cnt[k]) for k in self.CE if self.cnt[k] > 0]
        evs += [('d%d' % i, self.dcnt[i]) for i in range(self.NDMA) if self.dcnt[i] > 0]
        for e in self.lists:
            self.lists[e].append((self._waits(e, evs), None, None))
        with self.nc.Block() as block:
            @block.tensor
            def _(e):
                self._replay('pe', e)

            @block.scalar
            def _(e):
                self._replay('act', e)

            @block.vector
            def _(e):
                self._replay('dve', e)

            @block.gpsimd
            def _(e):
                self._replay('pool', e)

            @block.sync
            def _(e):
                self._replay('sp', e)
        for e in self.lists:
            self.lists[e] = []
        self.last_w = {}
        self.readers = {}


class _LazyInputs(dict):
    def __init__(self, nc, shapes):
        super().__init__()
        self.nc, self.shapes = nc, shapes

    def __missing__(self, name):
        shape, dt = self.shapes[name]
        ap = self.nc.dram_tensor(name, shape, dt, kind="ExternalInput").ap()
        self[name] = ap
        return ap


class KB:
    def __init__(self, T, L, dbg=()):
        self.T = T
        self.L = L
        self.NT = T // 128
        self.NB = T // 256
        self.dbg = dbg
        self.nc = bass.Bass("TRN2", target_bir_lowering=False)
        self.cur_layer = 'c'
        self.sparse = True

    def mm(self, out, lhsT, rhs, start, stop, r, w):
        self.S.op('pe', lambda e: e.matmul(out, lhsT, rhs, start=start, stop=stop), reads=r, writes=w)

    def tr(self, out, in_, ident, r, w):
        self.S.op('pe', lambda e: e.transpose(out, in_, ident), reads=r, writes=w)

    def act(self, out, in_, func, r, w, bias=None, scale=None, accum=None):
        kw = {}
        if bias is not None:
            kw['bias'] = bias
        if scale is not None:
            kw['scale'] = scale
        if accum is not None:
            kw['accum_out'] = accum
        self.S.op('act', lambda e: e.activation(out=out, in_=in_, func=func, **kw), reads=r, writes=w)

    def tt(self, out, in0, in1, op, r, w, eng='dve'):
        self.S.op(eng, lambda e: e.tensor_tensor(out=out, in0=in0, in1=in1, op=op), reads=r, writes=w)

    def ts(self, out, in0, s1, op0, r, w, s2=None, op1=None, eng='dve', accum=None):
        kw = {}
        if op1 is not None:
            kw['op1'] = op1
        if accum is not None:
            kw['accum_out'] = accum
        self.S.op(eng, lambda e: e.tensor_scalar(out=out, in0=in0, scalar1=s1, scalar2=s2, op0=op0, **kw),
                  reads=r, writes=w)

    def stt(self, out, in0, scalar, in1, op0, op1, r, w, eng='dve'):
        self.S.op(eng, lambda e: e.scalar_tensor_tensor(out=out, in0=in0, scalar=scalar, in1=in1, op0=op0, op1=op1),
                  reads=r, writes=w)

    def cp(self, out, in_, r, w, eng='dve'):
        if eng == 'act':
            self.S.op('act', lambda e: e.activation(out=out, in_=in_, func=AF.Copy), reads=r, writes=w)
        else:
            self.S.op(eng, lambda e: e.tensor_copy(out=out, in_=in_), reads=r, writes=w)

    def memset(self, ap, val, w, eng='pool'):
        self.S.op(eng, lambda e: e.memset(ap, val), writes=w)

    def dma(self, out, in_, r, w, q='sp', slow=False):
        if slow:
            self.S.dma(q, lambda e: e.dma_start(out=out, in_=in_, allow_slow_non_contiguous=True), reads=r, writes=w)
        else:
            self.S.dma(q, lambda e: e.dma_start(out=out, in_=in_), reads=r, writes=w)

    def sb(self, st, name, shape, dt):
        return st.enter_context(self.nc.sbuf_tensor("%s__L%s" % (name, self.cur_layer), shape, dt))

    def dram(self, name, shape, dt):
        return self.nc.dram_tensor(name, shape, dt).ap()

    def build(self, n_phases=99):
        nc, T, L, NT = self.nc, self.T, self.L, self.NT
        self.shapes = {
            'x': ([T, D_MODEL], F32), 'positions': ([1, T], I32), 'w_in': ([L, D_MODEL, D_IN], F32),
            'b_gate': ([L, 4, D_MODEL], F32), 'conv_w': ([L, 4, 1024], F32), 'conv_b': ([L, 1024], F32),
            'dt_bias': ([L, 8], F32), 'a_log': ([L, 8], F32), 'd_skip': ([L, 8], F32),
            'ssm_norm_w': ([L, 512], F32), 'q_norm_w': ([L, 384], F32), 'w_uq': ([L, 384, 768], F32),
            'kv_norm_w': ([L, 256], F32), 'w_ukv': ([L, 256, 1024], F32),
            'w_branch': ([L, 4, 512, D_MODEL], F32), 'w_out': ([L, D_MODEL, D_MODEL], F32),
            'ln1_g': ([L, D_MODEL], F32), 'ln1_b': ([L, D_MODEL], F32),
            'router_w': ([L, D_MODEL, N_EXPERTS], F32), 'router_b': ([L, N_EXPERTS], F32),
            'w_gate_up': ([L, N_EXPERTS, D_MODEL, 2 * D_FF], F32), 'b_gate_up': ([L, N_EXPERTS, 2 * D_FF], F32),
            'w_down': ([L, N_EXPERTS, D_FF, D_MODEL], F32), 'b_down': ([L, N_EXPERTS, D_MODEL], F32),
            'ln2_g': ([L, D_MODEL], F32), 'ln2_b': ([L, D_MODEL], F32)}
        I = _LazyInputs(nc, self.shapes)
        self.I = I
        self.out = nc.dram_tensor("out", [T, D_MODEL], F32, kind="ExternalOutput").ap()

        Dm = {}
        Dm['xT'] = self.dram('xT', [D_MODEL, T], BF16)
        Dm['xres'] = self.dram('xres', [T, D_MODEL], F32)
        Dm['proj'] = self.dram('proj', [T, 5632], F32)
        Dm['gates'] = self.dram('gates', [T, 4 * D_MODEL], BF16)
        Dm['featT'] = self.dram('featT', [1536, T], F32)
        Dm['xbcA'] = self.dram('xbcA', [1024, T], F32)
        for nm in ('A', 'C'):
            Dm['qT' + nm] = self.dram('qT' + nm, [8, 96 if nm == 'A' else 64, T], BF16)
            Dm['kT' + nm] = self.dram('kT' + nm, [8, 96 if nm == 'A' else 64, T], BF16)
            Dm['v' + nm] = self.dram('v' + nm, [T, 8, 64], BF16)
        Dm['qTD'] = self.dram('qTD', [8, 96, T], BF16)
        Dm['kTD'] = self.dram('kTD', [8, 96, T], BF16)
        Dm['vD'] = self.dram('vD', [T, 8, 64], BF16)
        Dm['qiT'] = self.dram('qiT', [4, 64, T], BF16)
        Dm['kiT'] = self.dram('kiT', [64, T], BF16)
        Dm['wtm'] = self.dram('wtm', [T, 4], F32)
        Dm['wT'] = self.dram('wT', [4, T], F32)
        Dm['cqT'] = self.dram('cqT', [384, T], BF16)
        Dm['ckvT'] = self.dram('ckvT', [256, T], BF16)
        Dm['kpe'] = self.dram('kpe', [T, 32], BF16)
        Dm['thr'] = self.dram('thr', [self.NT, 128], F32)
        Dm['maskT'] = self.dram('maskT', [T, T], BF16)
        Dm['oT'] = self.dram('oT', [4 * 512, T], BF16)
        Dm['x1'] = self.dram('x1', [T, D_MODEL], F32)
        Dm['x1T'] = self.dram('x1T', [D_MODEL, T], BF16)
        Dm['gsel'] = self.dram('gsel', [T, N_EXPERTS], F32)
        Dm['mergedT'] = self.dram('mergedT', [D_MODEL, T], BF16)
        Dm['mixed'] = self.dram('mixed', [T, D_MODEL], F32)
        Dm['yacc'] = self.dram('yacc', [T, D_MODEL], F32)
        self.CAP = max(256, ((T * 4 // N_EXPERTS) * 3 // 2 + 127) // 128 * 128)
        Dm['xg'] = self.dram('xg', [N_EXPERTS * self.CAP, D_MODEL], BF16)
        Dm['og'] = self.dram('og', [N_EXPERTS * self.CAP, D_MODEL], BF16)
        Dm['gsel4'] = self.dram('gsel4', [T, 4], F32)
        Dm['dest4'] = self.dram('dest4', [T, 4], I32)
        self.Dm = Dm

        with contextlib.ExitStack() as gst:
            self.S = Sched(nc, gst)
            self.ps = [gst.enter_context(nc.psum_tensor("ps%d" % i, [128, 512], F32)) for i in range(6)]
            self.pb = [gst.enter_context(nc.psum_tensor("pb%d" % i, [128, 1024], BF16)) for i in range(2)]
            self.phase_consts(gst)
            self.S.flush()
            ph = 1
            for l in range(L):
                self.cur_layer = str(l)
                src = I['x'] if l == 0 else Dm['xres']
                if l == 0:
                    self.phase_xT(src)
                    self.phase_moba_onehot()
                    if self.sparse:
                        self.phase_moe_zero()
                if ph >= n_phases:
                    break
                steps = [lambda: self.phase_inproj(l), lambda: self.phase_prep(l), lambda: self.phase_mla_up(l),
                         lambda: self.phase_attn(Dm['qTD'], Dm['kTD'], Dm['vD'], 96, 96 ** -0.5, 3 * 512, tag="atD"),
                         lambda: self.phase_moba_gates(),
                         lambda: self.phase_attn(Dm['qTA'], Dm['kTA'], Dm['vA'], 64 + self.NB, 0.125, 0, tag="atA"),
                         lambda: self.phase_ssm(l),
                         lambda: self.phase_dsa_thr(),
                         lambda: self.phase_dsa_mask(),
                         lambda: self.phase_attn(Dm['qTC'], Dm['kTC'], Dm['vC'], 64, 0.125, 2 * 512, use_mask=True, tag="atC"),
                         lambda: self.phase_merge(l),
                         lambda: self.phase_wout(l),
                         lambda: self.phase_ln1_router(l, src),
                         lambda: (self.phase_moe_sparse(l) if self.sparse else self.phase_moe_dense(l)),
                         lambda: self.phase_ln2(l, l == L - 1)]
                stop = False
                for stp in steps:
                    stp()
                    ph += 1
                    if ph >= n_phases:
                        stop = True
                        break
                if stop:
                    break
            self.finish()
        return nc

    def phase_consts(self, gst):
        nc, T, NT = self.nc, self.T, self.NT
        sb = self.sb
        self.ident_f = sb(gst, "ident_f", [128, 128], F32)
        self.ident_b = sb(gst, "ident_b", [128, 128], BF16)
        self.ones_b = sb(gst, "ones_b", [128, 128], BF16)
        self.ones_f = sb(gst, "ones_f", [128, 128], F32)
        self.cm = [sb(gst, "cm%d" % j, [128, 512], BF16) for j in range(4)]
        self.pos_tm = sb(gst, "pos_tm", [128, NT], F32)
        self.idx_tm = sb(gst, "idx_tm", [128, NT], F32)
        self.cosP = sb(gst, "cosP", [128, NT, 8], F32)
        self.sinP = sb(gst, "sinP", [128, NT, 8], F32)
        self.cosM = sb(gst, "cosM", [128, NT, 16], F32)
        self.sinM = sb(gst, "sinM", [128, NT, 16], F32)
        self.tri_f = sb(gst, "tri_f", [128, 128], F32)
        self.cmneg = sb(gst, "cmneg", [128, 128], F32)
        self.tris_b = sb(gst, "tris_b", [128, 128], BF16)
        self.erow = sb(gst, "erow", [128, N_EXPERTS], F32)
        self.negpi = sb(gst, "negpi", [128, 1], F32)
        self.memset(self.negpi[:], -math.pi, ['negpi'])
        with contextlib.ExitStack() as st:
            io = sb(st, "c_io", [128, 512], F32)
            posi = sb(st, "c_posi", [128, NT], I32)
            ang = sb(st, "c_ang", [128, NT, 16], F32)
            rr = sb(st, "c_rr", [128, NT, 16], F32)
            uu = sb(st, "c_uu", [128, NT, 16], F32)
            ki = sb(st, "c_ki", [128, NT, 16], I32)
            self.S.op('pool', lambda e: e.iota(io[:, 0:128], pattern=[[1, 128]], base=0, channel_multiplier=-1,
                                               allow_small_or_imprecise_dtypes=True), writes=['c_io'])
            self.ts(self.ident_f[:], io[:, 0:128], 0.0, ALU.is_equal, ['c_io'], ['ident_f'])
            self.cp(self.ident_b[:], self.ident_f[:], ['ident_f'], ['ident_b'])
            self.ts(self.tri_f[:], io[:, 0:128], 0.0, ALU.is_ge, ['c_io'], ['tri_f'])
            self.ts(self.cmneg[:], self.tri_f[:], 1.0, ALU.subtract, ['tri_f'], ['cmneg'], s2=BIG, op1=ALU.mult)
            self.memset(self.ones_b[:], 1.0, ['ones_b'])
            self.S.op('pool', lambda e: e.iota(io[:, 0:128], pattern=[[1, 128]], base=-1, channel_multiplier=-1,
                                               allow_small_or_imprecise_dtypes=True), reads=['ident_f', 'tri_f'], writes=['c_io'])
            self.ts(self.tris_b[:], io[:, 0:128], 0.0, ALU.is_ge, ['c_io'], ['tris_b'])
            self.S.op('pool', lambda e: e.iota(self.erow[:], pattern=[[1, N_EXPERTS]], base=0, channel_multiplier=0,
                                               allow_small_or_imprecise_dtypes=True), writes=['erow'])
            self.memset(self.ones_f[:], 1.0, ['ones_f'])
            for j in range(4):
                self.S.op('pool', lambda e, j=j: e.iota(io[:], pattern=[[1, 512]], base=-128 * j, channel_multiplier=-1,
                                                        allow_small_or_imprecise_dtypes=True),
                          reads=[], writes=['c_io'])
                self.ts(self.cm[j][:], io[:], 0.0, ALU.is_ge, ['c_io'], ['cm%d' % j])
            self.S.op('pool', lambda e: e.iota(self.idx_tm[:], pattern=[[128, NT]], base=0, channel_multiplier=1,
                                               allow_small_or_imprecise_dtypes=True), writes=['idx_tm'])
            self.dma(posi[:], self.I['positions'][0, :].rearrange("(n p) -> p n", p=128), [], ['c_posi'], slow=True)
            self.cp(self.pos_tm[:], posi[:], ['c_posi'], ['pos_tm'])
            for (cs, sn, nf, dim, theta) in ((self.cosP, self.sinP, 8, 16, 500000.0), (self.cosM, self.sinM, 16, 32, 10000.0)):
                for i in range(nf):
                    inv = float(np.float32(theta) ** (-np.float32(2 * i) / np.float32(dim)))
                    self.ts(ang[:, :, i], self.pos_tm[:], inv, ALU.mult, ['pos_tm'], ['c_ang'])
                tmp = ang[:, :, 0:nf]
                for (dst, shift) in ((sn, 0.0), (cs, 0.5 * math.pi)):
                    self.ts(rr[:, :, 0:nf], tmp, shift, ALU.add, ['c_ang'], ['c_rr'])
                    self.ts(uu[:, :, 0:nf], rr[:, :, 0:nf], 1.0 / (2 * math.pi), ALU.mult, ['c_rr'], ['c_uu'])
                    self.cp(ki[:, :, 0:nf], uu[:, :, 0:nf], ['c_uu'], ['c_ki'])
                    self.cp(uu[:, :, 0:nf], ki[:, :, 0:nf], ['c_ki'], ['c_uu'])
                    self.stt(rr[:, :, 0:nf], uu[:, :, 0:nf], -2 * math.pi, rr[:, :, 0:nf], ALU.mult, ALU.add,
                             ['c_uu', 'c_rr'], ['c_rr'])
                    self.ts(uu[:, :, 0:nf], rr[:, :, 0:nf], math.pi, ALU.is_gt, ['c_rr'], ['c_uu'])
                    self.stt(rr[:, :, 0:nf], uu[:, :, 0:nf], -2 * math.pi, rr[:, :, 0:nf], ALU.mult, ALU.add,
                             ['c_uu', 'c_rr'], ['c_rr'])
                    self.act(dst[:], rr[:, :, 0:nf], AF.Sin, ['c_rr'], [dst.name])
            self.S.flush()

    def negpi_ap(self, gst):
        return self.negpi[:, 0:1]

    def emit_xT(self, src_sb, src_name, n, dstT, xts, k):
        for g in range(4):
            ps = self.ps[(k * 4 + g) % 4]
            pn = ps.name
            for j in range(4):
                c = g * 4 + j
                self.tr(ps[:, j * 128:(j + 1) * 128], src_sb[:, c * 128:(c + 1) * 128], self.ident_f[:],
                        [src_name, 'ident_f'], [pn])
            self.cp(xts[:, g * 4:(g + 1) * 4, :], ps[:].rearrange("p (a b) -> p a b", a=4), [pn], [xts.name],
                    eng='act' if g % 2 else 'dve')
        self.dma(dstT.rearrange("(kc p) t -> p kc t", p=128)[:, :, n * 128:(n + 1) * 128], xts[:], [xts.name],
                 [], q='pool')

    def phase_xT(self, src):
        with contextlib.ExitStack() as st:
            xin = [self.sb(st, "xin%d" % i, [128, D_MODEL], F32) for i in range(2)]
            xts = [self.sb(st, "xts%d" % i, [128, 16, 128], BF16) for i in range(2)]
            for n in range(self.NT):
                b = n % 2
                self.dma(xin[b][:], src[n * 128:(n + 1) * 128, :], [], [xin[b].name])
                self.emit_xT(xin[b], xin[b].name, n, self.Dm['xT'], xts[b], n)
            self.S.flush()

    def linear_stream(self, st, aT, K, w, N, epilogue, tag):
        KC = K // 128
        wb = [self.sb(st, "%s_w%d" % (tag, i), [128, KC, 512], BF16) for i in range(2)]
        ab = [self.sb(st, "%s_a%d" % (tag, i), [128, KC, 512], BF16) for i in range(2)]
        aTv = aT.rearrange("(kc p) t -> p kc t", p=128)
        wv = w.rearrange("(kc p) n -> p kc n", p=128)
        it = 0
        pi = 0
        for ci, c0 in enumerate(range(0, N, 512)):
            cw = min(512, N - c0)
            wt = wb[ci % 2]
            self.dma(wt[:, :, 0:cw], wv[:, :, c0:c0 + cw], [], [wt.name], q='pool')
            for tb in range(self.T // 512):
                at = ab[it % 2]
                it += 1
                self.dma(at[:], aTv[:, :, tb * 512:(tb + 1) * 512], [], [at.name])
                for j in range(4):
                    ps = self.ps[pi % 4]
                    pi += 1
                    for kc in range(KC):
                        self.mm(ps[:, 0:cw], at[:, kc, j * 128:(j + 1) * 128], wt[:, kc, 0:cw], kc == 0, kc == KC - 1,
                                [at.name, wt.name], [ps.name])
                    epilogue(tb * 4 + j, c0, cw, ps, ps.name)

    def phase_inproj(self, l):
        T = self.T
        w_in = self.I['w_in'][l]
        proj = self.Dm['proj']
        with contextlib.ExitStack() as st:
            ev = [self.sb(st, "ip_ev%d" % i, [128, 512], F32) for i in range(4)]
            cnt = [0]

            def ep_plain(base):
                def ep(n, c0, cw, ps, pn):
                    b = ev[cnt[0] % 4]
                    self.cp(b[:, 0:cw], ps[:, 0:cw], [pn], [b.name], eng='act' if cnt[0] % 2 else 'dve')
                    cnt[0] += 1
                    self.dma(proj[n * 128:(n + 1) * 128, base + c0:base + c0 + cw], b[:, 0:cw], [b.name], [], q='pool')
                return ep
            self.linear_stream(st, self.Dm['xT'], D_MODEL, w_in[:, 0:1536], 1536, ep_plain(0), "ipa")
            self.linear_stream(st, self.Dm['xT'], D_MODEL, w_in[:, 3072:5612], 5612 - 3072, ep_plain(3072), "ipb")
            self.S.flush()
        with contextlib.ExitStack() as st:
            bg = self.sb(st, "ip_bg", [128, 4 * D_MODEL], F32)
            gf = [self.sb(st, "ip_gf%d" % i, [128, 512], F32) for i in range(2)]
            gb = [self.sb(st, "ip_gb%d" % i, [128, 512], BF16) for i in range(2)]
            self.dma(bg[:], self.I['b_gate'][l].rearrange("g d -> (g d)").rearrange("(o n) -> o n", o=1).partition_broadcast(128),
                     [], ['ip_bg'])
            cnt = [0]
            gates = self.Dm['gates']

            def ep_gate(n, c0, cw, ps, pn):
                i = cnt[0] % 2
                cnt[0] += 1
                self.tt(gf[i][:, 0:cw], ps[:, 0:cw], bg[:, c0:c0 + cw], ALU.add, [pn, 'ip_bg'], [gf[i].name])
                self.act(gb[i][:, 0:cw], gf[i][:, 0:cw], AF.Sigmoid, [gf[i].name], [gb[i].name])
                self.dma(gates[n * 128:(n + 1) * 128, c0:c0 + cw], gb[i][:, 0:cw], [gb[i].name], [], q='pool')
            self.linear_stream(st, self.Dm['xT'], D_MODEL, w_in[:, O_G:O_G + 4 * D_MODEL], 4 * D_MODEL, ep_gate, "ipg")
            self.S.flush()
        with contextlib.ExitStack() as st:
            NCH = 12
            wf = self.sb(st, "ipf_w", [128, 16, NCH * 128], BF16)
            ab = [self.sb(st, "ipf_a%d" % i, [128, 16, 512], BF16) for i in range(2)]
            ev = [self.sb(st, "ipf_ev%d" % i, [128, 512], F32) for i in range(4)]
            self.dma(wf[:], w_in[:, O_Z:O_Z + NCH * 128].rearrange("(kc p) n -> p kc n", p=128), [], ['ipf_w'], q='pool')
            aTv = self.Dm['xT'].rearrange("(kc p) t -> p kc t", p=128)
            k = 0
            for tb in range(T // 512):
                at = ab[tb % 2]
                self.dma(at[:], aTv[:, :, tb * 512:(tb + 1) * 512], [], [at.name])
                for c in range(NCH):
                    ps = self.ps[k % 4]
                    for kc in range(16):
                        self.mm(ps[:], wf[:, kc, c * 128:(c + 1) * 128], at[:, kc, :], kc == 0, kc == 15,
                                [at.name, 'ipf_w'], [ps.name])
                    b = ev[k % 4]
                    self.cp(b[:], ps[:], [ps.name], [b.name], eng='act' if k % 2 else 'dve')
                    k += 1
                    self.dma(self.Dm['featT'][c * 128:(c + 1) * 128, tb * 512:(tb + 1) * 512], b[:], [b.name], [], q='pool')
            self.S.flush()

    def rope(self, src, dst, H, o1, half, cs, sn, n, tmp, rd):
        x1 = src[:, :, o1:o1 + half]
        x2 = src[:, :, o1 + half:o1 + 2 * half]
        c = cs[:, n, :].unsqueeze(1).broadcast_to([128, H, half])
        sgn = sn[:, n, :].unsqueeze(1).broadcast_to([128, H, half])
        ta, tb_, tc, td = [t[:, 0:H, 0:half] for t in tmp]
        nm = [t.name for t in tmp]
        self.tt(ta, x1, c, ALU.mult, rd, [nm[0]])
        self.tt(tb_, x2, sgn, ALU.mult, rd, [nm[1]])
        self.tt(tc, x2, c, ALU.mult, rd, [nm[2]])
        self.tt(td, x1, sgn, ALU.mult, rd, [nm[3]])
        return ((dst[:, :, o1:o1 + half], ta, tb_, ALU.subtract, [nm[0], nm[1]]),
                (dst[:, :, o1 + half:o1 + 2 * half], tc, td, ALU.add, [nm[2], nm[3]]))

    def phase_prep(self, l):
        T, NT, Dm = self.T, self.NT, self.Dm
        proj = Dm['proj']
        with contextlib.ExitStack() as st:
            pa = [self.sb(st, "pp_a%d" % i, [128, 1536], F32) for i in range(2)]
            pc = [self.sb(st, "pp_c%d" % i, [128, 1860], F32) for i in range(2)]
            pd = [self.sb(st, "pp_d%d" % i, [128, 672], F32) for i in range(2)]
            ab = [self.sb(st, "pp_ab%d" % i, [128, 3, 8, 64], BF16) for i in range(2)]
            cb = [self.sb(st, "pp_cb%d" % i, [128, 1856], BF16) for i in range(2)]
            tmp = [self.sb(st, "pp_t%d" % i, [128, 8, 16], F32) for i in range(4)]
            tsb = [self.sb(st, "pp_ts%d" % i, [128, 1024], BF16) for i in range(2)]
            wsb = self.sb(st, "pp_w", [128, 4], F32)
            wts = self.sb(st, "pp_wt", [4, 128], F32)
            ssq = self.sb(st, "pp_ss", [128, 2], F32)
            junk = self.sb(st, "pp_junk", [128, 384], F32)
            qnw = self.sb(st, "pp_qnw", [128, 384], F32)
            kvnw = self.sb(st, "pp_kvnw", [128, 256], F32)
            cn = [self.sb(st, "pp_cn%d" % i, [128, 640], BF16) for i in range(2)]
            kpb = [self.sb(st, "pp_kp%d" % i, [128, 1, 32], BF16) for i in range(2)]
            self.dma(qnw[:], self.I['q_norm_w'][l].rearrange("(o n) -> o n", o=1).partition_broadcast(128), [], ['pp_qnw'])
            self.dma(kvnw[:], self.I['kv_norm_w'][l].rearrange("(o n) -> o n", o=1).partition_broadcast(128), [], ['pp_kvnw'])
            tcount = [0]

            def transposes(src_fn, H, Dk, dst_dram, tag):
                i = tcount[0] % 2
                tcount[0] += 1
                pbk = self.pb[i]
                for h in range(H):
                    self.tr(pbk[0:Dk, h * 128:(h + 1) * 128], src_fn(h), self.ident_b[:], [tag, 'ident_b'], [pbk.name])
                self.cp(tsb[i][0:Dk, 0:H * 128], pbk[0:Dk, 0:H * 128], [pbk.name], [tsb[i].name],
                        eng='act' if tcount[0] % 2 else 'dve')
                return tsb[i]

            for n in range(NT):
                b = n % 2
                rows = slice(n * 128, (n + 1) * 128)
                cols = slice(n * 128, (n + 1) * 128)
                self.dma(pa[b][:], proj[rows, 0:1536], [], [pa[b].name])
                self.dma(pc[b][:], proj[rows, O_CQ:O_CQ + 1860], [], [pc[b].name])
                self.dma(pd[b][:], proj[rows, O_DCQ:O_DCQ + 672], [], [pd[b].name])
                self.cp(ab[b][:].rearrange("p a h d -> p (a h d)"), pa[b][:], [pa[b].name], [ab[b].name], eng='act')
                for qi_ in range(2):
                    src = pa[b][:, qi_ * 512:(qi_ + 1) * 512].rearrange("p (h d) -> p h d", h=8)
                    for (o, x, y, op, rn) in self.rope(src, ab[b][:, qi_], 8, 0, 8, self.cosP, self.sinP, n, tmp,
                                                       [pa[b].name, 'cosP', 'sinP']):
                        self.tt(o, x, y, op, rn, [ab[b].name])
                for qi_, nm in ((0, 'qTA'), (1, 'kTA')):
                    t_ = transposes(lambda h: ab[b][:, qi_, h, :], 8, 64, None, ab[b].name)
                    self.dma(Dm[nm][:, 0:64, cols].rearrange("h d t -> d h t"),
                             t_[0:64, :].rearrange("d (h t) -> d h t", h=8), [t_.name], [], q='pool')
                self.dma(Dm['vA'][rows], ab[b][:, 2], [ab[b].name], [], q='pool')
                self.cp(cb[b][:], pc[b][:, 0:1856], [pc[b].name], [cb[b].name], eng='act')
                for (o0, H) in ((0, 8), (512, 8), (1536, 4), (1792, 1)):
                    src = pc[b][:, o0:o0 + H * 64].rearrange("p (h d) -> p h d", h=H)
                    dst = cb[b][:, o0:o0 + H * 64].rearrange("p (h d) -> p h d", h=H)
                    for (o, x, y, op, rn) in self.rope(src, dst, H, 0, 8, self.cosP, self.sinP, n, tmp,
                                                       [pc[b].name, 'cosP', 'sinP']):
                        self.tt(o, x, y, op, rn, [cb[b].name])
                for (o0, H, nm) in ((0, 8, 'qTC'), (512, 8, 'kTC'), (1536, 4, 'qiT')):
                    t_ = transposes(lambda h: cb[b][:, o0 + h * 64:o0 + (h + 1) * 64], H, 64, None, cb[b].name)
                    self.dma(Dm[nm][:, 0:64, cols].rearrange("h d t -> d h t"),
                             t_[0:64, 0:H * 128].rearrange("d (h t) -> d h t", h=H), [t_.name], [], q='pool')
                t_ = transposes(lambda h: cb[b][:, 1792:1856], 1, 64, None, cb[b].name)
                self.dma(Dm['kiT'][:, cols], t_[0:64, 0:128], [t_.name], [], q='pool')
                self.dma(Dm['vC'][rows], cb[b][:, 1024:1536].rearrange("p (h d) -> p h d", h=8), [cb[b].name], [], q='pool')
                self.ts(wsb[:], pc[b][:, 1856:1860], IDX_SCALE, ALU.mult, [pc[b].name], ['pp_w'])
                self.dma(Dm['wtm'][rows, :], wsb[:], ['pp_w'], [], q='pool')
                pw = self.ps[4]
                self.tr(pw[0:4, 0:128], wsb[:], self.ident_f[:], ['pp_w', 'ident_f'], [pw.name])
                self.cp(wts[:], pw[0:4, 0:128], [pw.name], ['pp_wt'])
                self.dma(Dm['wT'][:, cols], wts[:], ['pp_wt'], [], q='pool')
                for j, (o0, R, nw) in enumerate(((0, 384, qnw), (384, 256, kvnw))):
                    self.act(junk[:, 0:R], pd[b][:, o0:o0 + R], AF.Square, [pd[b].name], ['pp_junk', 'pp_ss%d' % j],
                             accum=ssq[:, j:j + 1])
                    self.ts(ssq[:, j:j + 1], ssq[:, j:j + 1], 1.0 / R, ALU.mult, ['pp_ss%d' % j], ['pp_ss%d' % j],
                            s2=LN_EPS, op1=ALU.add)
                    self.act(ssq[:, j:j + 1], ssq[:, j:j + 1], AF.Sqrt, ['pp_ss%d' % j], ['pp_ss%d' % j])
                    self.S.op('dve', lambda e, j=j: e.reciprocal(out=ssq[:, j:j + 1], in_=ssq[:, j:j + 1]),
                              reads=['pp_ss%d' % j], writes=['pp_ss%d' % j])
                    self.stt(cn[b][:, o0:o0 + R], pd[b][:, o0:o0 + R], ssq[:, j:j + 1], nw[:], ALU.mult, ALU.mult,
                             [pd[b].name, 'pp_ss%d' % j, nw.name], [cn[b].name])
                t_ = transposes(lambda h: cn[b][:, h * 128:(h + 1) * 128], 5, 128, None, cn[b].name)
                self.dma(Dm['cqT'].rearrange("(c p) t -> p c t", p=128)[:, :, cols],
                         t_[:, 0:384].rearrange("p (c t) -> p c t", c=3), [t_.name], [], q='pool')
                self.dma(Dm['ckvT'].rearrange("(c p) t -> p c t", p=128)[:, :, cols],
                         t_[:, 384:640].rearrange("p (c t) -> p c t", c=2), [t_.name], [], q='pool')
                src = pd[b][:, 640:672].rearrange("p (h d) -> p h d", h=1)
                for (o, x, y, op, rn) in self.rope(src, kpb[b][:], 1, 0, 16, self.cosM, self.sinM, n, tmp,
                                                   [pd[b].name, 'cosM', 'sinM']):
                    self.tt(o, x, y, op, rn, [kpb[b].name])
                self.dma(Dm['kpe'][rows, :], kpb[b][:, 0, :], [kpb[b].name], [], q='pool')
            self.S.flush()

    def phase_mla_up(self, l):
        T, NT, Dm = self.T, self.NT, self.Dm
        with contextlib.ExitStack() as st:
            wuq = self.sb(st, "mu_wq", [128, 3, 768], BF16)
            wukv = self.sb(st, "mu_wkv", [128, 2, 1024], BF16)
            self.dma(wuq[:], self.I['w_uq'][l].rearrange("(c p) n -> p c n", p=128), [], ['mu_wq'], q='pool')
            self.dma(wukv[:], self.I['w_ukv'][l].rearrange("(c p) n -> p c n", p=128), [], ['mu_wkv'], q='pool')
            cq = [self.sb(st, "mu_cq%d" % i, [128, 3, 128], BF16) for i in range(2)]
            ckv = [self.sb(st, "mu_ckv%d" % i, [128, 2, 128], BF16) for i in range(2)]
            kp = [self.sb(st, "mu_kp%d" % i, [128, 1, 32], BF16) for i in range(2)]
            qf = [self.sb(st, "mu_qf%d" % i, [128, 8, 96], F32) for i in range(2)]
            qb = [self.sb(st, "mu_qb%d" % i, [128, 8, 96], BF16) for i in range(2)]
            kvf = [self.sb(st, "mu_kvf%d" % i, [128, 8, 128], F32) for i in range(2)]
            kb_ = [self.sb(st, "mu_kb%d" % i, [128, 8, 96], BF16) for i in range(2)]
            vb = [self.sb(st, "mu_vb%d" % i, [128, 8, 64], BF16) for i in range(2)]
            tmp = [self.sb(st, "mu_t%d" % i, [128, 8, 16], F32) for i in range(4)]
            tsb = [self.sb(st, "mu_ts%d" % i, [128, 1024], BF16) for i in range(2)]
            cqv = Dm['cqT'].rearrange("(c p) t -> p c t", p=128)
            ckvv = Dm['ckvT'].rearrange("(c p) t -> p c t", p=128)
            for n in range(NT):
                b = n % 2
                rows = slice(n * 128, (n + 1) * 128)
                self.dma(cq[b][:], cqv[:, :, rows], [], [cq[b].name])
                self.dma(ckv[b][:], ckvv[:, :, rows], [], [ckv[b].name])
                self.dma(kp[b][:, 0, :], Dm['kpe'][rows, :], [], [kp[b].name])
                qfl = qf[b][:].rearrange("p h d -> p (h d)")
                for ci, (c0, cw) in enumerate(((0, 512), (512, 256))):
                    ps = self.ps[ci]
                    for kc in range(3):
                        self.mm(ps[:, 0:cw], cq[b][:, kc, :], wuq[:, kc, c0:c0 + cw], kc == 0, kc == 2,
                                [cq[b].name, 'mu_wq'], [ps.name])
                    self.cp(qfl[:, c0:c0 + cw], ps[:, 0:cw], [ps.name], [qf[b].name], eng='act' if ci else 'dve')
                self.cp(qb[b][:], qf[b][:], [qf[b].name], [qb[b].name], eng='act')
                for (o, x, y, op, rn) in self.rope(qf[b][:], qb[b][:], 8, 64, 16, self.cosM, self.sinM, n, tmp,
                                                   [qf[b].name, 'cosM', 'sinM']):
                    self.tt(o, x, y, op, rn, [qb[b].name])
                kvfl = kvf[b][:].rearrange("p h d -> p (h d)")
                for ci, c0 in enumerate((0, 512)):
                    ps = self.ps[2 + ci]
                    for kc in range(2):
                        self.mm(ps[:], ckv[b][:, kc, :], wukv[:, kc, c0:c0 + 512], kc == 0, kc == 1,
                                [ckv[b].name, 'mu_wkv'], [ps.name])
                    self.cp(kvfl[:, c0:c0 + 512], ps[:], [ps.name], [kvf[b].name], eng='act' if ci else 'dve')
                self.cp(kb_[b][:, :, 0:64], kvf[b][:, :, 0:64], [kvf[b].name], [kb_[b].name])
                self.cp(kb_[b][:, :, 64:96], kp[b][:].broadcast_to([128, 8, 32]), [kp[b].name], [kb_[b].name])
                self.cp(vb[b][:], kvf[b][:, :, 64:128], [kvf[b].name], [vb[b].name], eng='act')
                self.dma(Dm['vD'][rows], vb[b][:], [vb[b].name], [], q='pool')
                for (srcb, nm, i) in ((qb[b], 'qTD', 0), (kb_[b], 'kTD', 1)):
                    pbk = self.pb[i]
                    for h in range(8):
                        self.tr(pbk[0:96, h * 128:(h + 1) * 128], srcb[:, h, :], self.ident_b[:], [srcb.name, 'ident_b'],
                                [pbk.name])
                    self.cp(tsb[i][0:96, :], pbk[0:96, :], [pbk.name], [tsb[i].name], eng='act' if i else 'dve')
                    self.dma(Dm[nm][:, :, rows].rearrange("h d t -> d h t"),
                             tsb[i][0:96, :].rearrange("d (h t) -> d h t", h=8), [tsb[i].name], [], q='pool')
            self.S.flush()

    def phase_attn(self, qT, kT, v, Dk, scale, orow0, use_mask=False, tag="at"):
        T, NT, Dm = self.T, self.NT, self.Dm
        NQB = T // 512
        with contextlib.ExitStack() as st:
            ks = [self.sb(st, tag + "_k%d" % i, [128, T], BF16) for i in range(2)]
            qs = [self.sb(st, tag + "_q%d" % i, [128, T], BF16) for i in range(2)]
            vs = [self.sb(st, tag + "_v%d" % i, [128, NT, 128], BF16) for i in range(2)]
            pt = [self.sb(st, tag + "_p%d" % i, [128, 512], BF16) for i in range(3)]
            mk = [self.sb(st, tag + "_m%d" % i, [128, 512], BF16) for i in range(3)] if use_mask else None
            nd = [self.sb(st, tag + "_nd%d" % i, [128, 512], F32) for i in range(2)]
            dn = [self.sb(st, tag + "_dn%d" % i, [64, 512], F32) for i in range(2)]
            ob = [self.sb(st, tag + "_o%d" % i, [64, 512], BF16) for i in range(2)]
            for i in range(2):
                self.memset(vs[i][:, :, 64:128], 1.0, [vs[i].name])
            step = 0
            for h in range(8):
                b = h % 2
                self.dma(ks[b][0:Dk, :], kT[h, 0:Dk, :], [], [ks[b].name])
                self.dma(qs[b][0:Dk, :], qT[h, 0:Dk, :], [], [qs[b].name])
                self.dma(vs[b][:, :, 0:64], v[:, h, :].rearrange("(n p) d -> p n d", p=128), [], [vs[b].name])
                for qb in range(NQB):
                    nkt = 4 * (qb + 1)
                    psN = self.ps[2 + (qb % 2)]
                    for kt in range(nkt):
                        psS = self.ps[step % 2]
                        p_ = pt[step % 3]
                        self.mm(psS[:], ks[b][0:Dk, kt * 128:(kt + 1) * 128], qs[b][0:Dk, qb * 512:(qb + 1) * 512],
                                True, True, [ks[b].name, qs[b].name], [psS.name])
                        self.act(p_[:], psS[:], AF.Exp, [psS.name], [p_.name], scale=scale)
                        if use_mask:
                            m_ = mk[step % 3]
                            self.dma(m_[:], Dm['maskT'][kt * 128:(kt + 1) * 128, qb * 512:(qb + 1) * 512], [], [m_.name])
                            self.tt(p_[:], p_[:], m_[:], ALU.mult, [p_.name, m_.name], [p_.name])
                        elif kt >= 4 * qb:
                            self.tt(p_[:], p_[:], self.cm[kt - 4 * qb][:], ALU.mult, [p_.name, 'cm%d' % (kt - 4 * qb)], [p_.name])
                        self.mm(psN[:], vs[b][:, kt, :], p_[:], kt == 0, kt == nkt - 1, [vs[b].name, p_.name], [psN.name])
                        step += 1
                    i2 = qb % 2
                    o_ = ob[i2]
                    self.cp(nd[i2][:], psN[:], [psN.name], [nd[i2].name], eng='act' if i2 else 'dve')
                    self.dma(dn[i2][:], nd[i2][64:128, :], [nd[i2].name], [dn[i2].name])
                    self.S.op('dve', lambda e, d_=dn[i2]: e.reciprocal(out=d_[:], in_=d_[:]), reads=[dn[i2].name],
                              writes=[dn[i2].name])
                    self.tt(o_[:], nd[i2][0:64, :], dn[i2][:], ALU.mult, [nd[i2].name, dn[i2].name], [o_.name])
                    self.dma(Dm['oT'][orow0 + h * 64:orow0 + (h + 1) * 64, qb * 512:(qb + 1) * 512], o_[:], [o_.name], [],
                             q='pool')
            self.S.flush()

    def phase_moba_onehot(self):
        T, NB = self.T, self.NB
        with contextlib.ExitStack() as st:
            io = self.sb(st, "oh_io", [NB, T], F32)
            a = self.sb(st, "oh_a", [NB, T], F32)
            ob = self.sb(st, "oh_b", [NB, T], BF16)
            self.S.op('pool', lambda e: e.iota(io[:], pattern=[[1, T]], base=0, channel_multiplier=-256,
                                               allow_small_or_imprecise_dtypes=True), writes=['oh_io'])
            self.ts(a[:], io[:], 0.0, ALU.is_ge, ['oh_io'], ['oh_a'])
            self.ts(io[:], io[:], 255.0, ALU.is_le, ['oh_io'], ['oh_io'])
            self.tt(ob[:], a[:], io[:], ALU.mult, ['oh_a', 'oh_io'], ['oh_b'])
            for h in range(8):
                self.dma(self.Dm['kTA'][h, 64:64 + NB, :], ob[:], ['oh_b'], [], q='pool')
            self.S.flush()

    def phase_moba_gates(self):
        T, NT, NB, Dm = self.T, self.NT, self.NB, self.Dm
        W = max(NB, 8)
        with contextlib.ExitStack() as st:
            ks = [self.sb(st, "mg_k%d" % i, [64, T], BF16) for i in range(2)]
            kmf = self.sb(st, "mg_kmf", [64, NB], F32)
            km = self.sb(st, "mg_km", [64, 8, NB], BF16)
            qs = [self.sb(st, "mg_q%d" % i, [64, 8, 128], BF16) for i in range(2)]
            gm = self.sb(st, "mg_gm", [128, 8, W], F32)
            mx = self.sb(st, "mg_mx", [128, 8, 8], F32)
            sbias = self.sb(st, "mg_sb", [128, 8, NB], F32)
            sbb = self.sb(st, "mg_sbb", [128, 8, NB], BF16)
            tsb = [self.sb(st, "mg_ts%d" % i, [NB, 1024], BF16) for i in range(2)]
            for h in range(8):
                b = h % 2
                self.dma(ks[b][:], Dm['kTA'][h, 0:64, :], [], [ks[b].name])
                self.S.op('dve', lambda e, b=b: e.tensor_reduce(out=kmf[:], in_=ks[b][:].rearrange("d (n k) -> d n k", k=256),
                                                                axis=AX.X, op=ALU.add), reads=[ks[b].name], writes=['mg_kmf'])
                self.ts(km[:, h, :], kmf[:], 1.0 / 256, ALU.mult, ['mg_kmf'], ['mg_km'])
            for n in range(NT):
                b = n % 2
                own = n // 2
                cols = slice(n * 128, (n + 1) * 128)
                self.memset(sbias[:], -BIG, ['mg_sb'])
                self.memset(sbias[:, :, own:own + 1], 0.0, ['mg_sb'])
                if own > 0:
                    self.dma(qs[b][:], Dm['qTA'][:, 0:64, cols].rearrange("h d t -> d h t"), [], [qs[b].name])
                    ps = self.ps[n % 2]
                    for h in range(8):
                        self.mm(ps[:, h * NB:(h + 1) * NB], qs[b][:, h, :], km[:, h, :], True, True, [qs[b].name, 'mg_km'],
                                [ps.name])
                    self.memset(gm[:], -BIG, ['mg_gm'], eng='dve')
                    self.cp(gm[:, :, 0:own], ps[:, 0:8 * NB].rearrange("p (h n) -> p h n", h=8)[:, :, 0:own], [ps.name], ['mg_gm'])
                    for h in range(8):
                        self.S.op('dve', lambda e, h=h: e.max(out=mx[:, h, :], in_=gm[:, h, :]), reads=['mg_gm'], writes=['mg_mx'])
                        self.ts(sbias[:, h, 0:own], gm[:, h, 0:own], mx[:, h, 2:3], ALU.is_ge, ['mg_gm', 'mg_mx', 'mg_sb'],
                                ['mg_sb'], s2=1.0, op1=ALU.subtract)
                    self.ts(sbias[:, :, 0:own], sbias[:, :, 0:own], BIG, ALU.mult, ['mg_sb'], ['mg_sb'])
                self.cp(sbb[:], sbias[:], ['mg_sb'], ['mg_sbb'])
                pbk = self.pb[n % 2]
                for h in range(8):
                    self.tr(pbk[0:NB, h * 128:(h + 1) * 128], sbb[:, h, :], self.ident_b[:], ['mg_sbb', 'ident_b'], [pbk.name])
                t_ = tsb[n % 2]
                self.cp(t_[:], pbk[0:NB, :], [pbk.name], [t_.name], eng='act')
                self.dma(Dm['qTA'][:, 64:64 + NB, cols].rearrange("h d t -> d h t"), t_[:].rearrange("d (h t) -> d h t", h=8),
                         [t_.name], [], q='pool')
            self.S.flush()

    def phase_dsa_thr(self):
        T, NT, Dm = self.T, self.NT, self.Dm
        DELTA = 1e-10
        with contextlib.ExitStack() as st:
            ki = self.sb(st, "dt_ki", [64, T], BF16)
            srow = self.sb(st, "dt_srow", [128, T], F32)
            Isb = self.sb(st, "dt_I", [128, T], F32)
            work = self.sb(st, "dt_work", [128, T], F32)
            cmq = self.sb(st, "dt_cmq", [128, 128], F32)
            cmqn = self.sb(st, "dt_cmqn", [128, 128], F32)
            qi = [self.sb(st, "dt_qi%d" % i, [64, 4, 128], BF16) for i in range(2)]
            wsb = [self.sb(st, "dt_w%d" % i, [128, 4], F32) for i in range(2)]
            rt = [self.sb(st, "dt_r%d" % i, [128, 512], F32) for i in range(2)]
            mx = self.sb(st, "dt_mx", [128, 8], F32)
            thr = self.sb(st, "dt_thr", [128, NT], F32)
            thrT = self.sb(st, "dt_thrT", [NT, 128], F32)
            self.dma(ki[:], Dm['kiT'], [], ['dt_ki'])
            self.S.op('pool', lambda e: e.iota(srow[:], pattern=[[1, T]], base=0, channel_multiplier=0,
                                               allow_small_or_imprecise_dtypes=True), writes=['dt_srow'])
            self.S.op('pool', lambda e: e.iota(cmq[:], pattern=[[-1, 128]], base=0, channel_multiplier=1,
                                               allow_small_or_imprecise_dtypes=True), writes=['dt_cmq'])
            self.ts(cmq[:], cmq[:], 0.0, ALU.is_ge, ['dt_cmq'], ['dt_cmq'])
            self.ts(cmqn[:], cmq[:], 1.0, ALU.subtract, ['dt_cmq'], ['dt_cmqn'], s2=BIG, op1=ALU.mult)
            k = 0
            for n in range(NT):
                b = n % 2
                cols = slice(n * 128, (n + 1) * 128)
                nch = (n + 4) // 4
                nkp = nch * 512
                self.dma(qi[b][:], Dm['qiT'][:, :, cols].rearrange("h d t -> d h t"), [], [qi[b].name])
                self.dma(wsb[b][:], Dm['wtm'][cols, :], [], [wsb[b].name])
                for c in range(nch):
                    ch = slice(c * 512, (c + 1) * 512)
                    for h in range(4):
                        ps = self.ps[k % 4]
                        r_ = rt[k % 2]
                        k += 1
                        self.mm(ps[:], qi[b][:, h, :], ki[:, ch], True, True, [qi[b].name, 'dt_ki'], [ps.name])
                        self.act(r_[:], ps[:], AF.Relu, [ps.name], [r_.name])
                        if h == 0:
                            self.ts(Isb[:, ch], r_[:], wsb[b][:, 0:1], ALU.mult, [r_.name, wsb[b].name], ['dt_I'])
                        else:
                            self.stt(Isb[:, ch], r_[:], wsb[b][:, h:h + 1], Isb[:, ch], ALU.mult, ALU.add,
                                     [r_.name, wsb[b].name, 'dt_I'], ['dt_I'])
                self.ts(work[:, 0:nkp], Isb[:, 0:nkp], 0.0, ALU.is_equal, ['dt_I'], ['dt_work'])
                self.stt(work[:, 0:nkp], work[:, 0:nkp], -DELTA, srow[:, 0:nkp], ALU.mult, ALU.mult, ['dt_work', 'dt_srow'],
                         ['dt_work'])
                self.tt(Isb[:, 0:nkp], Isb[:, 0:nkp], work[:, 0:nkp], ALU.add, ['dt_I', 'dt_work'], ['dt_I'])
                self.tt(Isb[:, cols], Isb[:, cols], cmq[:], ALU.mult, ['dt_I', 'dt_cmq'], ['dt_I'])
                self.tt(Isb[:, cols], Isb[:, cols], cmqn[:], ALU.add, ['dt_I', 'dt_cmqn'], ['dt_I'])
                if (n + 1) * 128 < nkp:
                    self.memset(Isb[:, (n + 1) * 128:nkp], -BIG, ['dt_I'], eng='dve')
                self.cp(work[:, 0:nkp], Isb[:, 0:nkp], ['dt_I'], ['dt_work'], eng='pool')
                for r in range(32):
                    self.S.op('dve', lambda e, nkp=nkp: e.max(out=mx[:], in_=work[:, 0:nkp]), reads=['dt_work'], writes=['dt_mx'])
                    if r < 31:
                        self.S.op('dve', lambda e, nkp=nkp: e.match_replace(out=work[:, 0:nkp], in_to_replace=mx[:],
                                                                   in_values=work[:, 0:nkp], imm_value=-BIG),
                                  reads=['dt_work', 'dt_mx'], writes=['dt_work'])
                self.cp(thr[:, n:n + 1], mx[:, 7:8], ['dt_mx'], ['dt_thr'])
            pw = self.ps[4]
            self.tr(pw[0:NT, 0:128], thr[:], self.ident_f[:], ['dt_thr', 'ident_f'], [pw.name])
            self.cp(thrT[:], pw[0:NT, 0:128], [pw.name], ['dt_thrT'])
            self.dma(Dm['thr'], thrT[:], ['dt_thrT'], [], q='pool')
            self.S.flush()

    def phase_dsa_mask(self):
        T, NT, Dm = self.T, self.NT, self.Dm
        DELTA = 1e-10
        NQB = T // 512
        with contextlib.ExitStack() as st:
            ki = self.sb(st, "dm_ki", [64, T], BF16)
            qi = [self.sb(st, "dm_qi%d" % i, [64, 4, 512], BF16) for i in range(2)]
            wb = [self.sb(st, "dm_wb%d" % i, [128, 4, 512], F32) for i in range(2)]
            thrb = [self.sb(st, "dm_thr%d" % i, [128, 512], F32) for i in range(2)]
            rt = [self.sb(st, "dm_r%d" % i, [128, 512], F32) for i in range(2)]
            acc = [self.sb(st, "dm_acc%d" % i, [128, 512], F32) for i in range(2)]
            tmp2 = self.sb(st, "dm_tmp2", [128, 512], F32)
            zt = self.sb(st, "dm_z", [128, 512], F32)
            mb = [self.sb(st, "dm_mb%d" % i, [128, 512], BF16) for i in range(2)]
            self.dma(ki[:], Dm['kiT'], [], ['dm_ki'])
            thr_flat = Dm['thr'].rearrange("n p -> (n p)").rearrange("(o t) -> o t", o=1)
            k = 0
            step = 0
            for qb in range(NQB):
                b = qb % 2
                cols = slice(qb * 512, (qb + 1) * 512)
                self.dma(qi[b][:], Dm['qiT'][:, :, cols].rearrange("h d t -> d h t"), [], [qi[b].name])
                for h in range(4):
                    self.dma(wb[b][:, h, :], Dm['wT'][h:h + 1, cols].partition_broadcast(128), [], [wb[b].name])
                self.dma(thrb[b][:], thr_flat[:, cols].partition_broadcast(128), [], [thrb[b].name])
                for kt in range(4 * (qb + 1)):
                    a_ = acc[step % 2]
                    m_ = mb[step % 2]
                    step += 1
                    for h in range(4):
                        ps = self.ps[k % 4]
                        r_ = rt[k % 2]
                        k += 1
                        self.mm(ps[:], ki[:, kt * 128:(kt + 1) * 128], qi[b][:, h, :], True, True, [qi[b].name, 'dm_ki'],
                                [ps.name])
                        self.act(r_[:], ps[:], AF.Relu, [ps.name], [r_.name])
                        if h == 0:
                            self.tt(a_[:], r_[:], wb[b][:, 0, :], ALU.mult, [r_.name, wb[b].name], [a_.name])
                        else:
                            self.tt(tmp2[:], r_[:], wb[b][:, h, :], ALU.mult, [r_.name, wb[b].name], ['dm_tmp2'])
                            self.tt(a_[:], a_[:], tmp2[:], ALU.add, [a_.name, 'dm_tmp2'], [a_.name])
                    self.ts(zt[:], a_[:], 0.0, ALU.is_equal, [a_.name], ['dm_z'])
                    self.ts(zt[:], zt[:], self.idx_tm[:, kt:kt + 1], ALU.mult, ['dm_z', 'idx_tm'], ['dm_z'], s2=-DELTA,
                            op1=ALU.mult)
                    self.tt(a_[:], a_[:], zt[:], ALU.add, [a_.name, 'dm_z'], [a_.name])
                    if kt >= 4 * qb:
                        self.tt(zt[:], a_[:], thrb[b][:], ALU.is_ge, [a_.name, thrb[b].name], ['dm_z'])
                        self.tt(m_[:], zt[:], self.cm[kt - 4 * qb][:], ALU.mult, ['dm_z', 'cm%d' % (kt - 4 * qb)], [m_.name])
                    else:
                        self.tt(m_[:], a_[:], thrb[b][:], ALU.is_ge, [a_.name, thrb[b].name], [m_.name])
                    self.dma(Dm['maskT'][kt * 128:(kt + 1) * 128, cols], m_[:], [m_.name], [], q='pool')
            self.S.flush()

    def phase_ssm(self, l):
        T, NT, Dm, I = self.T, self.NT, self.Dm, self.I
        featT, xbcA, proj = Dm['featT'], Dm['xbcA'], Dm['proj']
        TB = min(T, 2048)
        with contextlib.ExitStack() as st:
            xin = [self.sb(st, "sc_x%d" % i, [128, TB + 3], F32) for i in range(2)]
            acc = [self.sb(st, "sc_a%d" % i, [128, TB], F32) for i in range(2)]
            cw = self.sb(st, "sc_cw", [128, 8, 4], F32)
            cbias = self.sb(st, "sc_cb", [128, 8], F32)
            for j in range(4):
                self.dma(cw[:, :, j], I['conv_w'][l, j].rearrange("(c p) -> p c", p=128), [], ['sc_cw'], slow=True)
            self.dma(cbias[:], I['conv_b'][l].rearrange("(c p) -> p c", p=128), [], ['sc_cb'], slow=True)
            k = 0
            for c in range(8):
                r0 = 512 + c * 128
                for tb in range(T // TB):
                    x_ = xin[k % 2]
                    a_ = acc[k % 2]
                    k += 1
                    t0 = tb * TB
                    if tb == 0:
                        self.memset(x_[:, 0:3], 0.0, [x_.name], eng='dve')
                        self.dma(x_[:, 3:3 + TB], featT[r0:r0 + 128, 0:TB], [], [x_.name])
                    else:
                        self.dma(x_[:], featT[r0:r0 + 128, t0 - 3:t0 + TB], [], [x_.name])
                    self.ts(a_[:], x_[:, 0:TB], cw[:, c, 0:1], ALU.mult, [x_.name, 'sc_cw'], [a_.name])
                    for j in range(1, 4):
                        self.stt(a_[:], x_[:, j:j + TB], cw[:, c, j:j + 1], a_[:], ALU.mult, ALU.add, [x_.name, 'sc_cw', a_.name],
                                 [a_.name])
                    self.act(a_[:], a_[:], AF.Silu, [a_.name, 'sc_cb'], [a_.name], bias=cbias[:, c:c + 1])
                    self.dma(xbcA[c * 128:(c + 1) * 128, t0:t0 + TB], a_[:], [a_.name], [], q='pool')
            self.S.flush()
        with contextlib.ExitStack() as st:
            sb = lambda nm, sh, dt=F32: self.sb(st, "ss_" + nm, sh, dt)
            dtb, arow, dsk, nw = sb("dtb", [128, 8]), sb("arow", [128, 8]), sb("dsk", [64, 8]), sb("nw", [64, 8])
            xsT, zT = sb("xsT", [64, 8, 256]), sb("zT", [64, 8, 256])
            BT, CT = sb("BT", [128, 2, 256]), sb("CT", [128, 2, 256])
            BTb, CTb = sb("BTb", [128, 2, 256], BF16), sb("CTb", [128, 2, 256], BF16)
            dtr, dt, da = sb("dtr", [128, 2, 8]), sb("dt", [128, 2, 8]), sb("da", [128, 2, 8])
            acum, dte = sb("acum", [128, 2, 8]), sb("dte", [128, 2, 8])
            Dg = sb("Dg", [128, 8, 128])
            arow_ = sb("acr", [128, 8, 256])
            expA = sb("expA", [128, 8, 256])
            xdt = sb("xdt", [128, 2, 8, 64], BF16)
            Btm = sb("Btm", [128, 2, 2, 128])
            Bdec = sb("Bdec", [128, 2, 8, 128], BF16)
            CB = sb("CB", [128, 2, 2, 256])
            Cdec = sb("Cdec", [128, 8, 256], BF16)
            dif = [sb("dif%d" % i, [128, 256]) for i in range(2)]
            M0 = [sb("M0%d" % i, [128, 256], BF16) for i in range(2)]
            M1 = [sb("M1%d" % i, [128, 128], BF16) for i in range(2)]
            hT = sb("hT", [128, 8, 64])
            hTb = sb("hTb", [128, 8, 64], BF16)
            yall = sb("yall", [64, 8, 256])
            sz = sb("sz", [64, 8, 256])
            rstd = sb("rstd", [64, 256])
            ob = sb("ob", [64, 8, 256], BF16)
            bc = lambda ap, n=128: ap.rearrange("(o n) -> o n", o=1).partition_broadcast(n)
            self.dma(dtb[:], bc(I['dt_bias'][l]), [], ['ss_dtb'])
            self.dma(arow[:], bc(I['a_log'][l]), [], ['ss_arow'])
            self.dma(dsk[:], bc(I['d_skip'][l], 64), [], ['ss_dsk'])
            self.dma(nw[:], I['ssm_norm_w'][l].rearrange("(h p) -> p h", p=64), [], ['ss_nw'], slow=True)
            self.act(arow[:], arow[:], AF.Exp, ['ss_arow'], ['ss_arow'])
            self.ts(arow[:], arow[:], -1.0, ALU.mult, ['ss_arow'], ['ss_arow'])
            self.memset(hT[:], 0.0, ['ss_hT'])
            self.memset(hTb[:], 0.0, ['ss_hTb%d' % h for h in range(8)])
            for c in range(T // 256):
                cols = slice(c * 256, (c + 1) * 256)
                self.dma(xsT[:], xbcA[0:512, cols].rearrange("(h p) t -> p h t", p=64), [], ['ss_xsT'])
                self.dma(zT[:], featT[0:512, cols].rearrange("(h p) t -> p h t", p=64), [], ['ss_zT'])
                self.dma(BT[:], xbcA[512:768, cols].rearrange("(g n) t -> n g t", n=128), [], ['ss_BT'])
                self.dma(CT[:], xbcA[768:1024, cols].rearrange("(g n) t -> n g t", n=128), [], ['ss_CT'])
                for i in range(2):
                    self.dma(dtr[:, i, :], proj[c * 256 + i * 128:c * 256 + (i + 1) * 128, O_DT:O_DT + 8], [], ['ss_dtr'])
                self.cp(BTb[:], BT[:], ['ss_BT'], ['ss_BTb'], eng='pool')
                self.cp(CTb[:], CT[:], ['ss_CT'], ['ss_CTb'], eng='pool')
                self.tt(dt[:], dtr[:], dtb[:].unsqueeze(1).broadcast_to([128, 2, 8]), ALU.add, ['ss_dtr', 'ss_dtb'], ['ss_dt'])
                self.act(dt[:], dt[:], AF.Exp, ['ss_dt'], ['ss_dt'])
                self.act(dt[:], dt[:], AF.Ln, ['ss_dt'], ['ss_dt'], bias=1.0)
                self.tt(da[:], dt[:], arow[:].unsqueeze(1).broadcast_to([128, 2, 8]), ALU.mult, ['ss_dt', 'ss_arow'], ['ss_da'])
                p3 = self.ps[3]
                self.mm(p3[:, 0:8], self.tri_f[:], da[:, 0, :], True, True, ['tri_f', 'ss_da'], [p3.name])
                self.mm(p3[:, 8:16], self.ones_f[:], da[:, 0, :], True, False, ['ones_f', 'ss_da'], [p3.name])
                self.mm(p3[:, 8:16], self.tri_f[:], da[:, 1, :], False, True, ['tri_f', 'ss_da'], [p3.name])
                self.cp(acum[:].rearrange("p i h -> p (i h)"), p3[:, 0:16], [p3.name], ['ss_acum'])
                for i in range(2):
                    self.tt(Dg[:], self.ident_f[:].unsqueeze(1).broadcast_to([128, 8, 128]),
                            acum[:, i, :].unsqueeze(2).broadcast_to([128, 8, 128]), ALU.mult, ['ident_f', 'ss_acum'], ['ss_Dg'])
                    for j in range(2):
                        pj = self.ps[4 + j]
                        self.mm(pj[:], self.ones_f[:], Dg[:, 4 * j:4 * j + 4, :].rearrange("p h l -> p (h l)"), True, True,
                                ['ones_f', 'ss_Dg'], [pj.name])
                        self.cp(arow_[:, 4 * j:4 * j + 4, i * 128:(i + 1) * 128], pj[:].rearrange("p (h l) -> p h l", h=4), [pj.name],
                                ['ss_acr'], eng='act' if j else 'dve')
                self.act(expA[:], arow_[:], AF.Exp, ['ss_acr'], ['ss_expA'])
                for i in range(2):
                    self.tt(dte[:, i, :], arow_[:, :, 255], acum[:, i, :], ALU.subtract, ['ss_acr', 'ss_acum'], ['ss_dte'])
                self.act(dte[:], dte[:], AF.Exp, ['ss_dte'], ['ss_dte'])
                for i in range(2):
                    px = self.ps[i]
                    for h in range(8):
                        self.tr(px[:, h * 64:(h + 1) * 64], xsT[:, h, i * 128:(i + 1) * 128], self.ident_f[0:64, 0:64],
                                ['ss_xsT', 'ident_f'], [px.name])
                    self.tt(xdt[:, i], px[:].rearrange("p (h d) -> p h d", h=8), dt[:, i, :].unsqueeze(2).broadcast_to([128, 8, 64]),
                            ALU.mult, [px.name, 'ss_dt'], ['ss_xdt'])
                    pbm = self.ps[2]
                    for g in range(2):
                        self.tr(pbm[:, g * 128:(g + 1) * 128], BT[:, g, i * 128:(i + 1) * 128], self.ident_f[:], ['ss_BT', 'ident_f'],
                                [pbm.name])
                    self.cp(Btm[:, i].rearrange("p g n -> p (g n)"), pbm[:, 0:256], [pbm.name], ['ss_Btm'])
                    for h in range(8):
                        self.ts(Bdec[:, i, h, :], Btm[:, i, h // 4, :], dte[:, i, h:h + 1], ALU.mult, ['ss_Btm', 'ss_dte'], ['ss_Bdec'],
                                eng='pool' if h % 2 else 'dve')
                for g in range(2):
                    for i in range(2):
                        pc_ = self.ps[(g * 2 + i) % 2]
                        self.mm(pc_[:, 0:256], BTb[:, g, i * 128:(i + 1) * 128], CTb[:, g, :], True, True, ['ss_BTb', 'ss_CTb'],
                                [pc_.name])
                        self.cp(CB[:, g, i, :], pc_[:, 0:256], [pc_.name], ['ss_CB'], eng='act' if i else 'dve')
                for h in range(8):
                    self.tt(Cdec[:, h, :], CT[:, h // 4, :], expA[:, h, :], ALU.mult, ['ss_CT', 'ss_expA'], ['ss_Cdec'],
                            eng='pool' if h % 2 else 'dve')
                for h in range(8):
                    g = h // 4
                    d0, d1 = dif[0], dif[1]
                    m0, m1 = M0[h % 2], M1[h % 2]
                    self.ts(d0[:], arow_[:, h, :], acum[:, 0, h:h + 1], ALU.subtract, ['ss_acr', 'ss_acum'], [d0.name])
                    self.tt(d0[:, 0:128], d0[:, 0:128], self.cmneg[:], ALU.add, [d0.name, 'cmneg'], [d0.name])
                    self.act(d0[:], d0[:], AF.Exp, [d0.name], [d0.name])
                    self.tt(m0[:], CB[:, g, 0, :], d0[:], ALU.mult, ['ss_CB', d0.name], [m0.name])
                    self.ts(d1[:, 0:128], arow_[:, h, 128:256], acum[:, 1, h:h + 1], ALU.subtract, ['ss_acr', 'ss_acum'], [d1.name])
                    self.tt(d1[:, 0:128], d1[:, 0:128], self.cmneg[:], ALU.add, [d1.name, 'cmneg'], [d1.name])
                    self.act(d1[:, 0:128], d1[:, 0:128], AF.Exp, [d1.name], [d1.name])
                    self.tt(m1[:], CB[:, g, 1, 128:256], d1[:, 0:128], ALU.mult, ['ss_CB', d1.name], [m1.name])
                    py = self.ps[2 + h % 2]
                    hn = 'ss_hTb%d' % h
                    self.mm(py[0:64, 0:256], xdt[:, 0, h, :], m0[:], True, False, ['ss_xdt', m0.name], [py.name])
                    self.mm(py[0:64, 128:256], xdt[:, 1, h, :], m1[:], False, False, ['ss_xdt', m1.name], [py.name])
                    self.mm(py[0:64, 0:256], hTb[:, h, :], Cdec[:, h, :], False, True, [hn, 'ss_Cdec'], [py.name])
                    self.stt(yall[:, h, :], xsT[:, h, :], dsk[:, h:h + 1], py[0:64, 0:256], ALU.mult, ALU.add,
                             ['ss_xsT', 'ss_dsk', py.name], ['ss_yall'])
                    pst = self.ps[h % 2]
                    self.mm(pst[:, 0:64], Bdec[:, 0, h, :], xdt[:, 0, h, :], True, False, ['ss_Bdec', 'ss_xdt'], [pst.name])
                    self.mm(pst[:, 0:64], Bdec[:, 1, h, :], xdt[:, 1, h, :], False, True, ['ss_Bdec', 'ss_xdt'], [pst.name])
                    self.stt(hT[:, h, :], hT[:, h, :], expA[:, h, 255:256], pst[:, 0:64], ALU.mult, ALU.add,
                             ['ss_hT', 'ss_expA', pst.name], ['ss_hT'])
                    self.cp(hTb[:, h, :], hT[:, h, :], ['ss_hT'], [hn], eng='pool')
                self.act(sz[:], zT[:], AF.Silu, ['ss_zT'], ['ss_sz'])
                self.tt(yall[:], yall[:], sz[:], ALU.mult, ['ss_yall', 'ss_sz'], ['ss_yall'])
                self.tt(sz[:], yall[:], yall[:], ALU.mult, ['ss_yall'], ['ss_sz'])
                pss = self.ps[4]
                for h in range(8):
                    self.mm(pss[0:64, 0:256], self.ones_f[0:64, 0:64], sz[:, h, :], h == 0, h == 7, ['ones_f', 'ss_sz'], [pss.name])
                self.ts(rstd[:], pss[0:64, 0:256], 1.0 / 512, ALU.mult, [pss.name], ['ss_rstd'], s2=LN_EPS, op1=ALU.add)
                self.act(rstd[:], rstd[:], AF.Sqrt, ['ss_rstd'], ['ss_rstd'])
                self.S.op('dve', lambda e: e.reciprocal(out=rstd[:], in_=rstd[:]), reads=['ss_rstd'], writes=['ss_rstd'])
                for h in range(8):
                    self.stt(ob[:, h, :], yall[:, h, :], nw[:, h:h + 1], rstd[:], ALU.mult, ALU.mult, ['ss_yall', 'ss_nw', 'ss_rstd'],
                             ['ss_ob'])
                self.dma(Dm['oT'][512:1024, cols].rearrange("(h p) t -> p h t", p=64), ob[:], ['ss_ob'], [], q='pool')
            self.S.flush()

    def phase_merge(self, l):
        T, NT, Dm, I = self.T, self.NT, self.Dm, self.I
        with contextlib.ExitStack() as st:
            wb = self.sb(st, "mr_w", [128, 16, D_MODEL], BF16)
            self.dma(wb[:], I['w_branch'][l].rearrange("g (c p) n -> p (g c) n", p=128), [], ['mr_w'], q='pool')
            oT = [self.sb(st, "mr_o%d" % i, [128, 16, 128], BF16) for i in range(2)]
            gt = [self.sb(st, "mr_g%d" % i, [128, 4, D_MODEL], BF16) for i in range(2)]
            mg = [self.sb(st, "mr_m%d" % i, [128, D_MODEL], F32) for i in range(2)]
            tmp = [self.sb(st, "mr_t%d" % i, [128, 512], F32) for i in range(2)]
            mb = self.sb(st, "mr_mb", [128, D_MODEL], BF16)
            mT = [self.sb(st, "mr_mT%d" % i, [128, 16, 128], BF16) for i in range(2)]
            oTv = Dm['oT'].rearrange("(c p) t -> p c t", p=128)
            k = 0
            for n in range(NT):
                b = n % 2
                cols = slice(n * 128, (n + 1) * 128)
                self.dma(oT[b][:], oTv[:, :, cols], [], [oT[b].name])
                self.dma(gt[b][:], Dm['gates'][cols, :].rearrange("t (g d) -> t g d", g=4), [], [gt[b].name])
                for cc in range(4):
                    cs_ = slice(cc * 512, (cc + 1) * 512)
                    for g in range(4):
                        ps = self.ps[k % 4]
                        k += 1
                        for kc in range(4):
                            self.mm(ps[:], oT[b][:, g * 4 + kc, :], wb[:, g * 4 + kc, cs_], kc == 0, kc == 3, [oT[b].name, 'mr_w'],
                                    [ps.name])
                        if g == 0:
                            self.tt(mg[b][:, cs_], ps[:], gt[b][:, 0, cs_], ALU.mult, [ps.name, gt[b].name], [mg[b].name])
                        else:
                            t_ = tmp[g % 2]
                            self.tt(t_[:], ps[:], gt[b][:, g, cs_], ALU.mult, [ps.name, gt[b].name], [t_.name])
                            self.tt(mg[b][:, cs_], mg[b][:, cs_], t_[:], ALU.add, [mg[b].name, t_.name], [mg[b].name], eng='pool')
                self.cp(mb[:], mg[b][:], [mg[b].name], ['mr_mb'], eng='act')
                for g4 in range(2):
                    pbk = self.pb[g4]
                    for j in range(8):
                        c = g4 * 8 + j
                        self.tr(pbk[:, j * 128:(j + 1) * 128], mb[:, c * 128:(c + 1) * 128], self.ident_b[:], ['mr_mb', 'ident_b'],
                                [pbk.name])
                    self.cp(mT[b][:, g4 * 8:(g4 + 1) * 8, :], pbk[:].rearrange("p (a t) -> p a t", a=8), [pbk.name], [mT[b].name],
                            eng='act' if g4 else 'dve')
                self.dma(Dm['mergedT'].rearrange("(c p) t -> p c t", p=128)[:, :, cols], mT[b][:], [mT[b].name], [], q='pool')
            self.S.flush()

    def phase_wout(self, l):
        with contextlib.ExitStack() as st:
            ev = [self.sb(st, "wo_ev%d" % i, [128, 512], F32) for i in range(4)]
            cnt = [0]
            mixed = self.Dm['mixed']

            def ep(n, c0, cw, ps, pn):
                b = ev[cnt[0] % 4]
                self.cp(b[:, 0:cw], ps[:, 0:cw], [pn], [b.name], eng='act' if cnt[0] % 2 else 'dve')
                cnt[0] += 1
                self.dma(mixed[n * 128:(n + 1) * 128, c0:c0 + cw], b[:, 0:cw], [b.name], [], q='pool')
            self.linear_stream(st, self.Dm['mergedT'], D_MODEL, self.I['w_out'][l], D_MODEL, ep, "wo")
            self.S.flush()

    def layer_norm_tile(self, v, vn, g_bc, b_bc, junk, stat, out, outn):
        self.act(junk[:], v[:], AF.Identity, [vn], [junk.name, stat.name], accum=stat[:, 0:1])
        self.ts(stat[:, 0:1], stat[:, 0:1], 1.0 / D_MODEL, ALU.mult, [stat.name], [stat.name])
        self.ts(v[:], v[:], stat[:, 0:1], ALU.subtract, [vn, stat.name], [vn])
        self.act(junk[:], v[:], AF.Square, [vn], [junk.name, stat.name], accum=stat[:, 1:2])
        self.ts(stat[:, 1:2], stat[:, 1:2], 1.0 / D_MODEL, ALU.mult, [stat.name], [stat.name], s2=LN_EPS, op1=ALU.add)
        self.act(stat[:, 1:2], stat[:, 1:2], AF.Sqrt, [stat.name], [stat.name])
        self.S.op('dve', lambda e: e.reciprocal(out=stat[:, 1:2], in_=stat[:, 1:2]), reads=[stat.name], writes=[stat.name])
        self.stt(out[:], v[:], stat[:, 1:2], g_bc[:], ALU.mult, ALU.mult, [vn, stat.name, g_bc.name], [outn])
        self.tt(out[:], out[:], b_bc[:], ALU.add, [outn, b_bc.name], [outn], eng='pool')

    def phase_ln1_router(self, l, src):
        T, NT, Dm, I = self.T, self.NT, self.Dm, self.I
        bc = lambda ap: ap.rearrange("(o n) -> o n", o=1).partition_broadcast(128)
        with contextlib.ExitStack() as st:
            g_bc = self.sb(st, "l1_g", [128, D_MODEL], F32)
            b_bc = self.sb(st, "l1_b", [128, D_MODEL], F32)
            rw = self.sb(st, "l1_rw", [128, 16, N_EXPERTS], F32)
            rb = self.sb(st, "l1_rb", [128, N_EXPERTS], F32)
            self.dma(g_bc[:], bc(I['ln1_g'][l]), [], ['l1_g'])
            self.dma(b_bc[:], bc(I['ln1_b'][l]), [], ['l1_b'])
            self.dma(rw[:], I['router_w'][l].rearrange("(c p) e -> p c e", p=128), [], ['l1_rw'])
            self.dma(rb[:], bc(I['router_b'][l]), [], ['l1_rb'])
            xin = [self.sb(st, "l1_x%d" % i, [128, D_MODEL], F32) for i in range(2)]
            mx_ = [self.sb(st, "l1_m%d" % i, [128, D_MODEL], F32) for i in range(2)]
            x1 = [self.sb(st, "l1_o%d" % i, [128, D_MODEL], F32) for i in range(2)]
            junk = self.sb(st, "l1_junk", [128, D_MODEL], F32)
            stat = self.sb(st, "l1_stat", [128, 2], F32)
            xtf = self.sb(st, "l1_xtf", [128, 16, 128], F32)
            xts = [self.sb(st, "l1_xts%d" % i, [128, 16, 128], BF16) for i in range(2)]
            lg = self.sb(st, "l1_lg", [128, N_EXPERTS], F32)
            m8 = self.sb(st, "l1_m8", [128, 8], F32)
            sel = self.sb(st, "l1_sel", [128, N_EXPERTS], F32)
            ex = self.sb(st, "l1_ex", [128, N_EXPERTS], F32)
            den = self.sb(st, "l1_den", [128, 2], F32)
            if self.sparse:
                i8 = self.sb(st, "l1_i8", [128, 8], U32)
                i8f = self.sb(st, "l1_i8f", [128, 8], F32)
                e4 = self.sb(st, "l1_e4", [128, 4], F32)
                den4 = self.sb(st, "l1_den4", [128, 1], F32)
                selb = self.sb(st, "l1_selb", [128, N_EXPERTS], BF16)
                dfull = self.sb(st, "l1_dfull", [128, N_EXPERTS], F32)
                basecap = self.sb(st, "l1_basecap", [128, N_EXPERTS], F32)
                oh = self.sb(st, "l1_oh", [128, N_EXPERTS], F32)
                d4f = self.sb(st, "l1_d4f", [128, 4], F32)
                d4i = [self.sb(st, "l1_d4i%d" % i, [128, 4], I32) for i in range(2)]
                xb16 = [self.sb(st, "l1_xb%d" % i, [128, D_MODEL], BF16) for i in range(2)]
                self.ts(basecap[:], self.erow[:], float(self.CAP), ALU.mult, ['erow'], ['l1_basecap'])
            for n in range(NT):
                b = n % 2
                rows = slice(n * 128, (n + 1) * 128)
                self.dma(xin[b][:], src[rows, :], [], [xin[b].name])
                self.dma(mx_[b][:], Dm['mixed'][rows, :], [], [mx_[b].name])
                self.stt(mx_[b][:], xin[b][:], DN_ALPHA, mx_[b][:], ALU.mult, ALU.add, [xin[b].name, mx_[b].name], [mx_[b].name])
                self.layer_norm_tile(mx_[b], mx_[b].name, g_bc, b_bc, junk, stat, x1[b], x1[b].name)
                self.dma(Dm['x1'][rows, :], x1[b][:], [x1[b].name], [], q='pool')
                for g in range(4):
                    ps = self.ps[g]
                    for j in range(4):
                        c = g * 4 + j
                        self.tr(ps[:, j * 128:(j + 1) * 128], x1[b][:, c * 128:(c + 1) * 128], self.ident_f[:], [x1[b].name, 'ident_f'],
                                [ps.name])
                    self.cp(xtf[:, g * 4:(g + 1) * 4, :], ps[:].rearrange("p (a t) -> p a t", a=4), [ps.name], ['l1_xtf'],
                            eng='act' if g % 2 else 'dve')
                self.cp(xts[b][:], xtf[:], ['l1_xtf'], [xts[b].name], eng='pool')
                self.dma(Dm['x1T'].rearrange("(c p) t -> p c t", p=128)[:, :, rows], xts[b][:], [xts[b].name], [], q='pool')
                pl = self.ps[4 + n % 2]
                for kc in range(16):
                    self.mm(pl[:, 0:N_EXPERTS], xtf[:, kc, :], rw[:, kc, :], kc == 0, kc == 15, ['l1_xtf', 'l1_rw'], [pl.name])
                self.tt(lg[:], pl[:, 0:N_EXPERTS], rb[:], ALU.add, [pl.name, 'l1_rb'], ['l1_lg'])
                self.S.op('dve', lambda e: e.max(out=m8[:], in_=lg[:]), reads=['l1_lg'], writes=['l1_m8'])
                self.ts(sel[:], lg[:], m8[:, 3:4], ALU.is_ge, ['l1_lg', 'l1_m8'], ['l1_sel'])
                self.ts(den[:, 0:1], m8[:, 0:1], -1.0, ALU.mult, ['l1_m8'], ['l1_den'])
                self.act(ex[:], lg[:], AF.Exp, ['l1_lg', 'l1_den'], ['l1_ex'], bias=den[:, 0:1])
                self.tt(ex[:], ex[:], sel[:], ALU.mult, ['l1_ex', 'l1_sel'], ['l1_ex'])
                self.S.op('dve', lambda e: e.tensor_reduce(out=den[:, 1:2], in_=ex[:], axis=AX.X, op=ALU.add), reads=['l1_ex'],
                          writes=['l1_den'])
                self.S.op('dve', lambda e: e.reciprocal(out=den[:, 1:2], in_=den[:, 1:2]), reads=['l1_den'], writes=['l1_den'])
                self.ts(sel[:], ex[:], den[:, 1:2], ALU.mult, ['l1_ex', 'l1_den'], ['l1_sel'])
                self.dma(Dm['gsel'][rows, :], sel[:], ['l1_sel'], [], q='pool')
                if self.sparse:
                    self.S.op('dve', lambda e: e.max_index(out=i8[:], in_max=m8[:], in_values=lg[:]), reads=['l1_lg', 'l1_m8'],
                              writes=['l1_i8'])
                    self.cp(i8f[:], i8[:], ['l1_i8'], ['l1_i8f'])
                    self.act(e4[:], m8[:, 0:4], AF.Exp, ['l1_m8', 'l1_den'], ['l1_e4'], bias=den[:, 0:1])
                    self.S.op('dve', lambda e: e.tensor_reduce(out=den4[:, 0:1], in_=e4[:], axis=AX.X, op=ALU.add), reads=['l1_e4'],
                              writes=['l1_den4'])
                    self.S.op('dve', lambda e: e.reciprocal(out=den4[:, 0:1], in_=den4[:, 0:1]), reads=['l1_den4'], writes=['l1_den4'])
                    self.ts(e4[:], e4[:], den4[:, 0:1], ALU.mult, ['l1_e4', 'l1_den4'], ['l1_e4'])
                    self.dma(Dm['gsel4'][rows, :], e4[:], ['l1_e4'], [], q='pool')
                    self.ts(selb[:], lg[:], m8[:, 3:4], ALU.is_ge, ['l1_lg', 'l1_m8'], ['l1_selb'])
                    pp = self.ps[4 + (n + 1) % 2]
                    self.mm(pp[:, 0:N_EXPERTS], self.tris_b[:], selb[:], True, True, ['tris_b', 'l1_selb'], [pp.name])
                    self.mm(pp[:, 64:64 + N_EXPERTS], self.ones_b[:], selb[:], True, True, ['ones_b', 'l1_selb'], [pp.name])
                    self.tt(dfull[:], pp[:, 0:N_EXPERTS], basecap[:], ALU.add, [pp.name, 'l1_basecap'], ['l1_dfull'])
                    self.tt(basecap[:], pp[:, 64:64 + N_EXPERTS], basecap[:], ALU.add, [pp.name, 'l1_basecap', 'l1_dfull'], ['l1_basecap'])
                    for k4 in range(4):
                        self.ts(oh[:], self.erow[:], i8f[:, k4:k4 + 1], ALU.is_equal, ['erow', 'l1_i8f'], ['l1_oh'])
                        self.tt(oh[:], oh[:], dfull[:], ALU.mult, ['l1_oh', 'l1_dfull'], ['l1_oh'])
                        self.S.op('dve', lambda e, k4=k4: e.tensor_reduce(out=d4f[:, k4:k4 + 1], in_=oh[:], axis=AX.X, op=ALU.add),
                                  reads=['l1_oh'], writes=['l1_d4f'])
                    d4 = d4i[b]
                    self.cp(d4[:], d4f[:], ['l1_d4f'], [d4.name])
                    self.dma(Dm['dest4'][rows, :], d4[:], [d4.name], [], q='pool')
                    self.cp(xb16[b][:], x1[b][:], [x1[b].name], [xb16[b].name], eng='pool')
                    for k4 in range(4):
                        self.S.dma('pool', lambda e, k4=k4, d4=d4, xb=xb16[b]: e.indirect_dma_start(
                            out=Dm['xg'], out_offset=bass.IndirectOffsetOnAxis(ap=d4[:, k4:k4 + 1].bitcast(U32), axis=0),
                            in_=xb[:], in_offset=None), reads=[d4.name, xb16[b].name], writes=[])
            self.S.flush()

    def phase_moe_dense(self, l):
        T, NT, Dm, I = self.T, self.NT, self.Dm, self.I
        bc = lambda ap: ap.rearrange("(o n) -> o n", o=1).partition_broadcast(128)
        with contextlib.ExitStack() as st:
            wgu = self.sb(st, "mo_wgu", [128, 16, 2 * D_FF], BF16)
            wd = [self.sb(st, "mo_wd%d" % i, [128, 6, D_MODEL], BF16) for i in range(2)]
            bgu = [self.sb(st, "mo_bgu%d" % i, [128, 2 * D_FF], F32) for i in range(2)]
            bd = [self.sb(st, "mo_bd%d" % i, [128, D_MODEL], F32) for i in range(2)]
            gs = self.sb(st, "mo_gs", [128, NT, N_EXPERTS], F32)
            xt = [self.sb(st, "mo_x%d" % i, [128, 16, 128], BF16) for i in range(2)]
            hb = self.sb(st, "mo_hb", [128, 2 * D_FF], F32)
            sg = self.sb(st, "mo_sg", [128, D_FF], F32)
            ab = self.sb(st, "mo_ab", [128, D_FF], BF16)
            aT = self.sb(st, "mo_aT", [128, 6, 128], BF16)
            yp = [self.sb(st, "mo_yp%d" % i, [128, D_MODEL], F32) for i in range(2)]
            tq = [self.sb(st, "mo_tq%d" % i, [128, 512], F32) for i in range(2)]
            self.dma(gs[:], Dm['gsel'].rearrange("(n p) e -> p n e", p=128), [], ['mo_gs'])
            x1Tv = Dm['x1T'].rearrange("(c p) t -> p c t", p=128)
            k = 0
            for e in range(N_EXPERTS):
                eb = e % 2
                self.dma(wgu[:], I['w_gate_up'][l, e].rearrange("(c p) n -> p c n", p=128), [], ['mo_wgu'], q='pool')
                self.dma(wd[eb][:], I['w_down'][l, e].rearrange("(c p) n -> p c n", p=128), [], [wd[eb].name], q='pool')
                self.dma(bgu[eb][:], bc(I['b_gate_up'][l, e]), [], [bgu[eb].name])
                self.dma(bd[eb][:], bc(I['b_down'][l, e]), [], [bd[eb].name])
                for n in range(NT):
                    b = k % 2
                    k += 1
                    rows = slice(n * 128, (n + 1) * 128)
                    self.dma(xt[b][:], x1Tv[:, :, rows], [], [xt[b].name])
                    if e > 0:
                        self.dma(yp[b][:], Dm['yacc'][rows, :], ['yacc%d' % n], [yp[b].name])
                    for cc in range(3):
                        ps = self.ps[cc]
                        cs_ = slice(cc * 512, (cc + 1) * 512)
                        for kc in range(16):
                            self.mm(ps[:], xt[b][:, kc, :], wgu[:, kc, cs_], kc == 0, kc == 15, [xt[b].name, 'mo_wgu'], [ps.name])
                        self.tt(hb[:, cs_], ps[:], bgu[eb][:, cs_], ALU.add, [ps.name, bgu[eb].name], ['mo_hb'])
                    self.ts(hb[:, 0:D_FF], hb[:, 0:D_FF], 7.0, ALU.min, ['mo_hb'], ['mo_hb'])
                    self.act(sg[:], hb[:, 0:D_FF], AF.Sigmoid, ['mo_hb'], ['mo_sg'], scale=1.702)
                    self.ts(hb[:, D_FF:], hb[:, D_FF:], 7.0, ALU.min, ['mo_hb'], ['mo_hb'], s2=-7.0, op1=ALU.max)
                    self.tt(sg[:], sg[:], hb[:, 0:D_FF], ALU.mult, ['mo_sg', 'mo_hb'], ['mo_sg'], eng='pool')
                    self.stt(ab[:], hb[:, D_FF:], 1.0, sg[:], ALU.add, ALU.mult, ['mo_hb', 'mo_sg'], ['mo_ab'])
                    pbk = self.pb[k % 2]
                    for j in range(6):
                        self.tr(pbk[:, j * 128:(j + 1) * 128], ab[:, j * 128:(j + 1) * 128], self.ident_b[:], ['mo_ab', 'ident_b'],
                                [pbk.name])
                    self.cp(aT[:], pbk[:, 0:768].rearrange("p (a t) -> p a t", a=6), [pbk.name], ['mo_aT'], eng='act')
                    for cc in range(4):
                        ps = self.ps[(3 + cc) % 6]
                        cs_ = slice(cc * 512, (cc + 1) * 512)
                        for kc in range(6):
                            self.mm(ps[:], aT[:, kc, :], wd[eb][:, kc, cs_], kc == 0, kc == 5, ['mo_aT', wd[eb].name], [ps.name])
                        t_ = tq[cc % 2]
                        self.tt(t_[:], ps[:], bd[eb][:, cs_], ALU.add, [ps.name, bd[eb].name], [t_.name])
                        if e == 0:
                            self.ts(yp[b][:, cs_], t_[:], gs[:, n, e:e + 1], ALU.mult, [t_.name, 'mo_gs'], [yp[b].name])
                        else:
                            self.stt(yp[b][:, cs_], t_[:], gs[:, n, e:e + 1], yp[b][:, cs_], ALU.mult, ALU.add,
                                     [t_.name, 'mo_gs', yp[b].name], [yp[b].name])
                    self.dma(Dm['yacc'][rows, :], yp[b][:], [yp[b].name], ['yacc%d' % n], q='pool')
            self.S.flush()

    def phase_moe_zero(self):
        with contextlib.ExitStack() as st:
            z = self.sb(st, "mz_z", [128, D_MODEL], BF16)
            self.memset(z[:], 0.0, ['mz_z'])
            for r in range(N_EXPERTS * self.CAP // 128):
                self.dma(self.Dm['xg'][r * 128:(r + 1) * 128, :], z[:], ['mz_z'], [], q='sp' if r % 2 else 'pool')
            self.S.flush()

    def phase_moe_sparse(self, l):
        T, NT, Dm, I, CAP = self.T, self.NT, self.Dm, self.I, self.CAP
        bc = lambda ap: ap.rearrange("(o n) -> o n", o=1).partition_broadcast(128)
        with contextlib.ExitStack() as st:
            wgu1 = self.sb(st, "ms_wgu", [128, 16, 2 * D_FF], BF16)
            wgu = [wgu1, wgu1]
            wd = [self.sb(st, "ms_wd%d" % i, [128, 6, D_MODEL], BF16) for i in range(2)]
            bgu = [self.sb(st, "ms_bgu%d" % i, [128, 2 * D_FF], F32) for i in range(2)]
            bd = [self.sb(st, "ms_bd%d" % i, [128, D_MODEL], F32) for i in range(2)]
            xr = [self.sb(st, "ms_xr%d" % i, [128, D_MODEL], BF16) for i in range(2)]
            xt = self.sb(st, "ms_xt", [128, 16, 128], BF16)
            hb = self.sb(st, "ms_hb", [128, 2 * D_FF], F32)
            sg = self.sb(st, "ms_sg", [128, D_FF], F32)
            ab = self.sb(st, "ms_ab", [128, D_FF], BF16)
            aT = self.sb(st, "ms_aT", [128, 6, 128], BF16)
            yo = [self.sb(st, "ms_yo%d" % i, [128, D_MODEL], BF16) for i in range(2)]
            k = 0
            for e in range(N_EXPERTS):
                eb = e % 2
                self.dma(wgu[eb][:], I['w_gate_up'][l, e].rearrange("(c p) n -> p c n", p=128), [], [wgu[eb].name], q='pool')
                self.dma(wd[eb][:], I['w_down'][l, e].rearrange("(c p) n -> p c n", p=128), [], [wd[eb].name], q='pool')
                self.dma(bgu[eb][:], bc(I['b_gate_up'][l, e]), [], [bgu[eb].name])
                self.dma(bd[eb][:], bc(I['b_down'][l, e]), [], [bd[eb].name])
                for j in range(CAP // 128):
                    b = k % 2
                    k += 1
                    rows = slice(e * CAP + j * 128, e * CAP + (j + 1) * 128)
                    self.dma(xr[b][:], Dm['xg'][rows, :], [], [xr[b].name])
                    for g4 in range(2):
                        pbk = self.pb[g4]
                        for jj in range(8):
                            c = g4 * 8 + jj
                            self.tr(pbk[:, jj * 128:(jj + 1) * 128], xr[b][:, c * 128:(c + 1) * 128], self.ident_b[:],
                                    [xr[b].name, 'ident_b'], [pbk.name])
                        self.cp(xt[:, g4 * 8:(g4 + 1) * 8, :], pbk[:].rearrange("p (a t) -> p a t", a=8), [pbk.name], ['ms_xt'],
                                eng='act' if g4 else 'dve')
                    for cc in range(3):
                        ps = self.ps[cc]
                        cs_ = slice(cc * 512, (cc + 1) * 512)
                        for kc in range(16):
                            self.mm(ps[:], xt[:, kc, :], wgu[eb][:, kc, cs_], kc == 0, kc == 15, ['ms_xt', wgu[eb].name], [ps.name])
                        self.tt(hb[:, cs_], ps[:], bgu[eb][:, cs_], ALU.add, [ps.name, bgu[eb].name], ['ms_hb'])
                    self.ts(hb[:, 0:D_FF], hb[:, 0:D_FF], 7.0, ALU.min, ['ms_hb'], ['ms_hb'])
                    self.act(sg[:], hb[:, 0:D_FF], AF.Sigmoid, ['ms_hb'], ['ms_sg'], scale=1.702)
                    self.ts(hb[:, D_FF:], hb[:, D_FF:], 7.0, ALU.min, ['ms_hb'], ['ms_hb'], s2=-7.0, op1=ALU.max)
                    self.tt(sg[:], sg[:], hb[:, 0:D_FF], ALU.mult, ['ms_sg', 'ms_hb'], ['ms_sg'], eng='pool')
                    self.stt(ab[:], hb[:, D_FF:], 1.0, sg[:], ALU.add, ALU.mult, ['ms_hb', 'ms_sg'], ['ms_ab'])
                    pbk = self.pb[k % 2]
                    for jj in range(6):
                        self.tr(pbk[:, jj * 128:(jj + 1) * 128], ab[:, jj * 128:(jj + 1) * 128], self.ident_b[:], ['ms_ab', 'ident_b'],
                                [pbk.name])
                    self.cp(aT[:], pbk[:, 0:768].rearrange("p (a t) -> p a t", a=6), [pbk.name], ['ms_aT'], eng='act')
                    for cc in range(4):
                        ps = self.ps[(3 + cc) % 6]
                        cs_ = slice(cc * 512, (cc + 1) * 512)
                        for kc in range(6):
                            self.mm(ps[:], aT[:, kc, :], wd[eb][:, kc, cs_], kc == 0, kc == 5, ['ms_aT', wd[eb].name], [ps.name])
                        self.tt(yo[b][:, cs_], ps[:], bd[eb][:, cs_], ALU.add, [ps.name, bd[eb].name], [yo[b].name])
                    self.dma(Dm['og'][rows, :], yo[b][:], [yo[b].name], [], q='sp')
            self.S.flush()

    def phase_ln2(self, l, last):
        T, NT, Dm, I = self.T, self.NT, self.Dm, self.I
        bc = lambda ap: ap.rearrange("(o n) -> o n", o=1).partition_broadcast(128)
        with contextlib.ExitStack() as st:
            g_bc = self.sb(st, "l2_g", [128, D_MODEL], F32)
            b_bc = self.sb(st, "l2_b", [128, D_MODEL], F32)
            self.dma(g_bc[:], bc(I['ln2_g'][l]), [], ['l2_g'])
            self.dma(b_bc[:], bc(I['ln2_b'][l]), [], ['l2_b'])
            xin = [self.sb(st, "l2_x%d" % i, [128, D_MODEL], F32) for i in range(2)]
            yy = [self.sb(st, "l2_y%d" % i, [128, D_MODEL], F32) for i in range(2)]
            xo = [self.sb(st, "l2_o%d" % i, [128, D_MODEL], F32) for i in range(2)]
            junk = self.sb(st, "l2_junk", [128, D_MODEL], F32)
            stat = self.sb(st, "l2_stat", [128, 2], F32)
            xts = [self.sb(st, "l2_xts%d" % i, [128, 16, 128], BF16) for i in range(2)]
            if self.sparse:
                d4 = [self.sb(st, "l2_d4%d" % i, [128, 4], I32) for i in range(2)]
                g4 = [self.sb(st, "l2_g4%d" % i, [128, 4], F32) for i in range(2)]
                gk = [self.sb(st, "l2_gk%d" % i, [128, D_MODEL], BF16) for i in range(2)]
            dst = self.out if last else Dm['xres']
            for n in range(NT):
                b = n % 2
                rows = slice(n * 128, (n + 1) * 128)
                self.dma(xin[b][:], Dm['x1'][rows, :], [], [xin[b].name])
                if self.sparse:
                    self.dma(d4[b][:], Dm['dest4'][rows, :], [], [d4[b].name])
                    self.dma(g4[b][:], Dm['gsel4'][rows, :], [], [g4[b].name])
                    for k4 in range(4):
                        gk_ = gk[k4 % 2]
                        self.S.dma('pool', lambda e, k4=k4, gk_=gk_, dd=d4[b]: e.indirect_dma_start(
                            out=gk_[:], out_offset=None, in_=Dm['og'],
                            in_offset=bass.IndirectOffsetOnAxis(ap=dd[:, k4:k4 + 1].bitcast(U32), axis=0)),
                            reads=[d4[b].name], writes=[gk_.name])
                        if k4 == 0:
                            self.ts(yy[b][:], gk_[:], g4[b][:, 0:1], ALU.mult, [gk_.name, g4[b].name], [yy[b].name])
                        else:
                            self.stt(yy[b][:], gk_[:], g4[b][:, k4:k4 + 1], yy[b][:], ALU.mult, ALU.add,
                                     [gk_.name, g4[b].name, yy[b].name], [yy[b].name])
                else:
                    self.dma(yy[b][:], Dm['yacc'][rows, :], [], [yy[b].name])
                self.stt(yy[b][:], xin[b][:], DN_ALPHA, yy[b][:], ALU.mult, ALU.add, [xin[b].name, yy[b].name], [yy[b].name])
                self.layer_norm_tile(yy[b], yy[b].name, g_bc, b_bc, junk, stat, xo[b], xo[b].name)
                self.dma(dst[rows, :], xo[b][:], [xo[b].name], [], q='pool')
                if not last:
                    self.emit_xT(xo[b], xo[b].name, n, Dm['xT'], xts[b], n)
            self.S.flush()

    def finish(self):
        for nm in self.dbg:
            src = self.Dm[nm]
            o = self.nc.dram_tensor("dbg_" + nm, list(src.shape), src.dtype, kind="ExternalOutput").ap()
            self.dma(o, src, [], ['dbgout'], q='sp')
        self.S.flush()


_CACHE = {}


def kernel(**inputs):
    T, L = SEQ, DEPTH
    if 'nc' not in _CACHE:
        _CACHE['kb'] = KB(T, L)
        _CACHE['nc'] = _CACHE['kb'].build()
    nc = _CACHE['nc']
    kb = _CACHE['kb']
    im = {k: np.ascontiguousarray(inputs[k]) for k in kb.I.keys()}
    im['x'] = im['x'].reshape(T, D_MODEL)
    res = run_bass_kernel_spmd(nc, [im], core_ids=[0])
    return res.results[0]['out'].reshape(1, T, D_MODEL)
```
